# Optimizing a Trainium2 kernel written in Bass

```python
import math
import jax
import jax.numpy as jnp
from jax import lax
import numpy as np

D_MODEL = 1024
BATCH = 16
SEQ = 2048
DEPTH = 4

CTX_LEN = 256
GRID_W = 64
N_MIXERS = 2
N_ATTN_LAYERS = (DEPTH + 1) // 2
N_SSM_LAYERS = DEPTH // 2
DIFF_HEADS = 8
DIFF_HEAD_DIM = D_MODEL // DIFF_HEADS // 2
DIFF_V_DIM = 2 * DIFF_HEAD_DIM
Q_BLOCK = 128
ROPE_BASE = 10000.0
ROPE_F = DIFF_HEAD_DIM // 4
SSM_GROUP = 16
SSM_GROUPS = D_MODEL // SSM_GROUP
SSM_STATE = 64
SSM_CHUNK = 128
SSM_DIRS = 2
DT_MIN = 0.001
DT_MAX = 0.1
N_EXPERTS = 16
EXPERT_FF = 2 * D_MODEL
EC_CAPACITY_FACTOR = 2
N_MOD = 6
EPS = 1e-6

kernel_name = 'hybrid_diffattn_s5_ecmoe_dit'


def rmsnorm(x, g):
    xf = x.astype(jnp.float32)
    y = xf * lax.rsqrt(jnp.mean(xf * xf, axis=-1, keepdims=True) + EPS)
    return (y * g.astype(jnp.float32)).astype(x.dtype)


def modulate(x, shift, scale):
    return x * (1 + scale) + shift


def axial_rope_tables(rows):
    row = jnp.repeat(jnp.arange(rows, dtype=jnp.float32), GRID_W)
    col = jnp.tile(jnp.arange(GRID_W, dtype=jnp.float32), rows)
    inv = ROPE_BASE ** (-jnp.arange(ROPE_F, dtype=jnp.float32) / ROPE_F)
    ang = jnp.stack([row[:, None] * inv, col[:, None] * inv], axis=1)
    return jnp.cos(ang), jnp.sin(ang)


def apply_axial_rope(x, cos, sin):
    shp = x.shape
    xr = x.astype(jnp.float32).reshape(*shp[:-1], 2, 2, ROPE_F)
    x0, x1 = xr[..., 0, :], xr[..., 1, :]
    cs = cos[None, :, None, None]
    sn = sin[None, :, None, None]
    out = jnp.stack([x0 * cs - x1 * sn, x0 * sn + x1 * cs], axis=-2)
    return out.reshape(shp).astype(x.dtype)


def diff_attention(h_lat, h_ctx, w_qkv, w_o, lq1, lk1, lq2, lk2, subln_g, lam_init, cos, sin, with_ctx_out):
    B, L, _ = h_lat.shape
    H, d, dv = DIFF_HEADS, DIFF_HEAD_DIM, DIFF_V_DIM

    def project(h):
        n = h.shape[1]
        q, k, v = jnp.split(h @ w_qkv, 3, axis=-1)
        return q.reshape(B, n, H, 2, d), k.reshape(B, n, H, 2, d), v.reshape(B, n, H, dv)

    q_l, k_l, v_l = project(h_lat)
    q_c, k_c, v_c = project(h_ctx)
    q_l = apply_axial_rope(q_l, cos, sin)
    k_l = apply_axial_rope(k_l, cos, sin)
    f32 = jnp.float32
    lam = (jnp.exp(jnp.sum(lq1.astype(f32) * lk1.astype(f32)))
           - jnp.exp(jnp.sum(lq2.astype(f32) * lk2.astype(f32))) + lam_init)
    scale = d ** -0.5

    def diff_attend(q, k, v):
        s = jnp.einsum('bqhcd,bkhcd->bhcqk', q, k).astype(f32) * scale
        p = jax.nn.softmax(s, axis=-1)
        a = p[:, :, 0] - lam * p[:, :, 1]
        return jnp.einsum('bhqk,bkhe->bqhe', a.astype(v.dtype), v)

    k_all = jnp.concatenate([k_l, k_c], axis=1)
    v_all = jnp.concatenate([v_l, v_c], axis=1)
    nb = L // Q_BLOCK
    q_blocks = q_l.reshape(B, nb, Q_BLOCK, H, 2, d).transpose(1, 0, 2, 3, 4, 5)
    o_l = lax.map(lambda qb: diff_attend(qb, k_all, v_all), q_blocks)
    o_l = o_l.transpose(1, 0, 2, 3, 4).reshape(B, L, H, dv)

    def finish(o):
        o = rmsnorm(o, subln_g) * (1.0 - lam_init)
        return o.reshape(o.shape[0], o.shape[1], H * dv) @ w_o

    out_l = finish(o_l)
    out_c = finish(diff_attend(q_c, k_c, v_c)) if with_ctx_out else None
    return out_l, out_c


def s5_combine(e1, e2):
    a1r, a1i, b1r, b1i = e1
    a2r, a2i, b2r, b2i = e2
    return (a2r * a1r - a2i * a1i, a2r * a1i + a2i * a1r,
            a2r * b1r - a2i * b1i + b2r, a2r * b1i + a2i * b1r + b2i)


def s5_scan(u, lam_re, lam_im, log_dt, b_re, b_im, c_re, c_im, h_re, h_im):
    f32 = jnp.float32
    lam_re, lam_im = lam_re.astype(f32), lam_im.astype(f32)
    b_re, b_im = b_re.astype(f32), b_im.astype(f32)
    c_re, c_im = c_re.astype(f32), c_im.astype(f32)
    dt = jnp.exp(log_dt.astype(f32))[:, None]
    mag = jnp.exp(lam_re * dt)
    ang = lam_im * dt
    ab_re, ab_im = mag * jnp.cos(ang), mag * jnp.sin(ang)
    den = lam_re * lam_re + lam_im * lam_im
    nr, ni = ab_re - 1.0, ab_im
    coef_re = (nr * lam_re + ni * lam_im) / den
    coef_im = (ni * lam_re - nr * lam_im) / den
    bb_re = coef_re[..., None] * b_re - coef_im[..., None] * b_im
    bb_im = coef_re[..., None] * b_im + coef_im[..., None] * b_re
    B, T, G, P = u.shape
    nc = T // SSM_CHUNK
    u_chunks = u.reshape(B, nc, SSM_CHUNK, G, P).transpose(1, 2, 0, 3, 4)

    def chunk_step(carry, u_blk):
        s_re, s_im = carry
        bu_re = jnp.einsum('tbgp,gnp->tbgn', u_blk, bb_re)
        bu_im = jnp.einsum('tbgp,gnp->tbgn', u_blk, bb_im)
        bu_re = bu_re.at[0].add(ab_re * s_re - ab_im * s_im)
        bu_im = bu_im.at[0].add(ab_re * s_im + ab_im * s_re)
        a_re = jnp.broadcast_to(ab_re, bu_re.shape)
        a_im = jnp.broadcast_to(ab_im, bu_im.shape)
        _, _, x_re, x_im = lax.associative_scan(s5_combine, (a_re, a_im, bu_re, bu_im), axis=0)
        y = jnp.einsum('tbgn,gpn->tbgp', x_re, c_re) - jnp.einsum('tbgn,gpn->tbgp', x_im, c_im)
        return (x_re[-1], x_im[-1]), y

    (s_re, s_im), y = lax.scan(chunk_step, (h_re, h_im), u_chunks)
    y = y.transpose(2, 0, 1, 3, 4).reshape(B, T, G, P)
    return y, s_re, s_im


def s5_mixer(h_lat, h_ctx, lam_re, lam_im, log_dt, b_re, b_im, c_re, c_im, d_skip, w_glu1, w_glu2, with_ctx_out):
    B, L, Dm = h_lat.shape
    Lc = h_ctx.shape[1]
    G, P, N = SSM_GROUPS, SSM_GROUP, SSM_STATE
    f32 = jnp.float32
    u_l = h_lat.astype(f32).reshape(B, L, G, P)
    u_c = h_ctx.astype(f32).reshape(B, Lc, G, P)
    dsk = d_skip.astype(f32).reshape(G, P)
    y_l = dsk * u_l
    y_c = dsk * u_c if with_ctx_out else None
    zeros = jnp.zeros((B, G, N), f32)
    for direction in range(SSM_DIRS):
        prm = (lam_re[direction], lam_im[direction], log_dt[direction],
               b_re[direction], b_im[direction], c_re[direction], c_im[direction])
        uc = u_c if direction == 0 else u_c[:, ::-1]
        ul = u_l if direction == 0 else u_l[:, ::-1]
        yc, s_re, s_im = s5_scan(uc, *prm, zeros, zeros)
        yl, _, _ = s5_scan(ul, *prm, s_re, s_im)
        if direction == 1:
            yc, yl = yc[:, ::-1], yl[:, ::-1]
        y_l = y_l + yl
        if with_ctx_out:
            y_c = y_c + yc

    def glu(y, n):
        g = jax.nn.gelu(y.reshape(B, n, Dm)).astype(h_lat.dtype)
        return (g @ w_glu1) * jax.nn.sigmoid(g @ w_glu2)

    out_l = glu(y_l, L)
    out_c = glu(y_c, Lc) if with_ctx_out else None
    return out_l, out_c


def ec_moe(h, w_router, b_router, w_gate, w_up, w_down):
    B, n, Dm = h.shape
    cap = EC_CAPACITY_FACTOR * n // N_EXPERTS
    logits = (h @ w_router + b_router).astype(jnp.float32)
    affinity = jax.nn.softmax(logits, axis=-1)
    weight, idx = lax.top_k(jnp.swapaxes(affinity, 1, 2), cap)
    xin = jax.vmap(lambda hb, ib: hb[ib])(h, idx)
    hid = jax.nn.silu(jnp.einsum('becd,edf->becf', xin, w_gate)) * jnp.einsum('becd,edf->becf', xin, w_up)
    yout = jnp.einsum('becf,efd->becd', hid, w_down) * weight[..., None].astype(h.dtype)

    def scatter_one(ib, yb):
        return jnp.zeros((n, Dm), yb.dtype).at[ib.reshape(-1)].add(yb.reshape(-1, Dm))

    return jax.vmap(scatter_one)(idx, yout)


def setup_inputs(seed: int = 0) -> dict:
    key = jax.random.key(seed)
    ks = iter(jax.random.split(key, 40))

    def nrm(shape, s):
        return jax.random.normal(next(ks), shape, jnp.float32) * s

    D, E, F = D_MODEL, N_EXPERTS, EXPERT_FF
    G, P, N = SSM_GROUPS, SSM_GROUP, SSM_STATE
    NA, NS = N_ATTN_LAYERS, N_SSM_LAYERS
    x = nrm((BATCH, SEQ, D), 1.0)
    c = nrm((BATCH, D), 1.0)
    ctx = nrm((BATCH, CTX_LEN, D), 1.0)
    c_ctx = nrm((D,), 1.0)
    ada_w = nrm((DEPTH, D, N_MOD * D), 0.5 * D ** -0.5)
    ada_b = nrm((DEPTH, N_MOD * D), 0.02)
    norm1_g = 1.0 + nrm((DEPTH, D), 0.01)
    norm2_g = 1.0 + nrm((DEPTH, D), 0.01)
    final_g = 1.0 + nrm((D,), 0.01)
    attn_w_qkv = nrm((NA, D, 3 * D), D ** -0.5)
    attn_w_o = nrm((NA, D, D), D ** -0.5)
    attn_lam_q1 = nrm((NA, DIFF_HEAD_DIM), 0.1)
    attn_lam_k1 = nrm((NA, DIFF_HEAD_DIM), 0.1)
    attn_lam_q2 = nrm((NA, DIFF_HEAD_DIM), 0.1)
    attn_lam_k2 = nrm((NA, DIFF_HEAD_DIM), 0.1)
    attn_subln_g = 1.0 + nrm((NA, DIFF_V_DIM), 0.01)
    ssm_lam_re = -0.5 + nrm((NS, SSM_DIRS, G, N), 0.01)
    ssm_lam_im = math.pi * jnp.arange(N, dtype=jnp.float32) + nrm((NS, SSM_DIRS, G, N), 0.01)
    ssm_log_dt = jax.random.uniform(next(ks), (NS, SSM_DIRS, G), jnp.float32,
                                    math.log(DT_MIN), math.log(DT_MAX))
    ssm_b_re = nrm((NS, SSM_DIRS, G, N, P), (2 * P) ** -0.5)
    ssm_b_im = nrm((NS, SSM_DIRS, G, N, P), (2 * P) ** -0.5)
    ssm_c_re = nrm((NS, SSM_DIRS, G, P, N), (2 * N) ** -0.5)
    ssm_c_im = nrm((NS, SSM_DIRS, G, P, N), (2 * N) ** -0.5)
    ssm_d = 1.0 + nrm((NS, D), 0.1)
    ssm_w_glu1 = nrm((NS, D, D), D ** -0.5)
    ssm_w_glu2 = nrm((NS, D, D), D ** -0.5)
    moe_w_router = nrm((DEPTH, D, E), D ** -0.5)
    moe_b_router = nrm((DEPTH, E), 0.01)
    moe_w_gate = nrm((DEPTH, E, D, F), D ** -0.5)
    moe_w_up = nrm((DEPTH, E, D, F), D ** -0.5)
    moe_w_down = nrm((DEPTH, E, F, D), F ** -0.5)
    return {'x': x, 'c': c, 'ctx': ctx, 'c_ctx': c_ctx,
            'ada_w': ada_w, 'ada_b': ada_b, 'norm1_g': norm1_g, 'norm2_g': norm2_g, 'final_g': final_g,
            'attn_w_qkv': attn_w_qkv, 'attn_w_o': attn_w_o,
            'attn_lam_q1': attn_lam_q1, 'attn_lam_k1': attn_lam_k1,
            'attn_lam_q2': attn_lam_q2, 'attn_lam_k2': attn_lam_k2, 'attn_subln_g': attn_subln_g,
            'ssm_lam_re': ssm_lam_re, 'ssm_lam_im': ssm_lam_im, 'ssm_log_dt': ssm_log_dt,
            'ssm_b_re': ssm_b_re, 'ssm_b_im': ssm_b_im, 'ssm_c_re': ssm_c_re, 'ssm_c_im': ssm_c_im,
            'ssm_d': ssm_d, 'ssm_w_glu1': ssm_w_glu1, 'ssm_w_glu2': ssm_w_glu2,
            'moe_w_router': moe_w_router, 'moe_b_router': moe_b_router,
            'moe_w_gate': moe_w_gate, 'moe_w_up': moe_w_up, 'moe_w_down': moe_w_down}


def reference(x, c, ctx, c_ctx, ada_w, ada_b, norm1_g, norm2_g, final_g,
              attn_w_qkv, attn_w_o, attn_lam_q1, attn_lam_k1, attn_lam_q2, attn_lam_k2, attn_subln_g,
              ssm_lam_re, ssm_lam_im, ssm_log_dt, ssm_b_re, ssm_b_im, ssm_c_re, ssm_c_im,
              ssm_d, ssm_w_glu1, ssm_w_glu2,
              moe_w_router, moe_b_router, moe_w_gate, moe_w_up, moe_w_down):
    B, L, Dm = x.shape
    rows = L // GRID_W
    cos, sin = axial_rope_tables(rows)
    sc_lat = jax.nn.silu(c)
    sc_ctx = jax.nn.silu(c_ctx)
    for i in range(DEPTH):
        with_ctx_out = i < DEPTH - 1
        j = i // N_MIXERS
        mod_l = (sc_lat @ ada_w[i] + ada_b[i])[:, None, :]
        mod_c = sc_ctx @ ada_w[i] + ada_b[i]
        sh1_l, sc1_l, g1_l, sh2_l, sc2_l, g2_l = jnp.split(mod_l, N_MOD, axis=-1)
        sh1_c, sc1_c, g1_c, sh2_c, sc2_c, g2_c = jnp.split(mod_c, N_MOD, axis=-1)
        h_l = modulate(rmsnorm(x, norm1_g[i]), sh1_l, sc1_l)
        h_c = modulate(rmsnorm(ctx, norm1_g[i]), sh1_c, sc1_c)
        if i % N_MIXERS == 0:
            lam_init = 0.8 - 0.6 * math.exp(-0.3 * i)
            o_l, o_c = diff_attention(h_l, h_c, attn_w_qkv[j], attn_w_o[j], attn_lam_q1[j], attn_lam_k1[j],
                                      attn_lam_q2[j], attn_lam_k2[j], attn_subln_g[j], lam_init,
                                      cos, sin, with_ctx_out)
        else:
            o_l, o_c = s5_mixer(h_l, h_c, ssm_lam_re[j], ssm_lam_im[j], ssm_log_dt[j], ssm_b_re[j], ssm_b_im[j],
                                ssm_c_re[j], ssm_c_im[j], ssm_d[j], ssm_w_glu1[j], ssm_w_glu2[j], with_ctx_out)
        x = x + g1_l * o_l
        h_l = modulate(rmsnorm(x, norm2_g[i]), sh2_l, sc2_l)
        x = x + g2_l * ec_moe(h_l, moe_w_router[i], moe_b_router[i], moe_w_gate[i], moe_w_up[i], moe_w_down[i])
        if with_ctx_out:
            ctx = ctx + g1_c * o_c
            h_c = modulate(rmsnorm(ctx, norm2_g[i]), sh2_c, sc2_c)
            ctx = ctx + g2_c * ec_moe(h_c, moe_w_router[i], moe_b_router[i], moe_w_gate[i], moe_w_up[i], moe_w_down[i])
    return rmsnorm(x, final_g)
```

```python
import math
from contextlib import ExitStack
import numpy as np
import ml_dtypes
import concourse.bass as bass
import concourse.mybir as mybir
from concourse.bass_utils import run_bass_kernel_spmd

F32 = mybir.dt.float32
BF16 = mybir.dt.bfloat16
U32 = mybir.dt.uint32
I32 = mybir.dt.int32
AF = mybir.ActivationFunctionType
ALU = mybir.AluOpType

D = 1024
NL = 2048
NCX = 256
NT = NL + NCX
KC = 8
NE = 16
FF = 2048
DEPTH = 4
EPS = 1e-6
NSAMP = 2
CAPL = 256
CAPC = 32
NCOL = 2 * CAPL + 2 * CAPC


class Buf:
    __slots__ = ("name", "ap", "writer", "readers")

    def __init__(self, name, ap):
        self.name = name
        self.ap = ap
        self.writer = None
        self.readers = []


class Sync:
    NDMA = 6

    def __init__(self, nc):
        self.nc = nc
        self.engs = {"pe": nc.tensor, "act": nc.scalar, "dve": nc.vector, "pool": nc.gpsimd, "sp": nc.sync}
        self.sem = {k: nc.alloc_semaphore(name=f"s_{k}") for k in self.engs}
        self.cnt = {k: 0 for k in self.engs}
        self.seen = {k: {} for k in self.engs}
        self.dsem = {k: [nc.alloc_semaphore(name=f"d_{k}{i}") for i in range(self.NDMA)] for k in ("sp", "act", "pool")}
        self.dcnt = {k: 0 for k in self.dsem}
        self.dlast = {k: [None] * self.NDMA for k in self.dsem}
        self.out_tickets = []
        self.ninstr = 0

    def _wait(self, e, tick):
        if tick is None:
            return
        sem, val = tick
        key = sem.name
        if e == "pe" and key == "s_pe":
            return
        if self.seen[e].get(key, 0) >= val:
            return
        self.engs[e].wait_ge(sem, val)
        self.seen[e][key] = val

    def deps(self, e, reads, writes):
        for b in reads:
            self._wait(e, b.writer)
        for b in writes:
            self._wait(e, b.writer)
            for r in b.readers:
                self._wait(e, r)

    def commit(self, tick, reads, writes):
        for b in reads:
            b.readers.append(tick)
            if len(b.readers) > 48:
                last = {}
                for t in b.readers:
                    last[t[0].name] = t
                b.readers = list(last.values())
        for b in writes:
            b.writer = tick
            b.readers = []

    def op(self, e, fn, reads=(), writes=()):
        self.deps(e, reads, writes)
        ins = fn()
        self.cnt[e] += 1
        ins.then_inc(self.sem[e], 1)
        tick = (self.sem[e], self.cnt[e])
        self.commit(tick, reads, writes)
        self.ninstr += 1
        return tick

    def group(self, e, fns, reads=(), writes=()):
        self.deps(e, reads, writes)
        ins = None
        for fn in fns:
            ins = fn()
            self.ninstr += 1
        self.cnt[e] += 1
        ins.then_inc(self.sem[e], 1)
        tick = (self.sem[e], self.cnt[e])
        self.commit(tick, reads, writes)
        return tick

    def _dma_common(self, q, issue, reads, writes, is_output):
        j = self.dcnt[q]
        slot = j % self.NDMA
        self._wait(q, self.dlast[q][slot])
        self.deps(q, reads, writes)
        sem = self.dsem[q][slot]
        val = 16 * (j // self.NDMA + 1)
        issue().then_inc(sem, 16)
        self.dcnt[q] += 1
        tick = (sem, val)
        self.dlast[q][slot] = tick
        self.commit(tick, reads, writes)
        if is_output:
            self.out_tickets.append(tick)
        self.ninstr += 1
        return tick

    def dma(self, q, out_ap, in_ap, reads=(), writes=(), is_output=False, **kw):
        return self._dma_common(q, lambda: self.engs[q].dma_start(out=out_ap, in_=in_ap, **kw), reads, writes, is_output)

    def idma(self, reads=(), writes=(), **kw):
        return self._dma_common("pool", lambda: self.nc.gpsimd.indirect_dma_start(**kw), reads, writes, False)

    def barrier(self):
        ticks = [(self.sem[e], self.cnt[e]) for e in self.engs if self.cnt[e] > 0]
        for q in self.dsem:
            ticks += [t for t in self.dlast[q] if t is not None]
        for e in self.engs:
            for t in ticks:
                self._wait(e, t)

    def finish(self):
        for q in self.dsem:
            for t in self.dlast[q]:
                self._wait(q, t)
        for t in self.out_tickets:
            self._wait("sp", t)


class Phase:
    def __init__(self, nc, S, name):
        self.nc, self.S, self.name = nc, S, name
        self.stack = ExitStack()
        self.n = 0

    def __enter__(self):
        self.stack.__enter__()
        return self

    def __exit__(self, *a):
        self.S.barrier()
        return self.stack.__exit__(*a)

    def sb(self, name, shape, dt):
        self.n += 1
        t = self.stack.enter_context(self.nc.sbuf_tensor(f"{self.name}_{name}_{self.n}", list(shape), dt))
        return Buf(name, t.ap())

    def ps(self, name, shape, dt=F32):
        self.n += 1
        t = self.stack.enter_context(self.nc.psum_tensor(f"{self.name}_{name}_{self.n}", list(shape), dt))
        return Buf(name, t.ap())

    def rot(self, name, n, shape, dt):
        return [self.sb(f"{name}{i}", shape, dt) for i in range(n)]


def build(cfg=None, dbg=False):
    if cfg is None:
        cfg = [(i, p) for i in range(DEPTH) for p in ("mix", "moe")]
    nc = bass.Bass("TRN2", target_bir_lowering=False)

    def din(name, shape, dt=F32):
        return nc.dram_tensor(name, list(shape), dt, kind="ExternalInput").ap()

    x_in = din("x", [NSAMP, NL, D])
    c_in = din("c", [NSAMP, D])
    ctx_in = din("ctx", [NSAMP, NCX, D])
    cctx_in = din("c_ctx", [D])
    ada_w = din("ada_w", [DEPTH, D, 6 * D])
    ada_b = din("ada_b", [DEPTH, 6 * D])
    norm1_g = din("norm1_g", [DEPTH, D])
    norm2_g = din("norm2_g", [DEPTH, D])
    final_g = din("final_g", [D])
    attn_w_qkv = din("attn_w_qkv", [2, D, 3 * D])
    attn_w_o = din("attn_w_o", [2, D, D])
    attn_lam = [din(n, [2, 64]) for n in ("attn_lam_q1", "attn_lam_k1", "attn_lam_q2", "attn_lam_k2")]
    attn_subln_g = din("attn_subln_g", [2, 128])
    ssm_lam_re = din("ssm_lam_re", [2, 2, 64, 64])
    ssm_lam_im = din("ssm_lam_im", [2, 2, 64, 64])
    ssm_log_dt = din("ssm_log_dt", [2, 2, 64])
    ssm_b_re = din("ssm_b_re", [2, 2, 64, 64, 16])
    ssm_b_im = din("ssm_b_im", [2, 2, 64, 64, 16])
    ssm_c_re = din("ssm_c_re", [2, 2, 64, 16, 64])
    ssm_c_im = din("ssm_c_im", [2, 2, 64, 16, 64])
    ssm_d = din("ssm_d", [2, D])
    ssm_w_glu1 = din("ssm_w_glu1", [2, D, D])
    ssm_w_glu2 = din("ssm_w_glu2", [2, D, D])
    moe_w_router = din("moe_w_router", [DEPTH, D, NE])
    moe_b_router = din("moe_b_router", [DEPTH, NE])
    moe_w_gate = din("moe_w_gate", [DEPTH, NE, D, FF])
    moe_w_up = din("moe_w_up", [DEPTH, NE, D, FF])
    moe_w_down = din("moe_w_down", [DEPTH, NE, FF, D])
    ident_in = din("k_ident", [128, 128])
    ropec_in = din("k_ropec", [128, NL])
    ropes_in = din("k_ropes", [128, NL])
    out_d = nc.dram_tensor("out", [NSAMP, NL, D], F32, kind="ExternalOutput").ap()

    xT_t = nc.dram_tensor("xT_scr", [NSAMP, KC, 128, NT], F32, kind="ExternalOutput" if dbg else "Internal").ap()
    h2tok_t = nc.dram_tensor("h2tok_scr", [NSAMP, NT, D], BF16, kind="Internal").ap()
    macc_t = nc.dram_tensor("macc_scr", [NSAMP, NT, D], F32, kind="Internal").ap()

    S = Sync(nc)
    xT_b = [Buf(f"xT{s}", xT_t[s]) for s in range(NSAMP)]
    h2l_b = [Buf(f"h2l{s}", h2tok_t[s, 0:NL, :]) for s in range(NSAMP)]
    h2c_b = [Buf(f"h2c{s}", h2tok_t[s, NL:NT, :]) for s in range(NSAMP)]
    mal_b = [Buf(f"mal{s}", macc_t[s, 0:NL, :]) for s in range(NSAMP)]
    mac_b = [Buf(f"mac{s}", macc_t[s, NL:NT, :]) for s in range(NSAMP)]

    def xT_view(s, t0, n):
        return xT_t[s].rearrange("k p t -> p k t")[:, :, t0:t0 + n]

    BLOCKS = [(0, 512), (512, 512), (1024, 512), (1536, 512), (2048, 256)]

    V, T, G, A = nc.vector, nc.tensor, nc.gpsimd, nc.scalar

    with ExitStack() as gstack:
        def gsb(name, shape, dt):
            t = gstack.enter_context(nc.sbuf_tensor("g_" + name, list(shape), dt))
            return Buf(name, t.ap())

        ident = gsb("ident", [128, 128], F32)
        identb = gsb("identb", [128, 128], BF16)
        ones = gsb("ones", [128, 128], F32)
        zeros = gsb("zeros", [128, 1024], F32)
        scT = gsb("scT", [128, KC, 4], F32)
        mod = gsb("mod", [128, 48, 4], F32)
        A1 = gsb("A1", [128, KC, 4], F32)
        A2 = gsb("A2", [128, KC, 4], F32)
        n1g = gsb("n1g", [128, KC], F32)
        n2g = gsb("n2g", [128, KC], F32)
        adab = gsb("adab", [128, 48], F32)

        with Phase(nc, S, "p0") as ph:
            S.dma("sp", ident.ap, ident_in, writes=[ident])
            S.op("dve", lambda: V.tensor_copy(identb.ap, ident.ap), reads=[ident], writes=[identb])
            S.op("pool", lambda: G.memset(ones.ap, 1.0), writes=[ones])
            S.op("pool", lambda: G.memset(zeros.ap, 0.0), writes=[zeros])
            S.op("pool", lambda: G.memset(scT.ap, 0.0), writes=[scT])
            craw = ph.sb("craw", [128, KC, 4], F32)
            S.op("pool", lambda: G.memset(craw.ap, 0.0), writes=[craw])
            for v in range(3):
                src = c_in[v] if v < 2 else cctx_in
                S.dma("sp", craw.ap[:, :, v], src.rearrange("(k p) -> p k", p=128), writes=[craw],
                      allow_slow_non_contiguous=True)
            S.op("act", lambda: A.activation(scT.ap, craw.ap, AF.Silu), reads=[craw], writes=[scT])
            xin = ph.rot("xin", 2, [128, D], F32)
            xtt = ph.rot("xtt", 2, [128, KC, 128], F32)
            pst = [ph.ps("pst0", [128, 512]), ph.ps("pst1", [128, 512])]
            it = 0
            for s in range(NSAMP):
                for tt in range(NT // 128):
                    src = x_in[s, tt * 128:(tt + 1) * 128, :] if tt < 16 else ctx_in[s, (tt - 16) * 128:(tt - 15) * 128, :]
                    xi = xin[it % 2]
                    xo = xtt[it % 2]
                    S.dma("sp" if it % 2 == 0 else "act", xi.ap, src, writes=[xi])
                    for half in range(2):
                        p = pst[half]
                        S.group("pe", [lambda j=j, p=p, xi=xi, half=half: T.transpose(
                            p.ap[:, j * 128:(j + 1) * 128], xi.ap[:, (half * 4 + j) * 128:(half * 4 + j + 1) * 128], ident.ap)
                            for j in range(4)], reads=[xi, ident], writes=[p])
                        S.op("dve" if half == 0 else "act",
                             (lambda p=p, xo=xo, half=half: V.tensor_copy(
                                 xo.ap[:, half * 4:half * 4 + 4, :], p.ap.rearrange("p (k t) -> p k t", k=4))) if half == 0 else
                             (lambda p=p, xo=xo, half=half: A.activation(
                                 xo.ap[:, half * 4:half * 4 + 4, :], p.ap.rearrange("p (k t) -> p k t", k=4), AF.Copy)),
                             reads=[p], writes=[xo])
                    S.dma("sp" if it % 2 == 1 else "act", xT_view(s, tt * 128, 128), xo.ap, reads=[xo], writes=[xT_b[s]])
                    it += 1

        def adaln(i):
            with Phase(nc, S, f"ada{i}") as ph:
                S.dma("sp", adab.ap, ada_b[i].rearrange("(j p) -> p j", p=128), writes=[adab], allow_slow_non_contiguous=True)
                S.dma("act", n1g.ap, norm1_g[i].rearrange("(k p) -> p k", p=128), writes=[n1g], allow_slow_non_contiguous=True)
                S.dma("act", n2g.ap, norm2_g[i].rearrange("(k p) -> p k", p=128), writes=[n2g], allow_slow_non_contiguous=True)
                wst = ph.rot("wst", 2, [128, KC, 512], F32)
                pm = ph.ps("pm", [128, 48, 4])
                for jb in range(12):
                    w = wst[jb % 2]
                    S.dma("sp" if jb % 2 == 0 else "act", w.ap,
                          ada_w[i][:, jb * 512:(jb + 1) * 512].rearrange("(k p) f -> p k f", p=128), writes=[w])
                    for fs in range(4):
                        j = jb * 4 + fs
                        S.group("pe", [lambda kc=kc, j=j, fs=fs, w=w: T.matmul(
                            pm.ap[:, j, :], w.ap[:, kc, fs * 128:(fs + 1) * 128], scT.ap[:, kc, :], start=(kc == 0), stop=(kc == KC - 1))
                            for kc in range(KC)], reads=[w, scT], writes=[pm])
                for v in range(3):
                    S.op("dve", lambda v=v: V.tensor_tensor(mod.ap[:, :, v], pm.ap[:, :, v], adab.ap, ALU.add),
                         reads=[pm, adab], writes=[mod])
                for v in range(3):
                    S.op("dve", lambda v=v: V.scalar_tensor_tensor(A1.ap[:, :, v], mod.ap[:, 8:16, v], 1.0, n1g.ap, ALU.add, ALU.mult),
                         reads=[mod, n1g], writes=[A1])
                    S.op("dve", lambda v=v: V.scalar_tensor_tensor(A2.ap[:, :, v], mod.ap[:, 32:40, v], 1.0, n2g.ap, ALU.add, ALU.mult),
                         reads=[mod, n2g], writes=[A2])

        def norm_block(ph, R, s, t0, n, Acoef, shbase, h32, hb_ap, hb_buf, it):
            v = s if t0 < NL else 2
            xb = R["xb"][it % 2]
            sq = R["sq"]
            S.dma("sp" if it % 2 == 0 else "act", xb.ap[:, :, :n], xT_view(s, t0, n), reads=[xT_b[s]], writes=[xb])
            S.op("act", lambda: A.activation(sq.ap[:, :, :n], xb.ap[:, :, :n], AF.Square), reads=[xb], writes=[sq])
            ssp = R["ssp"]
            S.group("pe", [lambda kc=kc: T.matmul(ssp.ap[:, :n], ones.ap, sq.ap[:, kc, :n], start=(kc == 0), stop=(kc == KC - 1))
                           for kc in range(KC)], reads=[ones, sq], writes=[ssp])
            rstd = R["rstd"]
            S.op("act", lambda: A.activation(rstd.ap[:, :n], ssp.ap[:, :n], AF.Sqrt, bias=R["eps"].ap, scale=1.0 / D),
                 reads=[ssp, R["eps"]], writes=[rstd])
            S.op("dve", lambda: V.reciprocal(rstd.ap[:, :n], rstd.ap[:, :n]), reads=[rstd], writes=[rstd])
            for kc in range(KC):
                S.op("dve", lambda kc=kc: V.tensor_tensor(sq.ap[:, kc, :n], xb.ap[:, kc, :n], rstd.ap[:, :n], ALU.mult),
                     reads=[xb, rstd], writes=[sq])
            for kc in range(KC):
                S.op("pool", lambda kc=kc: G.tensor_scalar(h32.ap[:, kc, :n], sq.ap[:, kc, :n], Acoef.ap[:, kc, v:v + 1],
                                                           mod.ap[:, shbase + kc, v:v + 1], ALU.mult, ALU.add),
                     reads=[sq, Acoef, mod], writes=[h32])
            S.op("act", lambda: A.activation(hb_ap, h32.ap[:, :, :n], AF.Copy), reads=[h32], writes=[hb_buf])

        def norm_resources(ph, ssp=None):
            R = {"xb": ph.rot("xb", 2, [128, KC, 512], F32), "sq": ph.sb("sq", [128, KC, 512], F32),
                 "ssp": ssp if ssp is not None else ph.ps("ssp", [128, 512]), "rstd": ph.sb("rstd", [128, 512], F32), "eps": ph.sb("eps", [128, 1], F32)}
            S.op("pool", lambda: G.memset(R["eps"].ap, EPS), writes=[R["eps"]])
            return R

        def moe_full(i):
            with_ctx = i < DEPTH - 1
            blocks = BLOCKS if with_ctx else BLOCKS[:4]
            with Phase(nc, S, f"moeO{i}") as pho:
                idxT = [pho.sb(f"idxT{s}", [128, 3, NE], U32) for s in range(NSAMP)]
                valT = [pho.sb(f"valT{s}", [128, 3, NE], F32) for s in range(NSAMP)]
                for s in range(NSAMP):
                    S.op("pool", lambda s=s: G.memset(idxT[s].ap, 0), writes=[idxT[s]])
                    S.op("pool", lambda s=s: G.memset(valT[s].ap, 0.0), writes=[valT[s]])
                with Phase(nc, S, f"moeR{i}") as ph:
                    R = norm_resources(ph)
                    wr = ph.sb("wr", [128, KC, NE], F32)
                    br = ph.sb("br", [NE, 1], F32)
                    S.dma("sp", wr.ap, moe_w_router[i].rearrange("(k p) e -> p k e", p=128), writes=[wr])
                    S.dma("sp", br.ap, moe_b_router[i].rearrange("(e o) -> e o", o=1), writes=[br])
                    zi = 0
                    for s in range(NSAMP):
                        for tt in range(NT // 128 if with_ctx else NL // 128):
                            S.dma("sp" if zi % 2 == 0 else "act", macc_t[s, tt * 128:(tt + 1) * 128, :], zeros.ap,
                                  reads=[zeros], writes=[mal_b[s] if tt < 16 else mac_b[s]])
                            zi += 1
                    h32 = ph.sb("h32", [128, KC, 512], F32)
                    hb = ph.rot("hb", 2, [128, KC, 512], BF16)
                    expT = [ph.sb(f"expT{s}", [NE, NT], F32) for s in range(NSAMP)]
                    aff = [ph.sb(f"aff{s}", [NE, NT], F32) for s in range(NSAMP)]
                    lgp = ph.ps("lgp", [128, 512])
                    smp = ph.ps("smp", [128, 512])
                    rs = ph.sb("rs", [NE, 512], F32)
                    ptb = ph.ps("ptb", [128, 512])
                    ptb_bf = ptb.ap.bitcast(BF16)
                    tok = ph.rot("tok", 2, [128, D], BF16)
                    it = 0
                    ti = 0
                    for s in range(NSAMP):
                        for (t0, n) in blocks:
                            hbb = hb[it % 2]
                            norm_block(ph, R, s, t0, n, A2, 24, h32, hbb.ap[:, :, :n], hbb, it)
                            S.group("pe", [lambda kc=kc, n=n: T.matmul(lgp.ap[0:NE, :n], wr.ap[:, kc, :], h32.ap[:, kc, :n],
                                                                         start=(kc == 0), stop=(kc == KC - 1)) for kc in range(KC)],
                                    reads=[wr, h32], writes=[lgp])
                            S.op("act", lambda s=s, t0=t0, n=n: A.activation(expT[s].ap[:, t0:t0 + n], lgp.ap[0:NE, :n], AF.Exp, bias=br.ap),
                                 reads=[lgp, br], writes=[expT[s]])
                            S.group("pe", [lambda s=s, t0=t0, n=n: T.matmul(smp.ap[0:NE, :n], ones.ap[0:NE, 0:NE], expT[s].ap[:, t0:t0 + n],
                                                                          start=True, stop=True)], reads=[ones, expT[s]], writes=[smp])
                            S.op("dve", lambda n=n: V.reciprocal(rs.ap[:, :n], smp.ap[0:NE, :n]), reads=[smp], writes=[rs])
                            S.op("dve", lambda s=s, t0=t0, n=n: V.tensor_tensor(aff[s].ap[:, t0:t0 + n], expT[s].ap[:, t0:t0 + n], rs.ap[:, :n], ALU.mult),
                                 reads=[expT[s], rs], writes=[aff[s]])
                            for tt in range(n // 128):
                                tk = tok[ti % 2]
                                S.group("pe", [lambda kc=kc, tt=tt, hbb=hbb: T.transpose(
                                    ptb_bf[:, kc * 128:(kc + 1) * 128], hbb.ap[:, kc, tt * 128:(tt + 1) * 128], identb.ap) for kc in range(KC)],
                                    reads=[hbb, identb], writes=[ptb])
                                S.op("dve", lambda tk=tk: V.tensor_copy(tk.ap, ptb_bf), reads=[ptb], writes=[tk])
                                r0 = t0 + tt * 128
                                S.dma("sp" if ti % 2 == 0 else "act", h2tok_t[s, r0:r0 + 128, :], tk.ap, reads=[tk],
                                      writes=[h2l_b[s] if r0 < NL else h2c_b[s]])
                                ti += 1
                            it += 1
                    vals = [ph.sb(f"vals{s}", [NE, CAPL + CAPC], F32) for s in range(NSAMP)]
                    idx = [ph.sb(f"idx{s}", [NE, CAPL + CAPC], U32) for s in range(NSAMP)]
                    idxf = [ph.sb(f"idxf{s}", [NE, CAPL + CAPC], F32) for s in range(NSAMP)]
                    segs = [(0, NL, 0, CAPL // 8)] + ([(NL, NCX, CAPL, CAPC // 8)] if with_ctx else [])
                    for (a0, an, c0, rounds) in segs:
                        for r in range(rounds):
                            for s in range(NSAMP):
                                av = aff[s].ap[:, a0:a0 + an]
                                vv = vals[s].ap[:, c0 + r * 8:c0 + r * 8 + 8]
                                S.op("dve", lambda av=av, vv=vv: V.max(vv, av), reads=[aff[s]], writes=[vals[s]])
                                S.op("dve", lambda av=av, vv=vv, s=s, c0=c0, r=r: V.max_index(idx[s].ap[:, c0 + r * 8:c0 + r * 8 + 8], vv, av),
                                     reads=[aff[s], vals[s]], writes=[idx[s]])
                                S.op("dve", lambda av=av, vv=vv: V.match_replace(av, vv, av, -1.0), reads=[vals[s]], writes=[aff[s]])
                    ncap = CAPL + (CAPC if with_ctx else 0)
                    for s in range(NSAMP):
                        S.op("dve", lambda s=s: V.tensor_copy(idxf[s].ap[:, :ncap], idx[s].ap[:, :ncap]), reads=[idx[s]], writes=[idxf[s]])
                        pieces = [(0, 128, 0), (128, 128, 1)] + ([(256, 32, 2)] if with_ctx else [])
                        for (c0, cn, slot) in pieces:
                            S.group("pe", [lambda s=s, c0=c0, cn=cn: T.transpose(lgp.ap[0:cn, 0:NE], idxf[s].ap[:, c0:c0 + cn], ident.ap[0:NE, 0:NE])],
                                    reads=[idxf[s], ident], writes=[lgp])
                            S.op("dve", lambda s=s, cn=cn, slot=slot: V.tensor_copy(idxT[s].ap[0:cn, slot, :], lgp.ap[0:cn, 0:NE]),
                                 reads=[lgp], writes=[idxT[s]])
                            S.group("pe", [lambda s=s, c0=c0, cn=cn: T.transpose(smp.ap[0:cn, 0:NE], vals[s].ap[:, c0:c0 + cn], ident.ap[0:NE, 0:NE])],
                                    reads=[vals[s], ident], writes=[smp])
                            S.op("dve", lambda s=s, cn=cn, slot=slot: V.tensor_copy(valT[s].ap[0:cn, slot, :], smp.ap[0:cn, 0:NE]),
                                 reads=[smp], writes=[valT[s]])

                with Phase(nc, S, f"moeE{i}") as ph:
                    xg = ph.rot("xg", 2, [128, D], BF16)
                    xinT = ph.rot("xinT", 2, [128, KC, NCOL], BF16)
                    wgs = ph.rot("wgs", 3, [128, KC, 256], F32)
                    wus = ph.rot("wus", 3, [128, KC, 256], F32)
                    wgb = ph.rot("wgb", 2, [128, KC, 256], BF16)
                    wub = ph.rot("wub", 2, [128, KC, 256], BF16)
                    wds = ph.rot("wds", 2, [128, 4, D], F32)
                    wdb = ph.sb("wdb", [128, 16, D], BF16)
                    hidT = ph.sb("hidT", [128, 16, NCOL], BF16)
                    sg = ph.rot("sg", 2, [128, NCOL], F32)
                    yT = ph.sb("yT", [128, KC, NCOL], F32)
                    yo = ph.rot("yo", 2, [128, D], F32)
                    Gl = [ph.ps(f"Gl{k}", [128, 512]) for k in range(2)]
                    Ul = [ph.ps(f"Ul{k}", [128, 512]) for k in range(2)]
                    GUc = ph.ps("GUc", [128, 512])
                    Yl2 = [ph.ps(f"Yl{k}", [128, 512]) for k in range(2)]
                    Yc = Buf("Yc", GUc.ap[:, 128:256])
                    ptr = ph.ps("ptr", [128, 512])
                    ptr_bf = ptr.ap.bitcast(BF16)
                    nctx = 2 * CAPC if with_ctx else 0
                    gi = 0
                    wi = 0
                    di = 0
                    fi = 0
                    yi = 0
                    gl = []
                    for s in range(NSAMP):
                        for hf in range(2):
                            gl.append((s, hf, 128, s * CAPL + hf * 128, None, h2l_b[s]))
                    if with_ctx:
                        for s in range(NSAMP):
                            gl.append((s, 2, CAPC, 2 * CAPL + s * CAPC, None, h2c_b[s]))
                    gctr = [0]

                    def gather(e):
                        xt = xinT[e % 2]
                        for (s, slot, cn, col0, src, srcb) in gl:
                            g = xg[gctr[0] % 2]
                            gctr[0] += 1
                            S.idma(out=g.ap[0:cn, :], out_offset=None, in_=h2tok_t.rearrange("s t d -> (s t) d"),
                                   in_offset=bass.IndirectOffsetOnAxis(ap=idxT[s].ap[0:cn, slot, e:e + 1], axis=0),
                                   element_offset=(s * NT + (0 if slot < 2 else NL)) * D,
                                   reads=[idxT[s], srcb], writes=[g])
                            S.group("pe", [lambda kc=kc, g=g, cn=cn: T.transpose(
                                ptr_bf[:, kc * 128:kc * 128 + cn], g.ap[0:cn, kc * 128:(kc + 1) * 128], identb.ap[0:cn, 0:cn]) for kc in range(KC)],
                                reads=[g, identb], writes=[ptr])
                            S.op("dve", lambda xt=xt, col0=col0, cn=cn: V.tensor_copy(
                                xt.ap[:, :, col0:col0 + cn], ptr_bf.rearrange("p (k c) -> p k c", k=KC)[:, :, 0:cn]),
                                reads=[ptr], writes=[xt])

                    gather(0)
                    for e in range(NE):
                        xt = xinT[e % 2]
                        for pp in range(4):
                            w = wds[di % 2]
                            S.dma("sp" if di % 2 == 0 else "act", w.ap,
                                  moe_w_down[i, e, pp * 512:(pp + 1) * 512, :].rearrange("(c q) d -> q c d", q=128), writes=[w])
                            S.op("act", lambda w=w, pp=pp: A.activation(wdb.ap[:, pp * 4:(pp + 1) * 4, :], w.ap, AF.Copy), reads=[w], writes=[wdb])
                            di += 1
                        for p in range(8):
                            ws_g, ws_u, wb_g, wb_u = wgs[wi % 3], wus[wi % 3], wgb[wi % 2], wub[wi % 2]
                            wi += 1
                            S.dma("sp", ws_g.ap, moe_w_gate[i, e][:, p * 256:(p + 1) * 256].rearrange("(k q) f -> q k f", q=128), writes=[ws_g])
                            S.dma("act", ws_u.ap, moe_w_up[i, e][:, p * 256:(p + 1) * 256].rearrange("(k q) f -> q k f", q=128), writes=[ws_u])
                            S.op("pool", lambda a=wb_g, b=ws_g: G.tensor_copy(a.ap, b.ap), reads=[ws_g], writes=[wb_g])
                            S.op("pool", lambda a=wb_u, b=ws_u: G.tensor_copy(a.ap, b.ap), reads=[ws_u], writes=[wb_u])
                            for fsub in range(2):
                                fc = p * 2 + fsub
                                gl_, ul_ = Gl[fi % 2], Ul[fi % 2]
                                sgt = sg[fi % 2]
                                fi += 1
                                fs = slice(fsub * 128, (fsub + 1) * 128)
                                S.group("pe", [lambda kc=kc, gl_=gl_, wb_g=wb_g, fs=fs, xt=xt: T.matmul(
                                    gl_.ap[:, 0:512], wb_g.ap[:, kc, fs], xt.ap[:, kc, 0:512], start=(kc == 0), stop=(kc == KC - 1))
                                    for kc in range(KC)], reads=[wb_g, xt], writes=[gl_])
                                S.group("pe", [lambda kc=kc, ul_=ul_, wb_u=wb_u, fs=fs, xt=xt: T.matmul(
                                    ul_.ap[:, 0:512], wb_u.ap[:, kc, fs], xt.ap[:, kc, 0:512], start=(kc == 0), stop=(kc == KC - 1))
                                    for kc in range(KC)], reads=[wb_u, xt], writes=[ul_])
                                if with_ctx:
                                    S.group("pe", [lambda kc=kc, wb_g=wb_g, fs=fs, xt=xt: T.matmul(
                                        GUc.ap[:, 0:64], wb_g.ap[:, kc, fs], xt.ap[:, kc, 512:576], start=(kc == 0), stop=(kc == KC - 1))
                                        for kc in range(KC)], reads=[wb_g, xt], writes=[GUc])
                                    S.group("pe", [lambda kc=kc, wb_u=wb_u, fs=fs, xt=xt: T.matmul(
                                        GUc.ap[:, 64:128], wb_u.ap[:, kc, fs], xt.ap[:, kc, 512:576], start=(kc == 0), stop=(kc == KC - 1))
                                        for kc in range(KC)], reads=[wb_u, xt], writes=[GUc])
                                S.op("act", lambda sgt=sgt, gl_=gl_: A.activation(sgt.ap[:, 0:512], gl_.ap, AF.Silu), reads=[gl_], writes=[sgt])
                                S.op("dve", lambda sgt=sgt, ul_=ul_, fc=fc: V.tensor_tensor(hidT.ap[:, fc, 0:512], sgt.ap[:, 0:512], ul_.ap, ALU.mult),
                                     reads=[sgt, ul_], writes=[hidT])
                                if with_ctx:
                                    S.op("act", lambda sgt=sgt: A.activation(sgt.ap[:, 512:576], GUc.ap[:, 0:64], AF.Silu), reads=[GUc], writes=[sgt])
                                    S.op("dve", lambda sgt=sgt, fc=fc: V.tensor_tensor(hidT.ap[:, fc, 512:576], sgt.ap[:, 512:576], GUc.ap[:, 64:128], ALU.mult),
                                         reads=[sgt, GUc], writes=[hidT])
                        if e + 1 < NE:
                            gather(e + 1)
                        for dc in range(KC):
                            ds_ = slice(dc * 128, (dc + 1) * 128)
                            Yl = Yl2[dc % 2]
                            S.group("pe", [lambda fc=fc, ds_=ds_, Yl=Yl: T.matmul(Yl.ap[:, 0:512], wdb.ap[:, fc, ds_], hidT.ap[:, fc, 0:512],
                                                                           start=(fc == 0), stop=(fc == 15)) for fc in range(16)],
                                    reads=[wdb, hidT], writes=[Yl])
                            S.op("act", lambda dc=dc, Yl=Yl: A.activation(yT.ap[:, dc, 0:512], Yl.ap, AF.Copy), reads=[Yl], writes=[yT])
                            if with_ctx:
                                S.group("pe", [lambda fc=fc, ds_=ds_: T.matmul(Yc.ap[:, 0:64], wdb.ap[:, fc, ds_], hidT.ap[:, fc, 512:576],
                                                                               start=(fc == 0), stop=(fc == 15)) for fc in range(16)],
                                        reads=[wdb, hidT], writes=[Yc])
                                S.op("act", lambda dc=dc: A.activation(yT.ap[:, dc, 512:576], Yc.ap[:, 0:64], AF.Copy), reads=[Yc], writes=[yT])
                        for (s, slot, cn, col0, src, srcb) in gl:
                            y = yo[yi % 2]
                            yi += 1
                            for half in range(2):
                                S.group("pe", [lambda j=j, half=half, col0=col0, cn=cn: T.transpose(
                                    ptr.ap[0:cn, j * 128:(j + 1) * 128], yT.ap[:, half * 4 + j, col0:col0 + cn], ident.ap)
                                    for j in range(4)], reads=[yT, ident], writes=[ptr])
                                S.op("dve", lambda y=y, half=half, cn=cn, s=s, slot=slot, e=e: V.tensor_scalar(
                                    y.ap[0:cn, half * 512:(half + 1) * 512], ptr.ap[0:cn, :], valT[s].ap[0:cn, slot, e:e + 1], None, ALU.mult),
                                    reads=[ptr, valT[s]], writes=[y])
                            dstb = mal_b[s] if slot < 2 else mac_b[s]
                            S.idma(out=macc_t.rearrange("s t d -> (s t) d"),
                                   out_offset=bass.IndirectOffsetOnAxis(ap=idxT[s].ap[0:cn, slot, e:e + 1], axis=0),
                                   in_=y.ap[0:cn, :], in_offset=None, compute_op=ALU.add,
                                   element_offset=(s * NT + (0 if slot < 2 else NL)) * D,
                                   reads=[y, idxT[s]], writes=[dstb])

                with Phase(nc, S, f"moeU{i}") as ph:
                    xb = ph.rot("xb", 2, [128, KC, 512], F32)
                    mt = ph.rot("mt", 2, [128, D], F32)
                    pt = [ph.ps(f"pt{k}", [128, 512]) for k in range(2)]
                    it = 0
                    mi = 0
                    for s in range(NSAMP):
                        for (t0, n) in blocks:
                            v = s if t0 < NL else 2
                            x = xb[it % 2]
                            it += 1
                            S.dma("sp", x.ap[:, :, :n], xT_view(s, t0, n), reads=[xT_b[s]], writes=[x])
                            for tt in range(n // 128):
                                m = mt[mi % 2]
                                mi += 1
                                r0 = t0 + tt * 128
                                S.dma("act", m.ap, macc_t[s, r0:r0 + 128, :], reads=[mal_b[s] if r0 < NL else mac_b[s]], writes=[m])
                                for half in range(2):
                                    p = pt[half]
                                    S.group("pe", [lambda j=j, half=half, m=m, p=p: T.transpose(
                                        p.ap[:, j * 128:(j + 1) * 128], m.ap[:, (half * 4 + j) * 128:(half * 4 + j + 1) * 128], ident.ap)
                                        for j in range(4)], reads=[m, ident], writes=[p])
                                    for j in range(4):
                                        kc = half * 4 + j
                                        S.op("dve", lambda j=j, kc=kc, p=p, x=x, tt=tt, v=v: V.scalar_tensor_tensor(
                                            x.ap[:, kc, tt * 128:(tt + 1) * 128], p.ap[:, j * 128:(j + 1) * 128], mod.ap[:, 40 + kc, v:v + 1],
                                            x.ap[:, kc, tt * 128:(tt + 1) * 128], ALU.mult, ALU.add), reads=[p, mod, x], writes=[x])
                            S.dma("sp", xT_view(s, t0, n), x.ap[:, :, :n], reads=[x], writes=[xT_b[s]])

        def final():
            with Phase(nc, S, "fin") as ph:
                R = norm_resources(ph)
                fg = ph.sb("fg", [128, KC], F32)
                S.dma("sp", fg.ap, final_g.rearrange("(k p) -> p k", p=128), writes=[fg], allow_slow_non_contiguous=True)
                xn = ph.sb("xn", [128, KC, 512], F32)
                pt = [ph.ps(f"pt{k}", [128, 512]) for k in range(2)]
                ot = ph.rot("ot", 2, [128, D], F32)
                it = 0
                oi = 0
                for s in range(NSAMP):
                    for (t0, n) in BLOCKS[:4]:
                        xb = R["xb"][it % 2]
                        sq = R["sq"]
                        S.dma("sp" if it % 2 == 0 else "act", xb.ap, xT_view(s, t0, n), reads=[xT_b[s]], writes=[xb])
                        it += 1
                        S.op("act", lambda xb=xb: A.activation(sq.ap, xb.ap, AF.Square), reads=[xb], writes=[sq])
                        ssp = R["ssp"]
                        S.group("pe", [lambda kc=kc: T.matmul(ssp.ap, ones.ap, sq.ap[:, kc, :], start=(kc == 0), stop=(kc == KC - 1))
                                       for kc in range(KC)], reads=[ones, sq], writes=[ssp])
                        rstd = R["rstd"]
                        S.op("act", lambda: A.activation(rstd.ap, ssp.ap, AF.Sqrt, bias=R["eps"].ap, scale=1.0 / D), reads=[ssp, R["eps"]], writes=[rstd])
                        S.op("dve", lambda: V.reciprocal(rstd.ap, rstd.ap), reads=[rstd], writes=[rstd])
                        for kc in range(KC):
                            S.op("dve", lambda kc=kc, xb=xb: V.scalar_tensor_tensor(xn.ap[:, kc, :], xb.ap[:, kc, :], fg.ap[:, kc:kc + 1], rstd.ap,
                                                                                    ALU.mult, ALU.mult), reads=[xb, fg, rstd], writes=[xn])
                        for tt in range(4):
                            o = ot[oi % 2]
                            oi += 1
                            for half in range(2):
                                p = pt[half]
                                S.group("pe", [lambda j=j, half=half, tt=tt, p=p: T.transpose(
                                    p.ap[:, j * 128:(j + 1) * 128], xn.ap[:, half * 4 + j, tt * 128:(tt + 1) * 128], ident.ap) for j in range(4)],
                                    reads=[xn, ident], writes=[p])
                                if half == 0:
                                    S.op("dve", lambda o=o, p=p: V.tensor_copy(o.ap[:, 0:512], p.ap), reads=[p], writes=[o])
                                else:
                                    S.op("act", lambda o=o, p=p: A.activation(o.ap[:, 512:1024], p.ap, AF.Copy), reads=[p], writes=[o])
                            r0 = t0 + tt * 128
                            S.dma("sp" if oi % 2 == 0 else "act", out_d[s, r0:r0 + 128, :], o.ap, reads=[o], is_output=True)


        def attn(i):
            j = i // 2
            lam_init = 0.8 - 0.6 * math.exp(-0.3 * i)
            with Phase(nc, S, f"att{i}") as ph:
                PA = ph.ps("PA", [128, 512])
                PB = ph.ps("PB", [128, 512])
                Sp = [ph.ps(f"Sp{k}", [128, 512]) for k in range(2)]
                Op = [ph.ps(f"Op{k}", [128, 512]) for k in range(4)]
                R = norm_resources(ph, ssp=PA)
                hT = ph.sb("hT", [128, KC, NT], BF16)
                h32 = R["sq"]
                onT = ph.sb("onT", [128, 8, NT], BF16)
                ropec = ph.sb("ropec", [128, NL], F32)
                ropes = ph.sb("ropes", [128, NL], F32)
                S.dma("sp", ropec.ap, ropec_in, writes=[ropec])
                S.dma("act", ropes.ap, ropes_in, writes=[ropes])
                wo = ph.sb("wo", [128, 8, D], BF16)
                S.dma("pool", wo.ap, attn_w_o[j].rearrange("(k p) f -> p k f", p=128), writes=[wo])
                lt = ph.sb("lt", [64, 4], F32)
                for a in range(4):
                    S.dma("sp", lt.ap[:, a:a + 1], attn_lam[a][j].rearrange("(p o) -> p o", o=1), writes=[lt], allow_slow_non_contiguous=True)
                pr = ph.sb("pr", [64, 2], F32)
                S.op("dve", lambda: V.tensor_tensor(pr.ap[:, 0:1], lt.ap[:, 0:1], lt.ap[:, 1:2], ALU.mult), reads=[lt], writes=[pr])
                S.op("dve", lambda: V.tensor_tensor(pr.ap[:, 1:2], lt.ap[:, 2:3], lt.ap[:, 3:4], ALU.mult), reads=[lt], writes=[pr])
                S.group("pe", [lambda: T.matmul(PB.ap[:, 0:2], ones.ap[0:64, :], pr.ap, start=True, stop=True)], reads=[ones, pr], writes=[PB])
                ex = ph.sb("ex", [128, 2], F32)
                S.op("act", lambda: A.activation(ex.ap, PB.ap[:, 0:2], AF.Exp), reads=[PB], writes=[ex])
                neglam = ph.sb("neglam", [128, 1], F32)
                S.op("dve", lambda: V.tensor_tensor(neglam.ap, ex.ap[:, 1:2], ex.ap[:, 0:1], ALU.subtract), reads=[ex], writes=[neglam])
                S.op("dve", lambda: V.tensor_scalar(neglam.ap, neglam.ap, -lam_init, None, ALU.add), reads=[neglam], writes=[neglam])
                subg = ph.sb("subg", [128, 1], F32)
                S.dma("sp", subg.ap, attn_subln_g[j].rearrange("(p o) -> p o", o=1), writes=[subg], allow_slow_non_contiguous=True)
                S.op("dve", lambda: V.tensor_scalar(subg.ap, subg.ap, 1.0 - lam_init, None, ALU.mult), reads=[subg], writes=[subg])
                wq = ph.rot("wq", 2, [128, KC, 128], BF16)
                wk = ph.rot("wk", 2, [128, KC, 128], BF16)
                wv = ph.rot("wv", 2, [128, KC, 128], BF16)
                wqs = ph.rot("wqs", 2, [128, KC, 128], BF16)
                wks = ph.rot("wks", 2, [128, KC, 128], BF16)
                qT = ph.sb("qT", [128, NT], BF16)
                kT = ph.sb("kT", [128, NT], BF16)
                vx = ph.sb("vx", [128, NT // 128, 130], BF16)
                S.op("pool", lambda: G.memset(vx.ap, 1.0), writes=[vx])
                t1 = ph.rot("t1", 2, [128, 512], F32)
                t2 = ph.rot("t2", 2, [128, 512], F32)
                Et = ph.rot("Et", 2, [128, 512], BF16)
                otmp = ph.sb("otmp", [128, 4, 128], F32)
                osq = ph.sb("osq", [128, 128], F32)
                sm = ph.rot("sm", 4, [128, 4], F32)
                xb = R["xb"]
                wsrc = attn_w_qkv[j]

                def wview(base):
                    return wsrc[:, base:base + 128].rearrange("(k p) f -> p k f", p=128)

                def wview_sw(base, two):
                    return wsrc[:, base:base + 128].rearrange("(k p) (b two q) -> p k b two q", p=128, two=2, q=16)[:, :, :, two, :]

                it = 0
                hi = 0
                ei = 0
                for s in range(NSAMP):
                    for (t0, n) in BLOCKS:
                        norm_block(ph, R, s, t0, n, A1, 0, h32, hT.ap[:, :, t0:t0 + n], hT, it)
                        it += 1
                    for h in range(8):
                        Wq, Wk, Wv, Wqs, Wks = wq[hi % 2], wk[hi % 2], wv[hi % 2], wqs[hi % 2], wks[hi % 2]
                        hi += 1
                        S.dma("pool", Wq.ap, wview(h * 128), writes=[Wq])
                        S.dma("pool", Wk.ap, wview(D + h * 128), writes=[Wk])
                        S.dma("pool", Wv.ap, wview(2 * D + h * 128), writes=[Wv])
                        for two in range(2):
                            for (Wd_, Ws_) in ((Wqs, Wq), (Wks, Wk)):
                                S.op("pool", lambda Wd_=Wd_, Ws_=Ws_, two=two: G.tensor_copy(
                                    Wd_.ap.rearrange("p k (b two q) -> p k b two q", two=2, q=16)[:, :, :, two, :],
                                    Ws_.ap.rearrange("p k (b two q) -> p k b two q", two=2, q=16)[:, :, :, 1 - two, :]),
                                    reads=[Ws_], writes=[Wd_])
                        for (W, Wsw, dst) in ((Wq, Wqs, qT), (Wk, Wks, kT)):
                            for (t0, n) in BLOCKS:
                                S.group("pe", [lambda kc=kc, W=W, t0=t0, n=n: T.matmul(PA.ap[:, :n], W.ap[:, kc, :], hT.ap[:, kc, t0:t0 + n],
                                                                                        start=(kc == 0), stop=(kc == KC - 1)) for kc in range(KC)],
                                        reads=[W, hT], writes=[PA])
                                if t0 < NL:
                                    S.group("pe", [lambda kc=kc, Wsw=Wsw, t0=t0, n=n: T.matmul(PB.ap[:, :n], Wsw.ap[:, kc, :], hT.ap[:, kc, t0:t0 + n],
                                                                                                start=(kc == 0), stop=(kc == KC - 1)) for kc in range(KC)],
                                            reads=[Wsw, hT], writes=[PB])
                                    a1, a2 = t1[ei % 2], t2[ei % 2]
                                    ei += 1
                                    S.op("dve", lambda a1=a1, t0=t0, n=n: V.tensor_tensor(a1.ap[:, :n], PA.ap[:, :n], ropec.ap[:, t0:t0 + n], ALU.mult),
                                         reads=[PA, ropec], writes=[a1])
                                    S.op("dve", lambda a2=a2, t0=t0, n=n: V.tensor_tensor(a2.ap[:, :n], PB.ap[:, :n], ropes.ap[:, t0:t0 + n], ALU.mult),
                                         reads=[PB, ropes], writes=[a2])
                                    S.op("pool", lambda a1=a1, a2=a2, dst=dst, t0=t0, n=n: G.tensor_tensor(dst.ap[:, t0:t0 + n], a1.ap[:, :n], a2.ap[:, :n], ALU.add),
                                         reads=[a1, a2], writes=[dst])
                                else:
                                    S.op("act", lambda dst=dst, t0=t0, n=n: A.activation(dst.ap[:, t0:t0 + n], PA.ap[:, :n], AF.Copy), reads=[PA], writes=[dst])
                        for tt in range(NT // 128):
                            S.group("pe", [lambda kc=kc, tt=tt, Wv=Wv: T.matmul(PB.ap[:, 0:128], hT.ap[:, kc, tt * 128:(tt + 1) * 128], Wv.ap[:, kc, :],
                                                                                 start=(kc == 0), stop=(kc == KC - 1)) for kc in range(KC)],
                                    reads=[Wv, hT], writes=[PB])
                            S.op("act", lambda tt=tt: A.activation(vx.ap[:, tt, 0:128], PB.ap[:, 0:128], AF.Copy), reads=[PB], writes=[vx])
                        for (q0, qn, ktiles) in [(0, 512, list(range(18))), (512, 512, list(range(18))), (1024, 512, list(range(18))),
                                                 (1536, 512, list(range(18))), (2048, 256, [16, 17])]:
                            nq = qn // 128
                            for c in range(2):
                                cs = slice(c * 64, (c + 1) * 64)
                                for ki, kt in enumerate(ktiles):
                                    sp_, et_ = Sp[ei % 2], Et[ei % 2]
                                    ei += 1
                                    S.group("pe", [lambda sp_=sp_, kt=kt, cs=cs, q0=q0, qn=qn: T.matmul(
                                        sp_.ap[:, :qn], kT.ap[cs, kt * 128:(kt + 1) * 128], qT.ap[cs, q0:q0 + qn], start=True, stop=True)],
                                        reads=[kT, qT], writes=[sp_])
                                    S.op("act", lambda sp_=sp_, et_=et_, qn=qn: A.activation(et_.ap[:, :qn], sp_.ap[:, :qn], AF.Exp, scale=0.125),
                                         reads=[sp_], writes=[et_])
                                    for qs in range(nq):
                                        S.group("pe", [lambda qs=qs, et_=et_, kt=kt, ki=ki, ktiles=ktiles: T.matmul(
                                            Op[qs].ap[:, 0:129], et_.ap[:, qs * 128:(qs + 1) * 128], vx.ap[:, kt, 0:129],
                                            start=(ki == 0), stop=(ki == len(ktiles) - 1))], reads=[et_, vx], writes=[Op[qs]])
                                for qs in range(nq):
                                    m = sm[qs]
                                    S.op("dve", lambda m=m, qs=qs: V.reciprocal(m.ap[:, 0:1], Op[qs].ap[:, 128:129]), reads=[Op[qs]], writes=[m])
                                    if c == 0:
                                        S.op("dve", lambda m=m, qs=qs: V.tensor_scalar(otmp.ap[:, qs, :], Op[qs].ap[:, 0:128], m.ap[:, 0:1], None, ALU.mult),
                                             reads=[Op[qs], m], writes=[otmp])
                                    else:
                                        S.op("dve", lambda m=m: V.tensor_tensor(m.ap[:, 1:2], m.ap[:, 0:1], neglam.ap, ALU.mult), reads=[m, neglam], writes=[m])
                                        S.op("dve", lambda m=m, qs=qs: V.scalar_tensor_tensor(otmp.ap[:, qs, :], Op[qs].ap[:, 0:128], m.ap[:, 1:2], otmp.ap[:, qs, :],
                                                                                              ALU.mult, ALU.add), reads=[Op[qs], m, otmp], writes=[otmp])
                                        S.op("act", lambda m=m, qs=qs: A.activation(osq.ap, otmp.ap[:, qs, :], AF.Square, accum_out=m.ap[:, 2:3]),
                                             reads=[otmp], writes=[osq, m])
                                        S.op("act", lambda m=m: A.activation(m.ap[:, 3:4], m.ap[:, 2:3], AF.Sqrt, bias=R["eps"].ap, scale=1.0 / 128),
                                             reads=[m, R["eps"]], writes=[m])
                                        S.op("dve", lambda m=m: V.reciprocal(m.ap[:, 3:4], m.ap[:, 3:4]), reads=[m], writes=[m])
                                        S.op("dve", lambda m=m, qs=qs: V.tensor_scalar(otmp.ap[:, qs, :], otmp.ap[:, qs, :], m.ap[:, 3:4], None, ALU.mult),
                                             reads=[otmp, m], writes=[otmp])
                                        S.group("pe", [lambda qs=qs: T.transpose(PA.ap[:, qs * 128:(qs + 1) * 128], otmp.ap[:, qs, :], ident.ap)],
                                                reads=[otmp, ident], writes=[PA])
                                        S.op("dve", lambda qs=qs, h=h, q0=q0: V.tensor_scalar(onT.ap[:, h, q0 + qs * 128:q0 + (qs + 1) * 128],
                                                                                              PA.ap[:, qs * 128:(qs + 1) * 128], subg.ap[:, 0:1], None, ALU.mult),
                                             reads=[PA, subg], writes=[onT])
                    for (t0, n) in BLOCKS:
                        v = s if t0 < NL else 2
                        x = xb[it % 2]
                        it += 1
                        S.dma("sp", x.ap[:, :, :n], xT_view(s, t0, n), reads=[xT_b[s]], writes=[x])
                        for fc in range(KC):
                            pp = PA if fc % 2 == 0 else PB
                            S.group("pe", [lambda hh=hh, fc=fc, pp=pp, t0=t0, n=n: T.matmul(pp.ap[:, :n], wo.ap[:, hh, fc * 128:(fc + 1) * 128], onT.ap[:, hh, t0:t0 + n],
                                                                                             start=(hh == 0), stop=(hh == 7)) for hh in range(8)],
                                    reads=[wo, onT], writes=[pp])
                            S.op("dve", lambda fc=fc, pp=pp, x=x, n=n, v=v: V.scalar_tensor_tensor(x.ap[:, fc, :n], pp.ap[:, :n], mod.ap[:, 16 + fc, v:v + 1],
                                                                                                   x.ap[:, fc, :n], ALU.mult, ALU.add), reads=[pp, mod, x], writes=[x])
                        S.dma("act", xT_view(s, t0, n), x.ap[:, :, :n], reads=[x], writes=[xT_b[s]])


        def s5(i):
            j = i // 2
            with_ctx = i < DEPTH - 1
            FB = [(0, 256), (256, 512), (768, 512), (1280, 512), (1792, 512)]
            TWO_PI = 2.0 * math.pi
            PIC = 3.1415925
            with Phase(nc, S, f"s5o{i}") as pho:
                R = norm_resources(pho)
                hF = pho.sb("hF", [128, KC, NT], BF16)
                PSre = pho.ps("PSre", [128, 512])
                PSim = pho.ps("PSim", [128, 512])
                PSy = pho.ps("PSy", [128, 512])
                PT = pho.ps("PT", [128, 512])

                def P(name, shape=(128, 64), dt=F32):
                    return pho.sb(name, list(shape), dt)

                lre, lim, ldt, mag, ang, red, red2, sn, cs = [P(n) for n in ("lre", "lim", "ldt", "mag", "ang", "red", "red2", "sn", "cs")]
                abre, abim, nr, den, cre, cim, ncim, tq = [P(n) for n in ("abre", "abim", "nr", "den", "cre", "cim", "ncim", "tq")]
                ki = P("ki", dt=I32)
                rc = P("rc", (128, 12, 64))
                rsn = P("rsn", (128, 12, 64))
                nrs = P("nrs", (128, 12, 64))
                dsk = P("dsk", (128, KC))
                for (dst, src) in ((lre, ssm_lam_re), (lim, ssm_lam_im)):
                    for d in range(2):
                        S.dma("sp", dst.ap[:, d * 32:(d + 1) * 32], src[j, d].rearrange("g n -> (g n)").rearrange("(gp p) -> p gp", p=128),
                              writes=[dst], allow_slow_non_contiguous=True)
                S.dma("act", dsk.ap, ssm_d[j].rearrange("(k p) -> p k", p=128), writes=[dsk], allow_slow_non_contiguous=True)
                ldrow = P("ldrow", (1, 128))
                S.dma("sp", ldrow.ap, ssm_log_dt[j].rearrange("d g -> (d g)").rearrange("(o f) -> o f", o=1), writes=[ldrow])
                S.group("pe", [lambda: T.matmul(PT.ap[:, 0:128], ones.ap[0:1, :], ldrow.ap, start=True, stop=True)], reads=[ones, ldrow], writes=[PT])
                for g2 in range(2):
                    S.op("dve", lambda g2=g2: V.tensor_copy(
                        ldt.ap[g2 * 64:(g2 + 1) * 64, :].rearrange("p (d g) -> p d g", d=2),
                        PT.ap[g2 * 64:(g2 + 1) * 64, 0:128].rearrange("p (d g two) -> p d g two", d=2, two=2)[:, :, :, g2]),
                        reads=[PT], writes=[ldt])
                S.op("act", lambda: A.activation(ldt.ap, ldt.ap, AF.Exp), reads=[ldt], writes=[ldt])
                S.op("dve", lambda: V.tensor_tensor(mag.ap, lre.ap, ldt.ap, ALU.mult), reads=[lre, ldt], writes=[mag])
                S.op("act", lambda: A.activation(mag.ap, mag.ap, AF.Exp), reads=[mag], writes=[mag])
                S.op("dve", lambda: V.tensor_tensor(ang.ap, lim.ap, ldt.ap, ALU.mult), reads=[lim, ldt], writes=[ang])
                S.op("dve", lambda: V.tensor_scalar(ki.ap, ang.ap, 1.0 / TWO_PI, None, ALU.mult), reads=[ang], writes=[ki])
                S.op("dve", lambda: V.tensor_copy(tq.ap, ki.ap), reads=[ki], writes=[tq])
                S.op("dve", lambda: V.scalar_tensor_tensor(red.ap, tq.ap, -TWO_PI, ang.ap, ALU.mult, ALU.add), reads=[tq, ang], writes=[red])
                S.op("dve", lambda: V.tensor_scalar(red.ap, red.ap, PIC, -PIC, ALU.min, ALU.max), reads=[red], writes=[red])
                S.op("act", lambda: A.activation(sn.ap, red.ap, AF.Sin), reads=[red], writes=[sn])
                S.op("dve", lambda: V.tensor_scalar(tq.ap, red.ap, math.pi / 2, -TWO_PI, ALU.is_gt, ALU.mult), reads=[red], writes=[tq])
                S.op("dve", lambda: V.scalar_tensor_tensor(red2.ap, red.ap, math.pi / 2, tq.ap, ALU.add, ALU.add), reads=[red, tq], writes=[red2])
                S.op("dve", lambda: V.tensor_scalar(red2.ap, red2.ap, PIC, -PIC, ALU.min, ALU.max), reads=[red2], writes=[red2])
                S.op("act", lambda: A.activation(cs.ap, red2.ap, AF.Sin), reads=[red2], writes=[cs])
                S.op("dve", lambda: V.tensor_tensor(abre.ap, mag.ap, cs.ap, ALU.mult), reads=[mag, cs], writes=[abre])
                S.op("dve", lambda: V.tensor_tensor(abim.ap, mag.ap, sn.ap, ALU.mult), reads=[mag, sn], writes=[abim])
                S.op("dve", lambda: V.tensor_scalar(nr.ap, abre.ap, -1.0, None, ALU.add), reads=[abre], writes=[nr])
                S.op("dve", lambda: V.tensor_tensor(den.ap, lre.ap, lre.ap, ALU.mult), reads=[lre], writes=[den])
                S.op("dve", lambda: V.tensor_tensor(tq.ap, lim.ap, lim.ap, ALU.mult), reads=[lim], writes=[tq])
                S.op("dve", lambda: V.tensor_tensor(den.ap, den.ap, tq.ap, ALU.add), reads=[den, tq], writes=[den])
                S.op("dve", lambda: V.reciprocal(den.ap, den.ap), reads=[den], writes=[den])
                S.op("dve", lambda: V.tensor_tensor(cre.ap, nr.ap, lre.ap, ALU.mult), reads=[nr, lre], writes=[cre])
                S.op("dve", lambda: V.tensor_tensor(tq.ap, abim.ap, lim.ap, ALU.mult), reads=[abim, lim], writes=[tq])
                S.op("dve", lambda: V.tensor_tensor(cre.ap, cre.ap, tq.ap, ALU.add), reads=[cre, tq], writes=[cre])
                S.op("dve", lambda: V.tensor_tensor(cre.ap, cre.ap, den.ap, ALU.mult), reads=[cre, den], writes=[cre])
                S.op("dve", lambda: V.tensor_tensor(cim.ap, abim.ap, lre.ap, ALU.mult), reads=[abim, lre], writes=[cim])
                S.op("dve", lambda: V.tensor_tensor(tq.ap, nr.ap, lim.ap, ALU.mult), reads=[nr, lim], writes=[tq])
                S.op("dve", lambda: V.tensor_tensor(cim.ap, cim.ap, tq.ap, ALU.subtract), reads=[cim, tq], writes=[cim])
                S.op("dve", lambda: V.tensor_tensor(cim.ap, cim.ap, den.ap, ALU.mult), reads=[cim, den], writes=[cim])
                S.op("dve", lambda: V.tensor_scalar(ncim.ap, cim.ap, -1.0, None, ALU.mult), reads=[cim], writes=[ncim])
                S.op("dve", lambda: V.tensor_copy(rc.ap[:, 0, :], cs.ap), reads=[cs], writes=[rc])
                S.op("dve", lambda: V.tensor_copy(rsn.ap[:, 0, :], sn.ap), reads=[sn], writes=[rsn])
                for k in range(1, 12):
                    S.op("dve", lambda k=k: V.tensor_tensor(tq.ap, rsn.ap[:, k - 1, :], rsn.ap[:, k - 1, :], ALU.mult), reads=[rsn], writes=[tq])
                    S.op("dve", lambda k=k: V.tensor_tensor(rc.ap[:, k, :], rc.ap[:, k - 1, :], rc.ap[:, k - 1, :], ALU.mult), reads=[rc], writes=[rc])
                    S.op("dve", lambda k=k: V.tensor_tensor(rc.ap[:, k, :], rc.ap[:, k, :], tq.ap, ALU.subtract), reads=[rc, tq], writes=[rc])
                    S.op("dve", lambda k=k: V.scalar_tensor_tensor(rsn.ap[:, k, :], rc.ap[:, k - 1, :], 2.0, rsn.ap[:, k - 1, :], ALU.mult, ALU.mult),
                         reads=[rc, rsn], writes=[rsn])
                S.op("dve", lambda: V.tensor_scalar(nrs.ap, rsn.ap, -1.0, None, ALU.mult), reads=[rsn], writes=[nrs])

                bsrc = [pho.sb(f"bsrc{k}", [128, 64, 16], F32) for k in range(2)]
                csrc = [pho.sb(f"csrc{k}", [128, 64, 16], F32) for k in range(2)]
                for k, (bs_, cs_) in enumerate(((ssm_b_re, ssm_c_re), (ssm_b_im, ssm_c_im))):
                    for d in range(2):
                        S.dma("sp", bsrc[k].ap[:, d * 32:(d + 1) * 32, :],
                              bs_[j, d].rearrange("g n q -> (g n) q").rearrange("(gp p) q -> p gp q", p=128), writes=[bsrc[k]])
                        for g2 in range(2):
                            for gp_ in range(32):
                                S.dma("act" if g2 else "sp", csrc[k].ap[g2 * 64:(g2 + 1) * 64, d * 32 + gp_, :],
                                      cs_[j, d, gp_ * 2 + g2].rearrange("p n -> n p"), writes=[csrc[k]],
                                      allow_slow_non_contiguous=True)

                for s in range(NSAMP):
                    it = 0
                    for (t0, n) in BLOCKS:
                        pos = t0 + NCX if t0 < NL else 0
                        norm_block(pho, R, s, t0, n, A1, 0, R["sq"], hF.ap[:, :, pos:pos + n], hF, it)
                        it += 1
                    with Phase(nc, S, f"s5s{i}_{s}") as ph:
                        hBk = ph.sb("hBk", [128, NT], BF16)
                        pad = [ph.sb(f"pad{k}", [128, 128], F32) for k in range(4)]
                        for k in range(4):
                            S.op("pool", lambda k=k: G.memset(pad[k].ap, 0.0), writes=[pad[k]])
                        bbt = ph.sb("bbt", [128, 2, 16], F32)
                        BBT = [ph.sb(f"BBT{k}", [128, 128], BF16) for k in range(2)]
                        CPt = [ph.sb(f"CP{k}", [128, 128], BF16) for k in range(3)]
                        Ct = ph.sb("Ct", [128, 512], F32)
                        St = ph.sb("St", [128, 512], F32)
                        tta = ph.sb("tta", [128, 256], F32)
                        ttb = ph.sb("ttb", [128, 256], F32)
                        wre = ph.rot("wre", 2, [128, 512], F32)
                        wim = ph.rot("wim", 2, [128, 512], F32)
                        prod = [ph.rot(f"prod{k}", 2, [128, 512], BF16) for k in range(4)]
                        ini = ph.rot("ini", 2, [128, 4], F32)
                        yacc = ph.sb("yacc", [128, NT], F32)
                        ga = ph.sb("ga", [128, NT], F32)
                        gb = ph.sb("gb", [128, NT], F32)
                        tm = [ph.sb(f"tm{k}", [128, 512], F32) for k in range(4)]
                        for k in range(3):
                            S.op("pool", lambda k=k: G.memset(CPt[k].ap, 0.0), writes=[CPt[k]])
                        bi = 0
                        for kc in range(KC):
                            S.op("act", lambda kc=kc: A.activation(hBk.ap[:, 0:NCX], hF.ap[:, kc, 0:NCX][:, ::-1], AF.Copy), reads=[hF], writes=[hBk])
                            S.op("act", lambda kc=kc: A.activation(hBk.ap[:, NCX:NT], hF.ap[:, kc, NCX:NT][:, ::-1], AF.Copy), reads=[hF], writes=[hBk])
                            S.op("pool", lambda: G.memset(yacc.ap, 0.0), writes=[yacc])
                            for d in range(2):
                                for gpl in range(4):
                                    gp = kc * 4 + gpl
                                    col = d * 32 + gp
                                    cc = slice(col, col + 1)
                                    pd = pad[gpl]
                                    for k in range(2):
                                        a_, b_ = (bsrc[0], bsrc[1]) if k == 0 else (bsrc[1], bsrc[0])
                                        sc2 = ncim if k == 0 else cim
                                        S.op("pool", lambda a_=a_, k=k, col=col, cc=cc: G.tensor_scalar(bbt.ap[:, k, :], a_.ap[:, col, :], cre.ap[:, cc], None, ALU.mult),
                                             reads=[a_, cre], writes=[bbt])
                                        S.op("dve", lambda b_=b_, k=k, col=col, cc=cc, sc2=sc2: V.scalar_tensor_tensor(
                                            bbt.ap[:, k, :], b_.ap[:, col, :], sc2.ap[:, cc], bbt.ap[:, k, :], ALU.mult, ALU.add), reads=[b_, sc2, bbt], writes=[bbt])
                                        for g2 in range(2):
                                            blk = (gpl * 2 + g2) * 16
                                            S.op("pool", lambda g2=g2, blk=blk, k=k, pd=pd: G.tensor_copy(pd.ap[g2 * 64:(g2 + 1) * 64, blk:blk + 16],
                                                                                                          bbt.ap[g2 * 64:(g2 + 1) * 64, k, :]), reads=[bbt], writes=[pd])
                                        S.group("pe", [lambda pd=pd: T.transpose(PT.ap[:, 0:128], pd.ap, ident.ap)], reads=[pd, ident], writes=[PT])
                                        S.op("act", lambda k=k: A.activation(BBT[k].ap, PT.ap[:, 0:128], AF.Copy), reads=[PT], writes=[BBT[k]])
                                    for g2 in range(2):
                                        blk = (gpl * 2 + g2) * 16
                                        for (kk, src_k, sgn) in ((0, 0, 1.0), (1, 0, -1.0), (2, 1, -1.0)):
                                            S.op("act", lambda g2=g2, blk=blk, kk=kk, src_k=src_k, sgn=sgn, col=col: A.activation(
                                                CPt[kk].ap[g2 * 64:(g2 + 1) * 64, blk:blk + 16], csrc[src_k].ap[g2 * 64:(g2 + 1) * 64, col, :],
                                                AF.Copy, scale=sgn), reads=[csrc[src_k]], writes=[CPt[kk]])
                                    S.op("pool", lambda: G.memset(Ct.ap[:, 0:1], 1.0), writes=[Ct])
                                    S.op("pool", lambda: G.memset(St.ap[:, 0:1], 0.0), writes=[St])
                                    for k in range(9):
                                        L = 1 << k
                                        ck, sk, nsk = rc.ap[:, k, cc], rsn.ap[:, k, cc], nrs.ap[:, k, cc]
                                        S.op("act", lambda L=L, nsk=nsk: A.activation(tta.ap[:, 0:L], St.ap[:, 0:L], AF.Copy, scale=nsk), reads=[St, nrs], writes=[tta])
                                        S.op("act", lambda L=L, sk=sk: A.activation(ttb.ap[:, 0:L], Ct.ap[:, 0:L], AF.Copy, scale=sk), reads=[Ct, rsn], writes=[ttb])
                                        S.op("dve", lambda L=L, ck=ck: V.scalar_tensor_tensor(Ct.ap[:, L:2 * L], Ct.ap[:, 0:L], ck, tta.ap[:, 0:L], ALU.mult, ALU.add),
                                             reads=[Ct, rc, tta], writes=[Ct])
                                        S.op("dve", lambda L=L, ck=ck: V.scalar_tensor_tensor(St.ap[:, L:2 * L], St.ap[:, 0:L], ck, ttb.ap[:, 0:L], ALU.mult, ALU.add),
                                             reads=[St, rc, ttb], writes=[St])
                                    prev = None
                                    for (p0, n) in FB:
                                        src = hF.ap[:, kc, p0:p0 + n] if d == 0 else hBk.ap[:, p0:p0 + n]
                                        sb_ = hF if d == 0 else hBk
                                        wr_, wi_ = wre[bi % 2], wim[bi % 2]
                                        pr_ = [prod[k][bi % 2] for k in range(4)]
                                        in_ = ini[bi % 2]
                                        bi += 1
                                        S.group("pe", [lambda src=src, n=n: T.matmul(PSre.ap[:, :n], BBT[0].ap, src, start=True, stop=True)], reads=[BBT[0], sb_], writes=[PSre])
                                        S.group("pe", [lambda src=src, n=n: T.matmul(PSim.ap[:, :n], BBT[1].ap, src, start=True, stop=True)], reads=[BBT[1], sb_], writes=[PSim])
                                        cb, sb2 = Ct.ap[:, 0:n], St.ap[:, 0:n]
                                        S.op("dve", lambda n=n, cb=cb: V.tensor_tensor(tm[0].ap[:, :n], PSre.ap[:, :n], cb, ALU.mult), reads=[PSre, Ct], writes=[tm[0]])
                                        S.op("dve", lambda n=n, sb2=sb2: V.tensor_tensor(tm[1].ap[:, :n], PSim.ap[:, :n], sb2, ALU.mult), reads=[PSim, St], writes=[tm[1]])
                                        S.op("pool", lambda n=n, wr_=wr_: G.tensor_tensor(wr_.ap[:, :n], tm[0].ap[:, :n], tm[1].ap[:, :n], ALU.add), reads=[tm[0], tm[1]], writes=[wr_])
                                        S.op("dve", lambda n=n, cb=cb: V.tensor_tensor(tm[2].ap[:, :n], PSim.ap[:, :n], cb, ALU.mult), reads=[PSim, Ct], writes=[tm[2]])
                                        S.op("dve", lambda n=n, sb2=sb2: V.tensor_tensor(tm[3].ap[:, :n], PSre.ap[:, :n], sb2, ALU.mult), reads=[PSre, St], writes=[tm[3]])
                                        S.op("pool", lambda n=n, wi_=wi_: G.tensor_tensor(wi_.ap[:, :n], tm[2].ap[:, :n], tm[3].ap[:, :n], ALU.subtract), reads=[tm[2], tm[3]], writes=[wi_])
                                        if prev is not None:
                                            (pw_r, pw_i, pn) = prev
                                            lvl = 8 if pn == 256 else 9
                                            cl, sl, nsl = rc.ap[:, lvl, cc], rsn.ap[:, lvl, cc], nrs.ap[:, lvl, cc]
                                            er, ei_ = pw_r.ap[:, pn - 1:pn], pw_i.ap[:, pn - 1:pn]
                                            S.op("dve", lambda in_=in_, ei_=ei_, nsl=nsl: V.tensor_scalar(in_.ap[:, 2:3], ei_, nsl, None, ALU.mult), reads=[pw_i, nrs], writes=[in_])
                                            S.op("dve", lambda in_=in_, er=er, cl=cl: V.scalar_tensor_tensor(in_.ap[:, 0:1], er, cl, in_.ap[:, 2:3], ALU.mult, ALU.add),
                                                 reads=[pw_r, rc, in_], writes=[in_])
                                            S.op("dve", lambda in_=in_, er=er, sl=sl: V.tensor_scalar(in_.ap[:, 3:4], er, sl, None, ALU.mult), reads=[pw_r, rsn], writes=[in_])
                                            S.op("dve", lambda in_=in_, ei_=ei_, cl=cl: V.scalar_tensor_tensor(in_.ap[:, 1:2], ei_, cl, in_.ap[:, 3:4], ALU.mult, ALU.add),
                                                 reads=[pw_i, rc, in_], writes=[in_])
                                            i_re, i_im = in_.ap[:, 0:1], in_.ap[:, 1:2]
                                            rd = [in_]
                                        else:
                                            i_re, i_im = 0.0, 0.0
                                            rd = []
                                        S.op("dve", lambda wr_=wr_, n=n, cc=cc, i_re=i_re: V.tensor_tensor_scan(
                                            wr_.ap[:, :n], mag.ap[:, cc].to_broadcast([128, n]), wr_.ap[:, :n], i_re, ALU.mult, ALU.add), reads=[wr_, mag] + rd, writes=[wr_])
                                        S.op("dve", lambda wi_=wi_, n=n, cc=cc, i_im=i_im: V.tensor_tensor_scan(
                                            wi_.ap[:, :n], mag.ap[:, cc].to_broadcast([128, n]), wi_.ap[:, :n], i_im, ALU.mult, ALU.add), reads=[wi_, mag] + rd, writes=[wi_])
                                        prev = (wr_, wi_, n)
                                        S.op("dve", lambda n=n, cb=cb, wr_=wr_, o=pr_[0]: V.tensor_tensor(o.ap[:, :n], wr_.ap[:, :n], cb, ALU.mult), reads=[wr_, Ct], writes=[pr_[0]])
                                        S.op("pool", lambda n=n, sb2=sb2, wi_=wi_, o=pr_[1]: G.tensor_tensor(o.ap[:, :n], wi_.ap[:, :n], sb2, ALU.mult), reads=[wi_, St], writes=[pr_[1]])
                                        S.op("pool", lambda n=n, sb2=sb2, wr_=wr_, o=pr_[2]: G.tensor_tensor(o.ap[:, :n], wr_.ap[:, :n], sb2, ALU.mult), reads=[wr_, St], writes=[pr_[2]])
                                        S.op("pool", lambda n=n, cb=cb, wi_=wi_, o=pr_[3]: G.tensor_tensor(o.ap[:, :n], wi_.ap[:, :n], cb, ALU.mult), reads=[wi_, Ct], writes=[pr_[3]])
                                        lts = [CPt[0], CPt[1], CPt[2], CPt[2]]
                                        S.group("pe", [lambda n=n, q=q, lts=lts, pr_=pr_: T.matmul(PSy.ap[:, :n], lts[q].ap, pr_[q].ap[:, :n], start=(q == 0), stop=(q == 3))
                                                       for q in range(4)], reads=[CPt[0], CPt[1], CPt[2]] + pr_, writes=[PSy])
                                        if d == 0:
                                            ya = yacc.ap[:, p0:p0 + n]
                                        elif p0 == 0:
                                            ya = yacc.ap[:, 0:NCX][:, ::-1]
                                        else:
                                            hi_ = NT - (p0 - NCX)
                                            ya = yacc.ap[:, hi_ - n:hi_][:, ::-1]
                                        S.op("dve", lambda n=n, ya=ya: V.tensor_tensor(ya, ya, PSy.ap[:, :n], ALU.add), reads=[PSy, yacc], writes=[yacc])
                                    for k in range(3):
                                        S.op("pool", lambda k=k: G.memset(CPt[k].ap, 0.0), writes=[CPt[k]])
                            S.op("dve", lambda kc=kc: V.scalar_tensor_tensor(yacc.ap, hF.ap[:, kc, :], dsk.ap[:, kc:kc + 1], yacc.ap, ALU.mult, ALU.add),
                                 reads=[hF, dsk, yacc], writes=[yacc])
                            S.op("act", lambda: A.activation(ga.ap, yacc.ap, AF.Square), reads=[yacc], writes=[ga])
                            S.op("pool", lambda: G.tensor_scalar(ga.ap, ga.ap, 0.044715, 1.0, ALU.mult, ALU.add), reads=[ga], writes=[ga])
                            S.op("pool", lambda: G.tensor_tensor(ga.ap, ga.ap, yacc.ap, ALU.mult), reads=[ga, yacc], writes=[ga])
                            S.op("act", lambda: A.activation(ga.ap, ga.ap, AF.Tanh, scale=0.7978845608028654), reads=[ga], writes=[ga])
                            S.op("act", lambda: A.activation(gb.ap, yacc.ap, AF.Copy, scale=0.5), reads=[yacc], writes=[gb])
                            S.op("dve", lambda kc=kc: V.scalar_tensor_tensor(hF.ap[:, kc, :], ga.ap, 1.0, gb.ap, ALU.add, ALU.mult),
                                 reads=[ga, gb], writes=[hF])
                    with Phase(nc, S, f"s5g{i}_{s}") as ph:
                        w1 = ph.sb("w1", [128, KC, D], BF16)
                        w2 = ph.sb("w2", [128, KC, D], BF16)
                        S.dma("pool", w1.ap, ssm_w_glu1[j].rearrange("(k p) f -> p k f", p=128), writes=[w1])
                        S.dma("pool", w2.ap, ssm_w_glu2[j].rearrange("(k p) f -> p k f", p=128), writes=[w2])
                        sg_ = ph.rot("sg", 2, [128, 512], F32)
                        xb = R["xb"]
                        for bi, (p0, n) in enumerate(FB):
                            if p0 == 0 and not with_ctx:
                                continue
                            t0 = NL if p0 == 0 else p0 - NCX
                            v = 2 if p0 == 0 else s
                            x = xb[bi % 2]
                            S.dma("sp", x.ap[:, :, :n], xT_view(s, t0, n), reads=[xT_b[s]], writes=[x])
                            for fc in range(KC):
                                fs = slice(fc * 128, (fc + 1) * 128)
                                S.group("pe", [lambda kc=kc, fs=fs, p0=p0, n=n: T.matmul(PSre.ap[:, :n], w1.ap[:, kc, fs], hF.ap[:, kc, p0:p0 + n],
                                                                                         start=(kc == 0), stop=(kc == KC - 1)) for kc in range(KC)], reads=[w1, hF], writes=[PSre])
                                S.group("pe", [lambda kc=kc, fs=fs, p0=p0, n=n: T.matmul(PSim.ap[:, :n], w2.ap[:, kc, fs], hF.ap[:, kc, p0:p0 + n],
                                                                                         start=(kc == 0), stop=(kc == KC - 1)) for kc in range(KC)], reads=[w2, hF], writes=[PSim])
                                g_ = sg_[fc % 2]
                                S.op("act", lambda g_=g_, n=n: A.activation(g_.ap[:, :n], PSim.ap[:, :n], AF.Sigmoid), reads=[PSim], writes=[g_])
                                S.op("dve", lambda g_=g_, n=n: V.tensor_tensor(g_.ap[:, :n], g_.ap[:, :n], PSre.ap[:, :n], ALU.mult), reads=[g_, PSre], writes=[g_])
                                S.op("dve", lambda g_=g_, n=n, fc=fc, x=x, v=v: V.scalar_tensor_tensor(x.ap[:, fc, :n], g_.ap[:, :n], mod.ap[:, 16 + fc, v:v + 1], x.ap[:, fc, :n],
                                                                                                       ALU.mult, ALU.add), reads=[g_, mod, x], writes=[x])
                            S.dma("act", xT_view(s, t0, n), x.ap[:, :, :n], reads=[x], writes=[xT_b[s]])

        MIXERS = {0: attn, 1: s5}
        cur = -1
        for (i, part) in cfg:
            if i != cur:
                adaln(i)
                cur = i
            if part == "mix":
                MIXERS[i % 2](i)
            else:
                moe_full(i)
        final()
        S.finish()
    return nc, S


_CONST = {}


def _consts():
    if not _CONST:
        _CONST["k_ident"] = np.eye(128, dtype=np.float32)
        t = np.arange(NL)
        row = (t // 64).astype(np.float32)
        col = (t % 64).astype(np.float32)
        inv = (10000.0 ** (-np.arange(16, dtype=np.float32) / 16)).astype(np.float32)
        C = np.zeros((128, NL), np.float32)
        Sg = np.zeros((128, NL), np.float32)
        for p in range(128):
            dd = p % 64
            pos = row if dd < 32 else col
            ang = pos * inv[dd % 16]
            C[p] = np.cos(ang)
            Sg[p] = np.sin(ang) * (-1.0 if (dd % 32) < 16 else 1.0)
        _CONST["k_ropec"] = C
        _CONST["k_ropes"] = Sg
    return _CONST


_NC_CACHE = {}


def kernel(**inputs):
    n = 8
    if "nc" not in _NC_CACHE:
        _NC_CACHE["nc"] = build()[0]
    nc = _NC_CACHE["nc"]
    shared = {k: np.ascontiguousarray(v) for k, v in inputs.items() if k not in ("x", "c", "ctx")}
    shared.update(_consts())
    in_maps = []
    for r in range(n):
        m = dict(shared)
        m["x"] = np.ascontiguousarray(inputs["x"][2 * r:2 * r + 2])
        m["c"] = np.ascontiguousarray(inputs["c"][2 * r:2 * r + 2])
        m["ctx"] = np.ascontiguousarray(inputs["ctx"][2 * r:2 * r + 2])
        in_maps.append(m)
    res = run_bass_kernel_spmd(nc, in_maps, core_ids=list(range(n)))
    return np.concatenate([r["out"] for r in res.results], axis=0).astype(np.float32)
```

```python
import math
from contextlib import ExitStack
import numpy as np
import ml_dtypes
import concourse.bass as bass
import concourse.mybir as mybir
from concourse.bass_utils import run_bass_kernel_spmd

F32 = mybir.dt.float32
BF16 = mybir.dt.bfloat16
U32 = mybir.dt.uint32
I32 = mybir.dt.int32
AF = mybir.ActivationFunctionType
ALU = mybir.AluOpType

D = 1024
NL = 2048
NCX = 256
NT = NL + NCX
KC = 8
NE = 16
FF = 2048
DEPTH = 4
EPS = 1e-6
NSAMP = 2
CAPL = 256
CAPC = 32
NCOL = 2 * CAPL + 2 * CAPC


class Buf:
    __slots__ = ("name", "ap", "writer", "readers")

    def __init__(self, name, ap):
        self.name = name
        self.ap = ap
        self.writer = None
        self.readers = []


class Sync:
    NDMA = 6

    def __init__(self, nc):
        self.nc = nc
        self.engs = {"pe": nc.tensor, "act": nc.scalar, "dve": nc.vector, "pool": nc.gpsimd, "sp": nc.sync}
        self.sem = {k: nc.alloc_semaphore(name=f"s_{k}") for k in self.engs}
        self.cnt = {k: 0 for k in self.engs}
        self.seen = {k: {} for k in self.engs}
        self.dsem = {k: [nc.alloc_semaphore(name=f"d_{k}{i}") for i in range(self.NDMA)] for k in ("sp", "act", "pool")}
        self.dcnt = {k: 0 for k in self.dsem}
        self.dlast = {k: [None] * self.NDMA for k in self.dsem}
        self.out_tickets = []
        self.ninstr = 0

    def _wait(self, e, tick):
        if tick is None:
            return
        sem, val = tick
        key = sem.name
        if e == "pe" and key == "s_pe":
            return
        if self.seen[e].get(key, 0) >= val:
            return
        self.engs[e].wait_ge(sem, val)
        self.seen[e][key] = val

    def deps(self, e, reads, writes):
        for b in reads:
            self._wait(e, b.writer)
        for b in writes:
            self._wait(e, b.writer)
            for r in b.readers:
                self._wait(e, r)

    def commit(self, tick, reads, writes):
        for b in reads:
            b.readers.append(tick)
            if len(b.readers) > 48:
                last = {}
                for t in b.readers:
                    last[t[0].name] = t
                b.readers = list(last.values())
        for b in writes:
            b.writer = tick
            b.readers = []

    def op(self, e, fn, reads=(), writes=()):
        self.deps(e, reads, writes)
        ins = fn()
        self.cnt[e] += 1
        ins.then_inc(self.sem[e], 1)
        tick = (self.sem[e], self.cnt[e])
        self.commit(tick, reads, writes)
        self.ninstr += 1
        return tick

    def group(self, e, fns, reads=(), writes=()):
        self.deps(e, reads, writes)
        ins = None
        for fn in fns:
            ins = fn()
            self.ninstr += 1
        self.cnt[e] += 1
        ins.then_inc(self.sem[e], 1)
        tick = (self.sem[e], self.cnt[e])
        self.commit(tick, reads, writes)
        return tick

    def _dma_common(self, q, issue, reads, writes, is_output):
        j = self.dcnt[q]
        slot = j % self.NDMA
        self._wait(q, self.dlast[q][slot])
        self.deps(q, reads, writes)
        sem = self.dsem[q][slot]
        val = 16 * (j // self.NDMA + 1)
        issue().then_inc(sem, 16)
        self.dcnt[q] += 1
        tick = (sem, val)
        self.dlast[q][slot] = tick
        self.commit(tick, reads, writes)
        if is_output:
            self.out_tickets.append(tick)
        self.ninstr += 1
        return tick

    def dma(self, q, out_ap, in_ap, reads=(), writes=(), is_output=False, **kw):
        return self._dma_common(q, lambda: self.engs[q].dma_start(out=out_ap, in_=in_ap, **kw), reads, writes, is_output)

    def idma(self, reads=(), writes=(), **kw):
        return self._dma_common("pool", lambda: self.nc.gpsimd.indirect_dma_start(**kw), reads, writes, False)

    def barrier(self):
        ticks = [(self.sem[e], self.cnt[e]) for e in self.engs if self.cnt[e] > 0]
        for q in self.dsem:
            ticks += [t for t in self.dlast[q] if t is not None]
        for e in self.engs:
            for t in ticks:
                self._wait(e, t)

    def finish(self):
        for q in self.dsem:
            for t in self.dlast[q]:
                self._wait(q, t)
        for t in self.out_tickets:
            self._wait("sp", t)


class Phase:
    def __init__(self, nc, S, name):
        self.nc, self.S, self.name = nc, S, name
        self.stack = ExitStack()
        self.n = 0

    def __enter__(self):
        self.stack.__enter__()
        return self

    def __exit__(self, *a):
        self.S.barrier()
        return self.stack.__exit__(*a)

    def sb(self, name, shape, dt):
        self.n += 1
        t = self.stack.enter_context(self.nc.sbuf_tensor(f"{self.name}_{name}_{self.n}", list(shape), dt))
        return Buf(name, t.ap())

    def ps(self, name, shape, dt=F32):
        self.n += 1
        t = self.stack.enter_context(self.nc.psum_tensor(f"{self.name}_{name}_{self.n}", list(shape), dt))
        return Buf(name, t.ap())

    def rot(self, name, n, shape, dt):
        return [self.sb(f"{name}{i}", shape, dt) for i in range(n)]


def build(cfg=None, dbg=False):
    if cfg is None:
        cfg = [(i, p) for i in range(DEPTH) for p in ("mix", "moe")]
    nc = bass.Bass("TRN2", target_bir_lowering=False)

    def din(name, shape, dt=F32):
        return nc.dram_tensor(name, list(shape), dt, kind="ExternalInput").ap()

    x_in = din("x", [NSAMP, NL, D])
    c_in = din("c", [NSAMP, D])
    ctx_in = din("ctx", [NSAMP, NCX, D])
    cctx_in = din("c_ctx", [D])
    ada_w = din("ada_w", [DEPTH, D, 6 * D])
    ada_b = din("ada_b", [DEPTH, 6 * D])
    norm1_g = din("norm1_g", [DEPTH, D])
    norm2_g = din("norm2_g", [DEPTH, D])
    final_g = din("final_g", [D])
    attn_w_qkv = din("attn_w_qkv", [2, D, 3 * D])
    attn_w_o = din("attn_w_o", [2, D, D])
    attn_lam = [din(n, [2, 64]) for n in ("attn_lam_q1", "attn_lam_k1", "attn_lam_q2", "attn_lam_k2")]
    attn_subln_g = din("attn_subln_g", [2, 128])
    ssm_lam_re = din("ssm_lam_re", [2, 2, 64, 64])
    ssm_lam_im = din("ssm_lam_im", [2, 2, 64, 64])
    ssm_log_dt = din("ssm_log_dt", [2, 2, 64])
    ssm_b_re = din("ssm_b_re", [2, 2, 64, 64, 16])
    ssm_b_im = din("ssm_b_im", [2, 2, 64, 64, 16])
    ssm_c_re = din("ssm_c_re", [2, 2, 64, 16, 64])
    ssm_c_im = din("ssm_c_im", [2, 2, 64, 16, 64])
    ssm_d = din("ssm_d", [2, D])
    ssm_w_glu1 = din("ssm_w_glu1", [2, D, D])
    ssm_w_glu2 = din("ssm_w_glu2", [2, D, D])
    moe_w_router = din("moe_w_router", [DEPTH, D, NE])
    moe_b_router = din("moe_b_router", [DEPTH, NE])
    moe_w_gate = din("moe_w_gate", [DEPTH, NE, D, FF])
    moe_w_up = din("moe_w_up", [DEPTH, NE, D, FF])
    moe_w_down = din("moe_w_down", [DEPTH, NE, FF, D])
    ident_in = din("k_ident", [128, 128])
    ropec_in = din("k_ropec", [128, NL])
    ropes_in = din("k_ropes", [128, NL])
    out_d = nc.dram_tensor("out", [NSAMP, NL, D], F32, kind="ExternalOutput").ap()

    xT_t = nc.dram_tensor("xT_scr", [NSAMP, KC, 128, NT], F32, kind="ExternalOutput" if dbg else "Internal").ap()
    h2tok_t = nc.dram_tensor("h2tok_scr", [NSAMP, NT, D], BF16, kind="Internal").ap()
    macc_t = nc.dram_tensor("macc_scr", [NSAMP, NT, D], F32, kind="Internal").ap()
    s5w_t = nc.dram_tensor("s5w_scr", [64, 5, 128, 128], BF16, kind="Internal").ap()
    s5tab_t = nc.dram_tensor("s5tab_scr", [64, 2, 128, 512], F32, kind="Internal").ap()

    S = Sync(nc)
    xT_b = [Buf(f"xT{s}", xT_t[s]) for s in range(NSAMP)]
    h2l_b = [Buf(f"h2l{s}", h2tok_t[s, 0:NL, :]) for s in range(NSAMP)]
    h2c_b = [Buf(f"h2c{s}", h2tok_t[s, NL:NT, :]) for s in range(NSAMP)]
    mal_b = [Buf(f"mal{s}", macc_t[s, 0:NL, :]) for s in range(NSAMP)]
    mac_b = [Buf(f"mac{s}", macc_t[s, NL:NT, :]) for s in range(NSAMP)]

    def xT_view(s, t0, n):
        return xT_t[s].rearrange("k p t -> p k t")[:, :, t0:t0 + n]

    BLOCKS = [(0, 512), (512, 512), (1024, 512), (1536, 512), (2048, 256)]

    V, T, G, A = nc.vector, nc.tensor, nc.gpsimd, nc.scalar

    with ExitStack() as gstack:
        def gsb(name, shape, dt):
            t = gstack.enter_context(nc.sbuf_tensor("g_" + name, list(shape), dt))
            return Buf(name, t.ap())

        ident = gsb("ident", [128, 128], F32)
        identb = gsb("identb", [128, 128], BF16)
        ones = gsb("ones", [128, 128], F32)
        zeros = gsb("zeros", [128, 1024], F32)
        scT = gsb("scT", [128, KC, 4], F32)
        mod = gsb("mod", [128, 48, 4], F32)
        A1 = gsb("A1", [128, KC, 4], F32)
        A2 = gsb("A2", [128, KC, 4], F32)
        n1g = gsb("n1g", [128, KC], F32)
        n2g = gsb("n2g", [128, KC], F32)
        adab = gsb("adab", [128, 48], F32)

        with Phase(nc, S, "p0") as ph:
            S.dma("sp", ident.ap, ident_in, writes=[ident])
            S.op("dve", lambda: V.tensor_copy(identb.ap, ident.ap), reads=[ident], writes=[identb])
            S.op("pool", lambda: G.memset(ones.ap, 1.0), writes=[ones])
            S.op("pool", lambda: G.memset(zeros.ap, 0.0), writes=[zeros])
            S.op("pool", lambda: G.memset(scT.ap, 0.0), writes=[scT])
            craw = ph.sb("craw", [128, KC, 4], F32)
            S.op("pool", lambda: G.memset(craw.ap, 0.0), writes=[craw])
            for v in range(3):
                src = c_in[v] if v < 2 else cctx_in
                S.dma("sp", craw.ap[:, :, v], src.rearrange("(k p) -> p k", p=128), writes=[craw],
                      allow_slow_non_contiguous=True)
            S.op("act", lambda: A.activation(scT.ap, craw.ap, AF.Silu), reads=[craw], writes=[scT])
            xin = ph.rot("xin", 2, [128, D], F32)
            xtt = ph.rot("xtt", 2, [128, KC, 128], F32)
            pst = [ph.ps("pst0", [128, 512]), ph.ps("pst1", [128, 512])]
            it = 0
            for s in range(NSAMP):
                for tt in range(NT // 128):
                    src = x_in[s, tt * 128:(tt + 1) * 128, :] if tt < 16 else ctx_in[s, (tt - 16) * 128:(tt - 15) * 128, :]
                    xi = xin[it % 2]
                    xo = xtt[it % 2]
                    S.dma("sp" if it % 2 == 0 else "act", xi.ap, src, writes=[xi])
                    for half in range(2):
                        p = pst[half]
                        S.group("pe", [lambda j=j, p=p, xi=xi, half=half: T.transpose(
                            p.ap[:, j * 128:(j + 1) * 128], xi.ap[:, (half * 4 + j) * 128:(half * 4 + j + 1) * 128], ident.ap)
                            for j in range(4)], reads=[xi, ident], writes=[p])
                        S.op("dve" if half == 0 else "act",
                             (lambda p=p, xo=xo, half=half: V.tensor_copy(
                                 xo.ap[:, half * 4:half * 4 + 4, :], p.ap.rearrange("p (k t) -> p k t", k=4))) if half == 0 else
                             (lambda p=p, xo=xo, half=half: A.activation(
                                 xo.ap[:, half * 4:half * 4 + 4, :], p.ap.rearrange("p (k t) -> p k t", k=4), AF.Copy)),
                             reads=[p], writes=[xo])
                    S.dma("sp" if it % 2 == 1 else "act", xT_view(s, tt * 128, 128), xo.ap, reads=[xo], writes=[xT_b[s]])
                    it += 1

        def adaln(i):
            with Phase(nc, S, f"ada{i}") as ph:
                S.dma("sp", adab.ap, ada_b[i].rearrange("(j p) -> p j", p=128), writes=[adab], allow_slow_non_contiguous=True)
                S.dma("act", n1g.ap, norm1_g[i].rearrange("(k p) -> p k", p=128), writes=[n1g], allow_slow_non_contiguous=True)
                S.dma("act", n2g.ap, norm2_g[i].rearrange("(k p) -> p k", p=128), writes=[n2g], allow_slow_non_contiguous=True)
                wst = ph.rot("wst", 2, [128, KC, 512], F32)
                pm = ph.ps("pm", [128, 48, 4])
                for jb in range(12):
                    w = wst[jb % 2]
                    S.dma("sp" if jb % 2 == 0 else "act", w.ap,
                          ada_w[i][:, jb * 512:(jb + 1) * 512].rearrange("(k p) f -> p k f", p=128), writes=[w])
                    for fs in range(4):
                        j = jb * 4 + fs
                        S.group("pe", [lambda kc=kc, j=j, fs=fs, w=w: T.matmul(
                            pm.ap[:, j, :], w.ap[:, kc, fs * 128:(fs + 1) * 128], scT.ap[:, kc, :], start=(kc == 0), stop=(kc == KC - 1))
                            for kc in range(KC)], reads=[w, scT], writes=[pm])
                for v in range(3):
                    S.op("dve", lambda v=v: V.tensor_tensor(mod.ap[:, :, v], pm.ap[:, :, v], adab.ap, ALU.add),
                         reads=[pm, adab], writes=[mod])
                for v in range(3):
                    S.op("dve", lambda v=v: V.scalar_tensor_tensor(A1.ap[:, :, v], mod.ap[:, 8:16, v], 1.0, n1g.ap, ALU.add, ALU.mult),
                         reads=[mod, n1g], writes=[A1])
                    S.op("dve", lambda v=v: V.scalar_tensor_tensor(A2.ap[:, :, v], mod.ap[:, 32:40, v], 1.0, n2g.ap, ALU.add, ALU.mult),
                         reads=[mod, n2g], writes=[A2])

        def norm_block(ph, R, s, t0, n, Acoef, shbase, h32, hb_ap, hb_buf, it):
            v = s if t0 < NL else 2
            xb = R["xb"][it % 2]
            sq = R["sq"]
            S.dma("sp" if it % 2 == 0 else "act", xb.ap[:, :, :n], xT_view(s, t0, n), reads=[xT_b[s]], writes=[xb])
            S.op("act", lambda: A.activation(sq.ap[:, :, :n], xb.ap[:, :, :n], AF.Square), reads=[xb], writes=[sq])
            ssp = R["ssp"]
            S.group("pe", [lambda kc=kc: T.matmul(ssp.ap[:, :n], ones.ap, sq.ap[:, kc, :n], start=(kc == 0), stop=(kc == KC - 1))
                           for kc in range(KC)], reads=[ones, sq], writes=[ssp])
            rstd = R["rstd"]
            S.op("act", lambda: A.activation(rstd.ap[:, :n], ssp.ap[:, :n], AF.Sqrt, bias=R["eps"].ap, scale=1.0 / D),
                 reads=[ssp, R["eps"]], writes=[rstd])
            S.op("dve", lambda: V.reciprocal(rstd.ap[:, :n], rstd.ap[:, :n]), reads=[rstd], writes=[rstd])
            for kc in range(KC):
                S.op("dve", lambda kc=kc: V.tensor_tensor(sq.ap[:, kc, :n], xb.ap[:, kc, :n], rstd.ap[:, :n], ALU.mult),
                     reads=[xb, rstd], writes=[sq])
            for kc in range(KC):
                S.op("pool", lambda kc=kc: G.tensor_scalar(h32.ap[:, kc, :n], sq.ap[:, kc, :n], Acoef.ap[:, kc, v:v + 1],
                                                           mod.ap[:, shbase + kc, v:v + 1], ALU.mult, ALU.add),
                     reads=[sq, Acoef, mod], writes=[h32])
            S.op("act", lambda: A.activation(hb_ap, h32.ap[:, :, :n], AF.Copy), reads=[h32], writes=[hb_buf])

        def norm_resources(ph, ssp=None):
            R = {"xb": ph.rot("xb", 2, [128, KC, 512], F32), "sq": ph.sb("sq", [128, KC, 512], F32),
                 "ssp": ssp if ssp is not None else ph.ps("ssp", [128, 512]), "rstd": ph.sb("rstd", [128, 512], F32), "eps": ph.sb("eps", [128, 1], F32)}
            S.op("pool", lambda: G.memset(R["eps"].ap, EPS), writes=[R["eps"]])
            return R

        def moe_full(i):
            with_ctx = i < DEPTH - 1
            blocks = BLOCKS if with_ctx else BLOCKS[:4]
            with Phase(nc, S, f"moeO{i}") as pho:
                idxT = [pho.sb(f"idxT{s}", [128, 3, NE], U32) for s in range(NSAMP)]
                valT = [pho.sb(f"valT{s}", [128, 3, NE], F32) for s in range(NSAMP)]
                for s in range(NSAMP):
                    S.op("pool", lambda s=s: G.memset(idxT[s].ap, 0), writes=[idxT[s]])
                    S.op("pool", lambda s=s: G.memset(valT[s].ap, 0.0), writes=[valT[s]])
                with Phase(nc, S, f"moeR{i}") as ph:
                    R = norm_resources(ph)
                    wr = ph.sb("wr", [128, KC, NE], F32)
                    br = ph.sb("br", [NE, 1], F32)
                    S.dma("sp", wr.ap, moe_w_router[i].rearrange("(k p) e -> p k e", p=128), writes=[wr])
                    S.dma("sp", br.ap, moe_b_router[i].rearrange("(e o) -> e o", o=1), writes=[br])
                    zi = 0
                    for s in range(NSAMP):
                        for tt in range(NT // 128 if with_ctx else NL // 128):
                            S.dma("sp" if zi % 2 == 0 else "act", macc_t[s, tt * 128:(tt + 1) * 128, :], zeros.ap,
                                  reads=[zeros], writes=[mal_b[s] if tt < 16 else mac_b[s]])
                            zi += 1
                    h32 = ph.sb("h32", [128, KC, 512], F32)
                    hb = ph.rot("hb", 2, [128, KC, 512], BF16)
                    expT = [ph.sb(f"expT{s}", [NE, NT], F32) for s in range(NSAMP)]
                    aff = [ph.sb(f"aff{s}", [NE, NT], F32) for s in range(NSAMP)]
                    lgp = ph.ps("lgp", [128, 512])
                    smp = ph.ps("smp", [128, 512])
                    rs = ph.sb("rs", [NE, 512], F32)
                    ptb = ph.ps("ptb", [128, 512])
                    ptb_bf = ptb.ap.bitcast(BF16)
                    tok = ph.rot("tok", 2, [128, D], BF16)
                    it = 0
                    ti = 0
                    for s in range(NSAMP):
                        for (t0, n) in blocks:
                            hbb = hb[it % 2]
                            norm_block(ph, R, s, t0, n, A2, 24, h32, hbb.ap[:, :, :n], hbb, it)
                            S.group("pe", [lambda kc=kc, n=n: T.matmul(lgp.ap[0:NE, :n], wr.ap[:, kc, :], h32.ap[:, kc, :n],
                                                                         start=(kc == 0), stop=(kc == KC - 1)) for kc in range(KC)],
                                    reads=[wr, h32], writes=[lgp])
                            S.op("act", lambda s=s, t0=t0, n=n: A.activation(expT[s].ap[:, t0:t0 + n], lgp.ap[0:NE, :n], AF.Exp, bias=br.ap),
                                 reads=[lgp, br], writes=[expT[s]])
                            S.group("pe", [lambda s=s, t0=t0, n=n: T.matmul(smp.ap[0:NE, :n], ones.ap[0:NE, 0:NE], expT[s].ap[:, t0:t0 + n],
                                                                          start=True, stop=True)], reads=[ones, expT[s]], writes=[smp])
                            S.op("dve", lambda n=n: V.reciprocal(rs.ap[:, :n], smp.ap[0:NE, :n]), reads=[smp], writes=[rs])
                            S.op("dve", lambda s=s, t0=t0, n=n: V.tensor_tensor(aff[s].ap[:, t0:t0 + n], expT[s].ap[:, t0:t0 + n], rs.ap[:, :n], ALU.mult),
                                 reads=[expT[s], rs], writes=[aff[s]])
                            for tt in range(n // 128):
                                tk = tok[ti % 2]
                                S.group("pe", [lambda kc=kc, tt=tt, hbb=hbb: T.transpose(
                                    ptb_bf[:, kc * 128:(kc + 1) * 128], hbb.ap[:, kc, tt * 128:(tt + 1) * 128], identb.ap) for kc in range(KC)],
                                    reads=[hbb, identb], writes=[ptb])
                                S.op("dve", lambda tk=tk: V.tensor_copy(tk.ap, ptb_bf), reads=[ptb], writes=[tk])
                                r0 = t0 + tt * 128
                                S.dma("sp" if ti % 2 == 0 else "act", h2tok_t[s, r0:r0 + 128, :], tk.ap, reads=[tk],
                                      writes=[h2l_b[s] if r0 < NL else h2c_b[s]])
                                ti += 1
                            it += 1
                    vals = [ph.sb(f"vals{s}", [NE, CAPL + CAPC], F32) for s in range(NSAMP)]
                    idx = [ph.sb(f"idx{s}", [NE, CAPL + CAPC], U32) for s in range(NSAMP)]
                    idxf = [ph.sb(f"idxf{s}", [NE, CAPL + CAPC], F32) for s in range(NSAMP)]
                    segs = [(0, NL, 0, CAPL // 8)] + ([(NL, NCX, CAPL, CAPC // 8)] if with_ctx else [])
                    for (a0, an, c0, rounds) in segs:
                        for r in range(rounds):
                            for s in range(NSAMP):
                                av = aff[s].ap[:, a0:a0 + an]
                                vv = vals[s].ap[:, c0 + r * 8:c0 + r * 8 + 8]
                                S.op("dve", lambda av=av, vv=vv: V.max(vv, av), reads=[aff[s]], writes=[vals[s]])
                                S.op("dve", lambda av=av, vv=vv, s=s, c0=c0, r=r: V.max_index(idx[s].ap[:, c0 + r * 8:c0 + r * 8 + 8], vv, av),
                                     reads=[aff[s], vals[s]], writes=[idx[s]])
                                S.op("dve", lambda av=av, vv=vv: V.match_replace(av, vv, av, -1.0), reads=[vals[s]], writes=[aff[s]])
                    ncap = CAPL + (CAPC if with_ctx else 0)
                    for s in range(NSAMP):
                        S.op("dve", lambda s=s: V.tensor_copy(idxf[s].ap[:, :ncap], idx[s].ap[:, :ncap]), reads=[idx[s]], writes=[idxf[s]])
                        pieces = [(0, 128, 0), (128, 128, 1)] + ([(256, 32, 2)] if with_ctx else [])
                        for (c0, cn, slot) in pieces:
                            S.group("pe", [lambda s=s, c0=c0, cn=cn: T.transpose(lgp.ap[0:cn, 0:NE], idxf[s].ap[:, c0:c0 + cn], ident.ap[0:NE, 0:NE])],
                                    reads=[idxf[s], ident], writes=[lgp])
                            S.op("dve", lambda s=s, cn=cn, slot=slot: V.tensor_copy(idxT[s].ap[0:cn, slot, :], lgp.ap[0:cn, 0:NE]),
                                 reads=[lgp], writes=[idxT[s]])
                            S.group("pe", [lambda s=s, c0=c0, cn=cn: T.transpose(smp.ap[0:cn, 0:NE], vals[s].ap[:, c0:c0 + cn], ident.ap[0:NE, 0:NE])],
                                    reads=[vals[s], ident], writes=[smp])
                            S.op("dve", lambda s=s, cn=cn, slot=slot: V.tensor_copy(valT[s].ap[0:cn, slot, :], smp.ap[0:cn, 0:NE]),
                                 reads=[smp], writes=[valT[s]])

                with Phase(nc, S, f"moeE{i}") as ph:
                    xg = ph.rot("xg", 4, [128, D], BF16)
                    xinT = ph.rot("xinT", 2, [128, KC, NCOL], BF16)
                    wgs = ph.rot("wgs", 3, [128, KC, 256], F32)
                    wus = ph.rot("wus", 3, [128, KC, 256], F32)
                    wgb = ph.rot("wgb", 2, [128, KC, 256], BF16)
                    wub = ph.rot("wub", 2, [128, KC, 256], BF16)
                    wds = ph.rot("wds", 2, [128, 2, D], F32)
                    wdb = ph.sb("wdb", [128, 16, D], BF16)
                    hidT = ph.sb("hidT", [128, 16, NCOL], BF16)
                    sg = ph.rot("sg", 2, [128, NCOL], F32)
                    yT = ph.sb("yT", [128, KC, NCOL], F32)
                    yo = ph.rot("yo", 4, [128, D], F32)
                    Gl = [ph.ps(f"Gl{k}", [128, 512]) for k in range(2)]
                    Ul = [ph.ps(f"Ul{k}", [128, 512]) for k in range(2)]
                    GUc = ph.ps("GUc", [128, 512])
                    Yl2 = [ph.ps(f"Yl{k}", [128, 512]) for k in range(2)]
                    Yc = Buf("Yc", GUc.ap[:, 128:256])
                    ptr = ph.ps("ptr", [128, 512])
                    ptr_bf = ptr.ap.bitcast(BF16)
                    nctx = 2 * CAPC if with_ctx else 0
                    gi = 0
                    wi = 0
                    di = 0
                    fi = 0
                    yi = 0
                    gl = []
                    for s in range(NSAMP):
                        for hf in range(2):
                            gl.append((s, hf, 128, s * CAPL + hf * 128, None, h2l_b[s]))
                    if with_ctx:
                        for s in range(NSAMP):
                            gl.append((s, 2, CAPC, 2 * CAPL + s * CAPC, None, h2c_b[s]))
                    gctr = [0]

                    def gather(e):
                        xt = xinT[e % 2]
                        for (s, slot, cn, col0, src, srcb) in gl:
                            g = xg[gctr[0] % 4]
                            gctr[0] += 1
                            S.idma(out=g.ap[0:cn, :], out_offset=None, in_=h2tok_t.rearrange("s t d -> (s t) d"),
                                   in_offset=bass.IndirectOffsetOnAxis(ap=idxT[s].ap[0:cn, slot, e:e + 1], axis=0),
                                   element_offset=(s * NT + (0 if slot < 2 else NL)) * D,
                                   reads=[idxT[s], srcb], writes=[g])
                            S.group("pe", [lambda kc=kc, g=g, cn=cn: T.transpose(
                                ptr_bf[:, kc * 128:kc * 128 + cn], g.ap[0:cn, kc * 128:(kc + 1) * 128], identb.ap[0:cn, 0:cn]) for kc in range(KC)],
                                reads=[g, identb], writes=[ptr])
                            S.op("dve", lambda xt=xt, col0=col0, cn=cn: V.tensor_copy(
                                xt.ap[:, :, col0:col0 + cn], ptr_bf.rearrange("p (k c) -> p k c", k=KC)[:, :, 0:cn]),
                                reads=[ptr], writes=[xt])

                    gather(0)
                    sc_cur = {}
                    sc_all = []
                    for e in range(NE):
                        xt = xinT[e % 2]
                        def prep(k):
                            e_, p_ = divmod(k, 8)
                            ws_g, ws_u, wb_g, wb_u = wgs[k % 3], wus[k % 3], wgb[k % 2], wub[k % 2]
                            S.dma("sp", ws_g.ap, moe_w_gate[i, e_][:, p_ * 256:(p_ + 1) * 256].rearrange("(k q) f -> q k f", q=128), writes=[ws_g])
                            S.dma("sp", ws_u.ap, moe_w_up[i, e_][:, p_ * 256:(p_ + 1) * 256].rearrange("(k q) f -> q k f", q=128), writes=[ws_u])
                            w = wds[k % 2]
                            S.dma("sp", w.ap, moe_w_down[i, e_, p_ * 256:(p_ + 1) * 256, :].rearrange("(c q) d -> q c d", q=128), writes=[w])
                            S.op("dve", lambda a=wb_g, b=ws_g: V.tensor_copy(a.ap, b.ap), reads=[ws_g], writes=[wb_g])
                            S.op("dve", lambda a=wb_u, b=ws_u: V.tensor_copy(a.ap, b.ap), reads=[ws_u], writes=[wb_u])
                            S.op("pool", lambda w=w, p_=p_: G.tensor_copy(wdb.ap[:, p_ * 2:(p_ + 1) * 2, :], w.ap), reads=[w], writes=[wdb])

                        if e == 0:
                            prep(0)
                        for p in range(8):
                            k = e * 8 + p
                            wb_g, wb_u = wgb[k % 2], wub[k % 2]
                            if k + 1 < NE * 8 and (p < 7):
                                prep(k + 1)
                            for fsub in range(2):
                                fc = p * 2 + fsub
                                gl_, ul_ = Gl[fi % 2], Ul[fi % 2]
                                sgt = sg[fi % 2]
                                fi += 1
                                fs = slice(fsub * 128, (fsub + 1) * 128)
                                S.group("pe", [lambda kc=kc, gl_=gl_, wb_g=wb_g, fs=fs, xt=xt: T.matmul(
                                    gl_.ap[:, 0:512], wb_g.ap[:, kc, fs], xt.ap[:, kc, 0:512], start=(kc == 0), stop=(kc == KC - 1))
                                    for kc in range(KC)], reads=[wb_g, xt], writes=[gl_])
                                S.group("pe", [lambda kc=kc, ul_=ul_, wb_u=wb_u, fs=fs, xt=xt: T.matmul(
                                    ul_.ap[:, 0:512], wb_u.ap[:, kc, fs], xt.ap[:, kc, 0:512], start=(kc == 0), stop=(kc == KC - 1))
                                    for kc in range(KC)], reads=[wb_u, xt], writes=[ul_])
                                if with_ctx:
                                    S.group("pe", [lambda kc=kc, wb_g=wb_g, fs=fs, xt=xt: T.matmul(
                                        GUc.ap[:, 0:64], wb_g.ap[:, kc, fs], xt.ap[:, kc, 512:576], start=(kc == 0), stop=(kc == KC - 1))
                                        for kc in range(KC)], reads=[wb_g, xt], writes=[GUc])
                                    S.group("pe", [lambda kc=kc, wb_u=wb_u, fs=fs, xt=xt: T.matmul(
                                        GUc.ap[:, 64:128], wb_u.ap[:, kc, fs], xt.ap[:, kc, 512:576], start=(kc == 0), stop=(kc == KC - 1))
                                        for kc in range(KC)], reads=[wb_u, xt], writes=[GUc])
                                S.op("act", lambda sgt=sgt, gl_=gl_: A.activation(sgt.ap[:, 0:512], gl_.ap, AF.Silu), reads=[gl_], writes=[sgt])
                                S.op("dve", lambda sgt=sgt, ul_=ul_, fc=fc: V.tensor_tensor(hidT.ap[:, fc, 0:512], sgt.ap[:, 0:512], ul_.ap, ALU.mult),
                                     reads=[sgt, ul_], writes=[hidT])
                                if with_ctx:
                                    S.op("act", lambda sgt=sgt: A.activation(sgt.ap[:, 512:576], GUc.ap[:, 0:64], AF.Silu), reads=[GUc], writes=[sgt])
                                    S.op("dve", lambda sgt=sgt, fc=fc: V.tensor_tensor(hidT.ap[:, fc, 512:576], sgt.ap[:, 512:576], GUc.ap[:, 64:128], ALU.mult),
                                         reads=[sgt, GUc], writes=[hidT])
                        if e + 1 < NE:
                            gather(e + 1)
                        for dc in range(KC):
                            ds_ = slice(dc * 128, (dc + 1) * 128)
                            Yl = Yl2[dc % 2]
                            S.group("pe", [lambda fc=fc, ds_=ds_, Yl=Yl: T.matmul(Yl.ap[:, 0:512], wdb.ap[:, fc, ds_], hidT.ap[:, fc, 0:512],
                                                                           start=(fc == 0), stop=(fc == 15)) for fc in range(16)],
                                    reads=[wdb, hidT], writes=[Yl])
                            S.op("act", lambda dc=dc, Yl=Yl: A.activation(yT.ap[:, dc, 0:512], Yl.ap, AF.Copy), reads=[Yl], writes=[yT])
                            if with_ctx:
                                S.group("pe", [lambda fc=fc, ds_=ds_: T.matmul(Yc.ap[:, 0:64], wdb.ap[:, fc, ds_], hidT.ap[:, fc, 512:576],
                                                                               start=(fc == 0), stop=(fc == 15)) for fc in range(16)],
                                        reads=[wdb, hidT], writes=[Yc])
                                S.op("act", lambda dc=dc: A.activation(yT.ap[:, dc, 512:576], Yc.ap[:, 0:64], AF.Copy), reads=[Yc], writes=[yT])
                        if e + 1 < NE:
                            prep((e + 1) * 8)
                        sc_prev, sc_cur = sc_cur, {}
                        for (s, slot, cn, col0, src, srcb) in gl:
                            y = yo[yi % 4]
                            yi += 1
                            for half in range(2):
                                S.group("pe", [lambda j=j, half=half, col0=col0, cn=cn: T.transpose(
                                    ptr.ap[0:cn, j * 128:(j + 1) * 128], yT.ap[:, half * 4 + j, col0:col0 + cn], ident.ap)
                                    for j in range(4)], reads=[yT, ident], writes=[ptr])
                                S.op("dve", lambda y=y, half=half, cn=cn, s=s, slot=slot, e=e: V.tensor_scalar(
                                    y.ap[0:cn, half * 512:(half + 1) * 512], ptr.ap[0:cn, :], valT[s].ap[0:cn, slot, e:e + 1], None, ALU.mult),
                                    reads=[ptr, valT[s]], writes=[y])
                            dstb = mal_b[s] if slot < 2 else mac_b[s]
                            for t_ in sc_prev.get(dstb.name, []):
                                S._wait("pool", t_)
                            S._wait("pool", dstb.writer)
                            tk_ = S.idma(out=macc_t.rearrange("s t d -> (s t) d"),
                                         out_offset=bass.IndirectOffsetOnAxis(ap=idxT[s].ap[0:cn, slot, e:e + 1], axis=0),
                                         in_=y.ap[0:cn, :], in_offset=None, compute_op=ALU.add,
                                         element_offset=(s * NT + (0 if slot < 2 else NL)) * D,
                                         reads=[y, idxT[s]], writes=[])
                            sc_cur.setdefault(dstb.name, []).append(tk_)
                            sc_all.append(tk_)

                    for t_ in sc_all[-12:]:
                        S._wait("pool", t_)
                    for s_ in range(NSAMP):
                        for b_ in (mal_b[s_], mac_b[s_]):
                            S.op("pool", lambda: G.memset(zeros.ap[:, 0:1], 0.0), reads=[], writes=[b_])
                with Phase(nc, S, f"moeU{i}") as ph:
                    xb = ph.rot("xb", 2, [128, KC, 512], F32)
                    mt = ph.rot("mt", 2, [128, D], F32)
                    pt = [ph.ps(f"pt{k}", [128, 512]) for k in range(2)]
                    it = 0
                    mi = 0
                    for s in range(NSAMP):
                        for (t0, n) in blocks:
                            v = s if t0 < NL else 2
                            x = xb[it % 2]
                            it += 1
                            S.dma("sp", x.ap[:, :, :n], xT_view(s, t0, n), reads=[xT_b[s]], writes=[x])
                            for tt in range(n // 128):
                                m = mt[mi % 2]
                                mi += 1
                                r0 = t0 + tt * 128
                                S.dma("act", m.ap, macc_t[s, r0:r0 + 128, :], reads=[mal_b[s] if r0 < NL else mac_b[s]], writes=[m])
                                for half in range(2):
                                    p = pt[half]
                                    S.group("pe", [lambda j=j, half=half, m=m, p=p: T.transpose(
                                        p.ap[:, j * 128:(j + 1) * 128], m.ap[:, (half * 4 + j) * 128:(half * 4 + j + 1) * 128], ident.ap)
                                        for j in range(4)], reads=[m, ident], writes=[p])
                                    for j in range(4):
                                        kc = half * 4 + j
                                        S.op("dve", lambda j=j, kc=kc, p=p, x=x, tt=tt, v=v: V.scalar_tensor_tensor(
                                            x.ap[:, kc, tt * 128:(tt + 1) * 128], p.ap[:, j * 128:(j + 1) * 128], mod.ap[:, 40 + kc, v:v + 1],
                                            x.ap[:, kc, tt * 128:(tt + 1) * 128], ALU.mult, ALU.add), reads=[p, mod, x], writes=[x])
                            S.dma("sp", xT_view(s, t0, n), x.ap[:, :, :n], reads=[x], writes=[xT_b[s]])

        def final():
            with Phase(nc, S, "fin") as ph:
                R = norm_resources(ph)
                fg = ph.sb("fg", [128, KC], F32)
                S.dma("sp", fg.ap, final_g.rearrange("(k p) -> p k", p=128), writes=[fg], allow_slow_non_contiguous=True)
                xn = ph.sb("xn", [128, KC, 512], F32)
                pt = [ph.ps(f"pt{k}", [128, 512]) for k in range(2)]
                ot = ph.rot("ot", 2, [128, D], F32)
                it = 0
                oi = 0
                for s in range(NSAMP):
                    for (t0, n) in BLOCKS[:4]:
                        xb = R["xb"][it % 2]
                        sq = R["sq"]
                        S.dma("sp" if it % 2 == 0 else "act", xb.ap, xT_view(s, t0, n), reads=[xT_b[s]], writes=[xb])
                        it += 1
                        S.op("act", lambda xb=xb: A.activation(sq.ap, xb.ap, AF.Square), reads=[xb], writes=[sq])
                        ssp = R["ssp"]
                        S.group("pe", [lambda kc=kc: T.matmul(ssp.ap, ones.ap, sq.ap[:, kc, :], start=(kc == 0), stop=(kc == KC - 1))
                                       for kc in range(KC)], reads=[ones, sq], writes=[ssp])
                        rstd = R["rstd"]
                        S.op("act", lambda: A.activation(rstd.ap, ssp.ap, AF.Sqrt, bias=R["eps"].ap, scale=1.0 / D), reads=[ssp, R["eps"]], writes=[rstd])
                        S.op("dve", lambda: V.reciprocal(rstd.ap, rstd.ap), reads=[rstd], writes=[rstd])
                        for kc in range(KC):
                            S.op("dve", lambda kc=kc, xb=xb: V.scalar_tensor_tensor(xn.ap[:, kc, :], xb.ap[:, kc, :], fg.ap[:, kc:kc + 1], rstd.ap,
                                                                                    ALU.mult, ALU.mult), reads=[xb, fg, rstd], writes=[xn])
                        for tt in range(4):
                            o = ot[oi % 2]
                            oi += 1
                            for half in range(2):
                                p = pt[half]
                                S.group("pe", [lambda j=j, half=half, tt=tt, p=p: T.transpose(
                                    p.ap[:, j * 128:(j + 1) * 128], xn.ap[:, half * 4 + j, tt * 128:(tt + 1) * 128], ident.ap) for j in range(4)],
                                    reads=[xn, ident], writes=[p])
                                if half == 0:
                                    S.op("dve", lambda o=o, p=p: V.tensor_copy(o.ap[:, 0:512], p.ap), reads=[p], writes=[o])
                                else:
                                    S.op("act", lambda o=o, p=p: A.activation(o.ap[:, 512:1024], p.ap, AF.Copy), reads=[p], writes=[o])
                            r0 = t0 + tt * 128
                            S.dma("sp" if oi % 2 == 0 else "act", out_d[s, r0:r0 + 128, :], o.ap, reads=[o], is_output=True)


        def attn(i):
            j = i // 2
            lam_init = 0.8 - 0.6 * math.exp(-0.3 * i)
            with Phase(nc, S, f"att{i}") as ph:
                PA = ph.ps("PA", [128, 512])
                PB = ph.ps("PB", [128, 512])
                Sp = [ph.ps(f"Sp{k}", [128, 512]) for k in range(2)]
                Op = [ph.ps(f"Op{k}", [128, 512]) for k in range(4)]
                R = norm_resources(ph, ssp=PA)
                hT = ph.sb("hT", [128, KC, NT], BF16)
                h32 = R["sq"]
                onT = ph.sb("onT", [128, 8, NT], BF16)
                ropec = ph.sb("ropec", [128, NL], F32)
                ropes = ph.sb("ropes", [128, NL], F32)
                S.dma("sp", ropec.ap, ropec_in, writes=[ropec])
                S.dma("act", ropes.ap, ropes_in, writes=[ropes])
                wo = ph.sb("wo", [128, 8, D], BF16)
                S.dma("pool", wo.ap, attn_w_o[j].rearrange("(k p) f -> p k f", p=128), writes=[wo])
                lt = ph.sb("lt", [64, 4], F32)
                for a in range(4):
                    S.dma("sp", lt.ap[:, a:a + 1], attn_lam[a][j].rearrange("(p o) -> p o", o=1), writes=[lt], allow_slow_non_contiguous=True)
                pr = ph.sb("pr", [64, 2], F32)
                S.op("dve", lambda: V.tensor_tensor(pr.ap[:, 0:1], lt.ap[:, 0:1], lt.ap[:, 1:2], ALU.mult), reads=[lt], writes=[pr])
                S.op("dve", lambda: V.tensor_tensor(pr.ap[:, 1:2], lt.ap[:, 2:3], lt.ap[:, 3:4], ALU.mult), reads=[lt], writes=[pr])
                S.group("pe", [lambda: T.matmul(PB.ap[:, 0:2], ones.ap[0:64, :], pr.ap, start=True, stop=True)], reads=[ones, pr], writes=[PB])
                ex = ph.sb("ex", [128, 2], F32)
                S.op("act", lambda: A.activation(ex.ap, PB.ap[:, 0:2], AF.Exp), reads=[PB], writes=[ex])
                neglam = ph.sb("neglam", [128, 1], F32)
                S.op("dve", lambda: V.tensor_tensor(neglam.ap, ex.ap[:, 1:2], ex.ap[:, 0:1], ALU.subtract), reads=[ex], writes=[neglam])
                S.op("dve", lambda: V.tensor_scalar(neglam.ap, neglam.ap, -lam_init, None, ALU.add), reads=[neglam], writes=[neglam])
                subg = ph.sb("subg", [128, 1], F32)
                S.dma("sp", subg.ap, attn_subln_g[j].rearrange("(p o) -> p o", o=1), writes=[subg], allow_slow_non_contiguous=True)
                S.op("dve", lambda: V.tensor_scalar(subg.ap, subg.ap, 1.0 - lam_init, None, ALU.mult), reads=[subg], writes=[subg])
                wq = ph.rot("wq", 2, [128, KC, 128], BF16)
                wk = ph.rot("wk", 2, [128, KC, 128], BF16)
                wv = ph.rot("wv", 2, [128, KC, 128], BF16)
                wqs = ph.rot("wqs", 2, [128, KC, 128], BF16)
                wks = ph.rot("wks", 2, [128, KC, 128], BF16)
                qT = ph.sb("qT", [128, NT], BF16)
                kT = ph.sb("kT", [128, NT], BF16)
                vx = ph.sb("vx", [128, NT // 128, 130], BF16)
                S.op("pool", lambda: G.memset(vx.ap, 1.0), writes=[vx])
                t1 = ph.rot("t1", 2, [128, 512], F32)
                t2 = ph.rot("t2", 2, [128, 512], F32)
                Et = ph.rot("Et", 2, [128, 512], BF16)
                otmp = ph.sb("otmp", [128, 4, 128], F32)
                osq = ph.sb("osq", [128, 128], F32)
                sm = ph.rot("sm", 4, [128, 4], F32)
                xb = R["xb"]
                wsrc = attn_w_qkv[j]

                def wview(base):
                    return wsrc[:, base:base + 128].rearrange("(k p) f -> p k f", p=128)

                def wview_sw(base, two):
                    return wsrc[:, base:base + 128].rearrange("(k p) (b two q) -> p k b two q", p=128, two=2, q=16)[:, :, :, two, :]

                it = 0
                hi = 0
                ei = 0
                for s in range(NSAMP):
                    for (t0, n) in BLOCKS:
                        norm_block(ph, R, s, t0, n, A1, 0, h32, hT.ap[:, :, t0:t0 + n], hT, it)
                        it += 1
                    for h in range(8):
                        Wq, Wk, Wv, Wqs, Wks = wq[hi % 2], wk[hi % 2], wv[hi % 2], wqs[hi % 2], wks[hi % 2]
                        hi += 1
                        S.dma("pool", Wq.ap, wview(h * 128), writes=[Wq])
                        S.dma("pool", Wk.ap, wview(D + h * 128), writes=[Wk])
                        S.dma("pool", Wv.ap, wview(2 * D + h * 128), writes=[Wv])
                        for two in range(2):
                            for (Wd_, Ws_) in ((Wqs, Wq), (Wks, Wk)):
                                S.op("pool", lambda Wd_=Wd_, Ws_=Ws_, two=two: G.tensor_copy(
                                    Wd_.ap.rearrange("p k (b two q) -> p k b two q", two=2, q=16)[:, :, :, two, :],
                                    Ws_.ap.rearrange("p k (b two q) -> p k b two q", two=2, q=16)[:, :, :, 1 - two, :]),
                                    reads=[Ws_], writes=[Wd_])
                        for (W, Wsw, dst) in ((Wq, Wqs, qT), (Wk, Wks, kT)):
                            for (t0, n) in BLOCKS:
                                S.group("pe", [lambda kc=kc, W=W, t0=t0, n=n: T.matmul(PA.ap[:, :n], W.ap[:, kc, :], hT.ap[:, kc, t0:t0 + n],
                                                                                        start=(kc == 0), stop=(kc == KC - 1)) for kc in range(KC)],
                                        reads=[W, hT], writes=[PA])
                                if t0 < NL:
                                    S.group("pe", [lambda kc=kc, Wsw=Wsw, t0=t0, n=n: T.matmul(PB.ap[:, :n], Wsw.ap[:, kc, :], hT.ap[:, kc, t0:t0 + n],
                                                                                                start=(kc == 0), stop=(kc == KC - 1)) for kc in range(KC)],
                                            reads=[Wsw, hT], writes=[PB])
                                    a1, a2 = t1[ei % 2], t2[ei % 2]
                                    ei += 1
                                    S.op("dve", lambda a1=a1, t0=t0, n=n: V.tensor_tensor(a1.ap[:, :n], PA.ap[:, :n], ropec.ap[:, t0:t0 + n], ALU.mult),
                                         reads=[PA, ropec], writes=[a1])
                                    S.op("dve", lambda a2=a2, t0=t0, n=n: V.tensor_tensor(a2.ap[:, :n], PB.ap[:, :n], ropes.ap[:, t0:t0 + n], ALU.mult),
                                         reads=[PB, ropes], writes=[a2])
                                    S.op("pool", lambda a1=a1, a2=a2, dst=dst, t0=t0, n=n: G.tensor_tensor(dst.ap[:, t0:t0 + n], a1.ap[:, :n], a2.ap[:, :n], ALU.add),
                                         reads=[a1, a2], writes=[dst])
                                else:
                                    S.op("act", lambda dst=dst, t0=t0, n=n: A.activation(dst.ap[:, t0:t0 + n], PA.ap[:, :n], AF.Copy), reads=[PA], writes=[dst])
                        for tt in range(NT // 128):
                            S.group("pe", [lambda kc=kc, tt=tt, Wv=Wv: T.matmul(PB.ap[:, 0:128], hT.ap[:, kc, tt * 128:(tt + 1) * 128], Wv.ap[:, kc, :],
                                                                                 start=(kc == 0), stop=(kc == KC - 1)) for kc in range(KC)],
                                    reads=[Wv, hT], writes=[PB])
                            S.op("act", lambda tt=tt: A.activation(vx.ap[:, tt, 0:128], PB.ap[:, 0:128], AF.Copy), reads=[PB], writes=[vx])
                        for (q0, qn, ktiles) in [(0, 512, list(range(18))), (512, 512, list(range(18))), (1024, 512, list(range(18))),
                                                 (1536, 512, list(range(18))), (2048, 256, [16, 17])]:
                            nq = qn // 128
                            for c in range(2):
                                cs = slice(c * 64, (c + 1) * 64)
                                for ki, kt in enumerate(ktiles):
                                    sp_, et_ = Sp[ei % 2], Et[ei % 2]
                                    ei += 1
                                    S.group("pe", [lambda sp_=sp_, kt=kt, cs=cs, q0=q0, qn=qn: T.matmul(
                                        sp_.ap[:, :qn], kT.ap[cs, kt * 128:(kt + 1) * 128], qT.ap[cs, q0:q0 + qn], start=True, stop=True)],
                                        reads=[kT, qT], writes=[sp_])
                                    S.op("act", lambda sp_=sp_, et_=et_, qn=qn: A.activation(et_.ap[:, :qn], sp_.ap[:, :qn], AF.Exp, scale=0.125),
                                         reads=[sp_], writes=[et_])
                                    for qs in range(nq):
                                        S.group("pe", [lambda qs=qs, et_=et_, kt=kt, ki=ki, ktiles=ktiles: T.matmul(
                                            Op[qs].ap[:, 0:129], et_.ap[:, qs * 128:(qs + 1) * 128], vx.ap[:, kt, 0:129],
                                            start=(ki == 0), stop=(ki == len(ktiles) - 1))], reads=[et_, vx], writes=[Op[qs]])
                                for qs in range(nq):
                                    m = sm[qs]
                                    S.op("dve", lambda m=m, qs=qs: V.reciprocal(m.ap[:, 0:1], Op[qs].ap[:, 128:129]), reads=[Op[qs]], writes=[m])
                                    if c == 0:
                                        S.op("dve", lambda m=m, qs=qs: V.tensor_scalar(otmp.ap[:, qs, :], Op[qs].ap[:, 0:128], m.ap[:, 0:1], None, ALU.mult),
                                             reads=[Op[qs], m], writes=[otmp])
                                    else:
                                        S.op("dve", lambda m=m: V.tensor_tensor(m.ap[:, 1:2], m.ap[:, 0:1], neglam.ap, ALU.mult), reads=[m, neglam], writes=[m])
                                        S.op("dve", lambda m=m, qs=qs: V.scalar_tensor_tensor(otmp.ap[:, qs, :], Op[qs].ap[:, 0:128], m.ap[:, 1:2], otmp.ap[:, qs, :],
                                                                                              ALU.mult, ALU.add), reads=[Op[qs], m, otmp], writes=[otmp])
                                        S.op("act", lambda m=m, qs=qs: A.activation(osq.ap, otmp.ap[:, qs, :], AF.Square, accum_out=m.ap[:, 2:3]),
                                             reads=[otmp], writes=[osq, m])
                                        S.op("act", lambda m=m: A.activation(m.ap[:, 3:4], m.ap[:, 2:3], AF.Sqrt, bias=R["eps"].ap, scale=1.0 / 128),
                                             reads=[m, R["eps"]], writes=[m])
                                        S.op("dve", lambda m=m: V.reciprocal(m.ap[:, 3:4], m.ap[:, 3:4]), reads=[m], writes=[m])
                                        S.op("dve", lambda m=m, qs=qs: V.tensor_scalar(otmp.ap[:, qs, :], otmp.ap[:, qs, :], m.ap[:, 3:4], None, ALU.mult),
                                             reads=[otmp, m], writes=[otmp])
                                        S.group("pe", [lambda qs=qs: T.transpose(PA.ap[:, qs * 128:(qs + 1) * 128], otmp.ap[:, qs, :], ident.ap)],
                                                reads=[otmp, ident], writes=[PA])
                                        S.op("dve", lambda qs=qs, h=h, q0=q0: V.tensor_scalar(onT.ap[:, h, q0 + qs * 128:q0 + (qs + 1) * 128],
                                                                                              PA.ap[:, qs * 128:(qs + 1) * 128], subg.ap[:, 0:1], None, ALU.mult),
                                             reads=[PA, subg], writes=[onT])
                    for (t0, n) in BLOCKS:
                        v = s if t0 < NL else 2
                        x = xb[it % 2]
                        it += 1
                        S.dma("sp", x.ap[:, :, :n], xT_view(s, t0, n), reads=[xT_b[s]], writes=[x])
                        for fc in range(KC):
                            pp = PA if fc % 2 == 0 else PB
                            S.group("pe", [lambda hh=hh, fc=fc, pp=pp, t0=t0, n=n: T.matmul(pp.ap[:, :n], wo.ap[:, hh, fc * 128:(fc + 1) * 128], onT.ap[:, hh, t0:t0 + n],
                                                                                             start=(hh == 0), stop=(hh == 7)) for hh in range(8)],
                                    reads=[wo, onT], writes=[pp])
                            S.op("dve", lambda fc=fc, pp=pp, x=x, n=n, v=v: V.scalar_tensor_tensor(x.ap[:, fc, :n], pp.ap[:, :n], mod.ap[:, 16 + fc, v:v + 1],
                                                                                                   x.ap[:, fc, :n], ALU.mult, ALU.add), reads=[pp, mod, x], writes=[x])
                        S.dma("act", xT_view(s, t0, n), x.ap[:, :, :n], reads=[x], writes=[xT_b[s]])


        def s5(i):
            j = i // 2
            with_ctx = i < DEPTH - 1
            FB = [(0, 256), (256, 512), (768, 512), (1280, 512), (1792, 512)]
            TWO_PI = 2.0 * math.pi
            PIC = 3.1415925
            with Phase(nc, S, f"s5o{i}") as pho:
                R = norm_resources(pho)
                hF = pho.sb("hF", [128, KC, NT], BF16)
                PSre = pho.ps("PSre", [128, 512])
                PSim = pho.ps("PSim", [128, 512])
                PSy = pho.ps("PSy", [128, 512])
                PT = pho.ps("PT", [128, 512])
                PSre2 = pho.ps("PSre2", [128, 512])
                PSim2 = pho.ps("PSim2", [128, 512])
                PSy2 = pho.ps("PSy2", [128, 512])

                def P(name, shape=(128, 64), dt=F32):
                    return pho.sb(name, list(shape), dt)

                lre, lim, ldt, mag, ang, red, red2, sn, cs = [P(n) for n in ("lre", "lim", "ldt", "mag", "ang", "red", "red2", "sn", "cs")]
                abre, abim, nr, den, cre, cim, ncim, tq = [P(n) for n in ("abre", "abim", "nr", "den", "cre", "cim", "ncim", "tq")]
                ki = P("ki", dt=I32)
                rc = P("rc", (128, 12, 64))
                rsn = P("rsn", (128, 12, 64))
                nrs = P("nrs", (128, 12, 64))
                dsk = P("dsk", (128, KC))
                for (dst, src) in ((lre, ssm_lam_re), (lim, ssm_lam_im)):
                    for d in range(2):
                        S.dma("sp", dst.ap[:, d * 32:(d + 1) * 32], src[j, d].rearrange("g n -> (g n)").rearrange("(gp p) -> p gp", p=128),
                              writes=[dst], allow_slow_non_contiguous=True)
                S.dma("act", dsk.ap, ssm_d[j].rearrange("(k p) -> p k", p=128), writes=[dsk], allow_slow_non_contiguous=True)
                ldrow = P("ldrow", (1, 128))
                S.dma("sp", ldrow.ap, ssm_log_dt[j].rearrange("d g -> (d g)").rearrange("(o f) -> o f", o=1), writes=[ldrow])
                S.group("pe", [lambda: T.matmul(PT.ap[:, 0:128], ones.ap[0:1, :], ldrow.ap, start=True, stop=True)], reads=[ones, ldrow], writes=[PT])
                for g2 in range(2):
                    S.op("dve", lambda g2=g2: V.tensor_copy(
                        ldt.ap[g2 * 64:(g2 + 1) * 64, :].rearrange("p (d g) -> p d g", d=2),
                        PT.ap[g2 * 64:(g2 + 1) * 64, 0:128].rearrange("p (d g two) -> p d g two", d=2, two=2)[:, :, :, g2]),
                        reads=[PT], writes=[ldt])
                S.op("act", lambda: A.activation(ldt.ap, ldt.ap, AF.Exp), reads=[ldt], writes=[ldt])
                S.op("dve", lambda: V.tensor_tensor(mag.ap, lre.ap, ldt.ap, ALU.mult), reads=[lre, ldt], writes=[mag])
                S.op("act", lambda: A.activation(mag.ap, mag.ap, AF.Exp), reads=[mag], writes=[mag])
                S.op("dve", lambda: V.tensor_tensor(ang.ap, lim.ap, ldt.ap, ALU.mult), reads=[lim, ldt], writes=[ang])
                S.op("dve", lambda: V.tensor_scalar(ki.ap, ang.ap, 1.0 / TWO_PI, None, ALU.mult), reads=[ang], writes=[ki])
                S.op("dve", lambda: V.tensor_copy(tq.ap, ki.ap), reads=[ki], writes=[tq])
                S.op("dve", lambda: V.scalar_tensor_tensor(red.ap, tq.ap, -TWO_PI, ang.ap, ALU.mult, ALU.add), reads=[tq, ang], writes=[red])
                S.op("dve", lambda: V.tensor_scalar(red.ap, red.ap, PIC, -PIC, ALU.min, ALU.max), reads=[red], writes=[red])
                S.op("act", lambda: A.activation(sn.ap, red.ap, AF.Sin), reads=[red], writes=[sn])
                S.op("dve", lambda: V.tensor_scalar(tq.ap, red.ap, math.pi / 2, -TWO_PI, ALU.is_gt, ALU.mult), reads=[red], writes=[tq])
                S.op("dve", lambda: V.scalar_tensor_tensor(red2.ap, red.ap, math.pi / 2, tq.ap, ALU.add, ALU.add), reads=[red, tq], writes=[red2])
                S.op("dve", lambda: V.tensor_scalar(red2.ap, red2.ap, PIC, -PIC, ALU.min, ALU.max), reads=[red2], writes=[red2])
                S.op("act", lambda: A.activation(cs.ap, red2.ap, AF.Sin), reads=[red2], writes=[cs])
                S.op("dve", lambda: V.tensor_tensor(abre.ap, mag.ap, cs.ap, ALU.mult), reads=[mag, cs], writes=[abre])
                S.op("dve", lambda: V.tensor_tensor(abim.ap, mag.ap, sn.ap, ALU.mult), reads=[mag, sn], writes=[abim])
                S.op("dve", lambda: V.tensor_scalar(nr.ap, abre.ap, -1.0, None, ALU.add), reads=[abre], writes=[nr])
                S.op("dve", lambda: V.tensor_tensor(den.ap, lre.ap, lre.ap, ALU.mult), reads=[lre], writes=[den])
                S.op("dve", lambda: V.tensor_tensor(tq.ap, lim.ap, lim.ap, ALU.mult), reads=[lim], writes=[tq])
                S.op("dve", lambda: V.tensor_tensor(den.ap, den.ap, tq.ap, ALU.add), reads=[den, tq], writes=[den])
                S.op("dve", lambda: V.reciprocal(den.ap, den.ap), reads=[den], writes=[den])
                S.op("dve", lambda: V.tensor_tensor(cre.ap, nr.ap, lre.ap, ALU.mult), reads=[nr, lre], writes=[cre])
                S.op("dve", lambda: V.tensor_tensor(tq.ap, abim.ap, lim.ap, ALU.mult), reads=[abim, lim], writes=[tq])
                S.op("dve", lambda: V.tensor_tensor(cre.ap, cre.ap, tq.ap, ALU.add), reads=[cre, tq], writes=[cre])
                S.op("dve", lambda: V.tensor_tensor(cre.ap, cre.ap, den.ap, ALU.mult), reads=[cre, den], writes=[cre])
                S.op("dve", lambda: V.tensor_tensor(cim.ap, abim.ap, lre.ap, ALU.mult), reads=[abim, lre], writes=[cim])
                S.op("dve", lambda: V.tensor_tensor(tq.ap, nr.ap, lim.ap, ALU.mult), reads=[nr, lim], writes=[tq])
                S.op("dve", lambda: V.tensor_tensor(cim.ap, cim.ap, tq.ap, ALU.subtract), reads=[cim, tq], writes=[cim])
                S.op("dve", lambda: V.tensor_tensor(cim.ap, cim.ap, den.ap, ALU.mult), reads=[cim, den], writes=[cim])
                S.op("dve", lambda: V.tensor_scalar(ncim.ap, cim.ap, -1.0, None, ALU.mult), reads=[cim], writes=[ncim])
                S.op("dve", lambda: V.tensor_copy(rc.ap[:, 0, :], cs.ap), reads=[cs], writes=[rc])
                S.op("dve", lambda: V.tensor_copy(rsn.ap[:, 0, :], sn.ap), reads=[sn], writes=[rsn])
                for k in range(1, 12):
                    S.op("dve", lambda k=k: V.tensor_tensor(tq.ap, rsn.ap[:, k - 1, :], rsn.ap[:, k - 1, :], ALU.mult), reads=[rsn], writes=[tq])
                    S.op("dve", lambda k=k: V.tensor_tensor(rc.ap[:, k, :], rc.ap[:, k - 1, :], rc.ap[:, k - 1, :], ALU.mult), reads=[rc], writes=[rc])
                    S.op("dve", lambda k=k: V.tensor_tensor(rc.ap[:, k, :], rc.ap[:, k, :], tq.ap, ALU.subtract), reads=[rc, tq], writes=[rc])
                    S.op("dve", lambda k=k: V.scalar_tensor_tensor(rsn.ap[:, k, :], rc.ap[:, k - 1, :], 2.0, rsn.ap[:, k - 1, :], ALU.mult, ALU.mult),
                         reads=[rc, rsn], writes=[rsn])
                S.op("dve", lambda: V.tensor_scalar(nrs.ap, rsn.ap, -1.0, None, ALU.mult), reads=[rsn], writes=[nrs])

                bsrc = [pho.sb(f"bsrc{k}", [128, 64, 16], F32) for k in range(2)]
                csrc = [pho.sb(f"csrc{k}", [128, 64, 16], F32) for k in range(2)]
                for k, (bs_, cs_) in enumerate(((ssm_b_re, ssm_c_re), (ssm_b_im, ssm_c_im))):
                    for d in range(2):
                        S.dma("sp", bsrc[k].ap[:, d * 32:(d + 1) * 32, :],
                              bs_[j, d].rearrange("g n q -> (g n) q").rearrange("(gp p) q -> p gp q", p=128), writes=[bsrc[k]])
                        for g2 in range(2):
                            for gp_ in range(32):
                                S.dma("act" if g2 else "sp", csrc[k].ap[g2 * 64:(g2 + 1) * 64, d * 32 + gp_, :],
                                      cs_[j, d, gp_ * 2 + g2].rearrange("p n -> n p"), writes=[csrc[k]],
                                      allow_slow_non_contiguous=True)

                wB = [Buf(f"s5w{c}", s5w_t[c]) for c in range(64)]
                tB = [Buf(f"s5t{c}", s5tab_t[c]) for c in range(64)]
                with Phase(nc, S, f"s5p{i}") as ph:
                    pad_all = [[ph.sb(f"pad{k}_{r}", [128, 128], F32) for r in range(2)] for k in range(4)]
                    for k in range(4):
                        for r in range(2):
                            S.op("pool", lambda k=k, r=r: G.memset(pad_all[k][r].ap, 0.0), writes=[pad_all[k][r]])
                    bbt_all = ph.rot("bbt", 2, [128, 2, 16], F32)
                    W5_all = ph.rot("W5", 2, [128, 5, 128], BF16)
                    CS_all = ph.rot("CS", 2, [128, 2, 512], F32)
                    tta_all = ph.rot("tta", 2, [128, 256], F32)
                    ttb_all = ph.rot("ttb", 2, [128, 256], F32)
                    pi_ = 0
                    for d in range(2):
                        for gp in range(32):
                            gpl = gp % 4
                            col = d * 32 + gp
                            cc = slice(col, col + 1)
                            r_ = pi_ % 2
                            pi_ += 1
                            bbt, W5, CS, tta, ttb = bbt_all[r_], W5_all[r_], CS_all[r_], tta_all[r_], ttb_all[r_]
                            S.op("pool", lambda W5=W5: G.memset(W5.ap[:, 2:5, :], 0.0), writes=[W5])
                            for k in range(2):
                                pd = pad_all[gpl][k]
                                a_, b_ = (bsrc[0], bsrc[1]) if k == 0 else (bsrc[1], bsrc[0])
                                sc2 = ncim if k == 0 else cim
                                S.op("pool", lambda a_=a_, k=k, col=col, cc=cc, bbt=bbt: G.tensor_scalar(bbt.ap[:, k, :], a_.ap[:, col, :], cre.ap[:, cc], None, ALU.mult),
                                     reads=[a_, cre], writes=[bbt])
                                S.op("dve", lambda b_=b_, k=k, col=col, cc=cc, sc2=sc2, bbt=bbt: V.scalar_tensor_tensor(
                                    bbt.ap[:, k, :], b_.ap[:, col, :], sc2.ap[:, cc], bbt.ap[:, k, :], ALU.mult, ALU.add), reads=[b_, sc2, bbt], writes=[bbt])
                                for g2 in range(2):
                                    blk = (gpl * 2 + g2) * 16
                                    S.op("pool", lambda g2=g2, blk=blk, k=k, pd=pd, bbt=bbt: G.tensor_copy(pd.ap[g2 * 64:(g2 + 1) * 64, blk:blk + 16],
                                                                                                  bbt.ap[g2 * 64:(g2 + 1) * 64, k, :]), reads=[bbt], writes=[pd])
                                S.group("pe", [lambda pd=pd: T.transpose(PT.ap[:, 0:128], pd.ap, ident.ap)], reads=[pd, ident], writes=[PT])
                                S.op("act", lambda k=k, W5=W5: A.activation(W5.ap[:, k, :], PT.ap[:, 0:128], AF.Copy), reads=[PT], writes=[W5])
                            for g2 in range(2):
                                blk = (gpl * 2 + g2) * 16
                                for (kk, src_k, sgn) in ((2, 0, 1.0), (3, 0, -1.0), (4, 1, -1.0)):
                                    S.op("act", lambda g2=g2, blk=blk, kk=kk, src_k=src_k, sgn=sgn, col=col, W5=W5: A.activation(
                                        W5.ap[g2 * 64:(g2 + 1) * 64, kk, blk:blk + 16], csrc[src_k].ap[g2 * 64:(g2 + 1) * 64, col, :],
                                        AF.Copy, scale=sgn), reads=[csrc[src_k]], writes=[W5])
                            Ct_ap, St_ap = CS.ap[:, 0, :], CS.ap[:, 1, :]
                            S.op("pool", lambda Ct_ap=Ct_ap: G.memset(Ct_ap[:, 0:1], 1.0), writes=[CS])
                            S.op("pool", lambda St_ap=St_ap: G.memset(St_ap[:, 0:1], 0.0), writes=[CS])
                            for k in range(9):
                                L = 1 << k
                                ck, sk, nsk = rc.ap[:, k, cc], rsn.ap[:, k, cc], nrs.ap[:, k, cc]
                                S.op("act", lambda L=L, nsk=nsk, tta=tta, St_ap=St_ap: A.activation(tta.ap[:, 0:L], St_ap[:, 0:L], AF.Copy, scale=nsk), reads=[CS, nrs], writes=[tta])
                                S.op("act", lambda L=L, sk=sk, ttb=ttb, Ct_ap=Ct_ap: A.activation(ttb.ap[:, 0:L], Ct_ap[:, 0:L], AF.Copy, scale=sk), reads=[CS, rsn], writes=[ttb])
                                S.op("dve", lambda L=L, ck=ck, tta=tta, Ct_ap=Ct_ap: V.scalar_tensor_tensor(Ct_ap[:, L:2 * L], Ct_ap[:, 0:L], ck, tta.ap[:, 0:L], ALU.mult, ALU.add),
                                     reads=[CS, rc, tta], writes=[CS])
                                S.op("dve", lambda L=L, ck=ck, ttb=ttb, St_ap=St_ap: V.scalar_tensor_tensor(St_ap[:, L:2 * L], St_ap[:, 0:L], ck, ttb.ap[:, 0:L], ALU.mult, ALU.add),
                                     reads=[CS, rc, ttb], writes=[CS])
                            S.dma("sp", s5w_t[col].rearrange("k p f -> p k f"), W5.ap, reads=[W5], writes=[wB[col]])
                            S.dma("act", s5tab_t[col].rearrange("k p f -> p k f"), CS.ap, reads=[CS], writes=[tB[col]])

                for s in range(NSAMP):
                    it = 0
                    for (t0, n) in BLOCKS:
                        pos = t0 + NCX if t0 < NL else 0
                        norm_block(pho, R, s, t0, n, A1, 0, R["sq"], hF.ap[:, :, pos:pos + n], hF, it)
                        it += 1
                    with Phase(nc, S, f"s5s{i}_{s}") as ph:
                        hBk = ph.sb("hBk", [128, NT], BF16)
                        W5_all = ph.rot("W5", 3, [128, 5, 128], BF16)
                        CS_all = ph.rot("CS", 3, [128, 2, 512], F32)
                        PSre_all = [PSre, PSre2]
                        PSim_all = [PSim, PSim2]
                        PSy_all = [PSy, PSy2]
                        wre = ph.rot("wre", 4, [128, 512], F32)
                        wim = ph.rot("wim", 4, [128, 512], F32)
                        prod = [ph.rot(f"prod{k}", 2, [128, 512], BF16) for k in range(4)]
                        ini = ph.rot("ini", 2, [128, 4], F32)
                        yacc = ph.sb("yacc", [128, NT], F32)
                        ga = ph.sb("ga", [128, NT], F32)
                        tm_all = [[ph.sb(f"tm{k}_{r}", [128, 512], F32) for k in range(4)] for r in range(2)]
                        bi = 0
                        pi_ = 0
                        for kc in range(KC):
                            S.op("act", lambda kc=kc: A.activation(hBk.ap[:, 0:NCX], hF.ap[:, kc, 0:NCX][:, ::-1], AF.Copy), reads=[hF], writes=[hBk])
                            S.op("act", lambda kc=kc: A.activation(hBk.ap[:, NCX:NT], hF.ap[:, kc, NCX:NT][:, ::-1], AF.Copy), reads=[hF], writes=[hBk])
                            S.op("pool", lambda: G.memset(yacc.ap, 0.0), writes=[yacc])
                            loads = []
                            tasks = []
                            for d in range(2):
                                for gpl in range(4):
                                    gp = kc * 4 + gpl
                                    col = d * 32 + gp
                                    cc = slice(col, col + 1)
                                    W5, CS = W5_all[pi_ % 3], CS_all[pi_ % 3]
                                    pi_ += 1

                                    def load(W5=W5, CS=CS, col=col):
                                        S.dma("sp", W5.ap, s5w_t[col].rearrange("k p f -> p k f"), reads=[wB[col]], writes=[W5])
                                        S.dma("act", CS.ap, s5tab_t[col].rearrange("k p f -> p k f"), reads=[tB[col]], writes=[CS])
                                    loads.append(load)
                                    prev = None
                                    for (p0, n) in FB:
                                        src = hF.ap[:, kc, p0:p0 + n] if d == 0 else hBk.ap[:, p0:p0 + n]
                                        sb_ = hF if d == 0 else hBk
                                        wr_, wi_ = wre[bi % 4], wim[bi % 4]
                                        pr_ = [prod[k][bi % 2] for k in range(4)]
                                        in_ = ini[bi % 2]
                                        tm = tm_all[bi % 2]
                                        Pre, Pim, Py = PSre_all[bi % 2], PSim_all[bi % 2], PSy_all[bi % 2]
                                        bi += 1
                                        cb, sb2 = CS.ap[:, 0, 0:n], CS.ap[:, 1, 0:n]

                                        def stA(src=src, sb_=sb_, n=n, W5=W5, CS=CS, Pre=Pre, Pim=Pim, tm=tm, wr_=wr_, wi_=wi_, cb=cb, sb2=sb2):
                                            S.group("pe", [lambda: T.matmul(Pre.ap[:, :n], W5.ap[:, 0, :], src, start=True, stop=True)], reads=[W5, sb_], writes=[Pre])
                                            S.group("pe", [lambda: T.matmul(Pim.ap[:, :n], W5.ap[:, 1, :], src, start=True, stop=True)], reads=[W5, sb_], writes=[Pim])
                                            S.op("dve", lambda: V.tensor_tensor(tm[0].ap[:, :n], Pre.ap[:, :n], cb, ALU.mult), reads=[Pre, CS], writes=[tm[0]])
                                            S.op("dve", lambda: V.tensor_tensor(tm[1].ap[:, :n], Pim.ap[:, :n], sb2, ALU.mult), reads=[Pim, CS], writes=[tm[1]])
                                            S.op("pool", lambda: G.tensor_tensor(wr_.ap[:, :n], tm[0].ap[:, :n], tm[1].ap[:, :n], ALU.add), reads=[tm[0], tm[1]], writes=[wr_])
                                            S.op("dve", lambda: V.tensor_tensor(tm[2].ap[:, :n], Pim.ap[:, :n], cb, ALU.mult), reads=[Pim, CS], writes=[tm[2]])
                                            S.op("dve", lambda: V.tensor_tensor(tm[3].ap[:, :n], Pre.ap[:, :n], sb2, ALU.mult), reads=[Pre, CS], writes=[tm[3]])
                                            S.op("pool", lambda: G.tensor_tensor(wi_.ap[:, :n], tm[2].ap[:, :n], tm[3].ap[:, :n], ALU.subtract), reads=[tm[2], tm[3]], writes=[wi_])

                                        def stB(prev=prev, in_=in_, wr_=wr_, wi_=wi_, n=n, cc=cc):
                                            if prev is not None:
                                                (pw_r, pw_i, pn) = prev
                                                lvl = 8 if pn == 256 else 9
                                                cl, sl, nsl = rc.ap[:, lvl, cc], rsn.ap[:, lvl, cc], nrs.ap[:, lvl, cc]
                                                er, ei_ = pw_r.ap[:, pn - 1:pn], pw_i.ap[:, pn - 1:pn]
                                                S.op("act", lambda: A.activation(in_.ap[:, 2:3], ei_, AF.Copy, scale=nsl), reads=[pw_i, nrs], writes=[in_])
                                                S.op("act", lambda: A.activation(in_.ap[:, 3:4], er, AF.Copy, scale=sl), reads=[pw_r, rsn], writes=[in_])
                                                S.op("act", lambda: A.activation(in_.ap[:, 0:1], er, AF.Identity, scale=cl, bias=in_.ap[:, 2:3]),
                                                     reads=[pw_r, rc, in_], writes=[in_])
                                                S.op("act", lambda: A.activation(in_.ap[:, 1:2], ei_, AF.Identity, scale=cl, bias=in_.ap[:, 3:4]),
                                                     reads=[pw_i, rc, in_], writes=[in_])
                                                i_re, i_im = in_.ap[:, 0:1], in_.ap[:, 1:2]
                                                rd = [in_]
                                            else:
                                                i_re, i_im = 0.0, 0.0
                                                rd = []
                                            S.op("dve", lambda: V.tensor_tensor_scan(
                                                wr_.ap[:, :n], mag.ap[:, cc].to_broadcast([128, n]), wr_.ap[:, :n], i_re, ALU.mult, ALU.add), reads=[wr_, mag] + rd, writes=[wr_])
                                            S.op("dve", lambda: V.tensor_tensor_scan(
                                                wi_.ap[:, :n], mag.ap[:, cc].to_broadcast([128, n]), wi_.ap[:, :n], i_im, ALU.mult, ALU.add), reads=[wi_, mag] + rd, writes=[wi_])

                                        def stC(n=n, p0=p0, d=d, W5=W5, CS=CS, wr_=wr_, wi_=wi_, pr_=pr_, Py=Py, cb=cb, sb2=sb2):
                                            S.op("dve", lambda: V.tensor_tensor(pr_[0].ap[:, :n], wr_.ap[:, :n], cb, ALU.mult), reads=[wr_, CS], writes=[pr_[0]])
                                            S.op("pool", lambda: G.tensor_tensor(pr_[1].ap[:, :n], wi_.ap[:, :n], sb2, ALU.mult), reads=[wi_, CS], writes=[pr_[1]])
                                            S.op("pool", lambda: G.tensor_tensor(pr_[2].ap[:, :n], wr_.ap[:, :n], sb2, ALU.mult), reads=[wr_, CS], writes=[pr_[2]])
                                            S.op("dve", lambda: V.tensor_tensor(pr_[3].ap[:, :n], wi_.ap[:, :n], cb, ALU.mult), reads=[wi_, CS], writes=[pr_[3]])
                                            lts = [2, 3, 4, 4]
                                            S.group("pe", [lambda q=q: T.matmul(Py.ap[:, :n], W5.ap[:, lts[q], :], pr_[q].ap[:, :n], start=(q == 0), stop=(q == 3))
                                                           for q in range(4)], reads=[W5] + pr_, writes=[Py])

                                        def stD(n=n, p0=p0, d=d, Py=Py):
                                            if d == 0:
                                                ya = yacc.ap[:, p0:p0 + n]
                                            elif p0 == 0:
                                                ya = yacc.ap[:, 0:NCX][:, ::-1]
                                            else:
                                                hi_ = NT - (p0 - NCX)
                                                ya = yacc.ap[:, hi_ - n:hi_][:, ::-1]
                                            S.op("dve", lambda: V.tensor_tensor(ya, ya, Py.ap[:, :n], ALU.add), reads=[Py, yacc], writes=[yacc])

                                        tasks.append((stA, stB, stC, len(loads) - 1 if p0 == 0 else None, stD))
                                        prev = (wr_, wi_, n)
                            loads[0]()
                            loads[1]()
                            tasks[0][0]()
                            tasks[1][0]()
                            for ti_, (stA, stB, stC, li, stD) in enumerate(tasks):
                                if li is not None and li + 2 < len(loads):
                                    loads[li + 2]()
                                stB()
                                if ti_ + 2 < len(tasks):
                                    tasks[ti_ + 2][0]()
                                if ti_ >= 1:
                                    tasks[ti_ - 1][4]()
                                stC()
                            tasks[-1][4]()
                            S.op("dve", lambda kc=kc: V.scalar_tensor_tensor(yacc.ap, hF.ap[:, kc, :], dsk.ap[:, kc:kc + 1], yacc.ap, ALU.mult, ALU.add),
                                 reads=[hF, dsk, yacc], writes=[yacc])
                            S.op("act", lambda: A.activation(ga.ap, yacc.ap, AF.Square), reads=[yacc], writes=[ga])
                            S.op("pool", lambda: G.tensor_scalar(ga.ap, ga.ap, 0.044715, 1.0, ALU.mult, ALU.add), reads=[ga], writes=[ga])
                            S.op("pool", lambda: G.tensor_tensor(ga.ap, ga.ap, yacc.ap, ALU.mult), reads=[ga, yacc], writes=[ga])
                            S.op("act", lambda: A.activation(ga.ap, ga.ap, AF.Tanh, scale=0.7978845608028654), reads=[ga], writes=[ga])
                            S.op("dve", lambda: V.scalar_tensor_tensor(ga.ap, ga.ap, 1.0, yacc.ap, ALU.add, ALU.mult), reads=[ga, yacc], writes=[ga])
                            S.op("act", lambda kc=kc: A.activation(hF.ap[:, kc, :], ga.ap, AF.Copy, scale=0.5), reads=[ga], writes=[hF])
                    with Phase(nc, S, f"s5g{i}_{s}") as ph:
                        w1 = ph.sb("w1", [128, KC, D], BF16)
                        w2 = ph.sb("w2", [128, KC, D], BF16)
                        S.dma("pool", w1.ap, ssm_w_glu1[j].rearrange("(k p) f -> p k f", p=128), writes=[w1])
                        S.dma("pool", w2.ap, ssm_w_glu2[j].rearrange("(k p) f -> p k f", p=128), writes=[w2])
                        sg_ = ph.rot("sg", 2, [128, 512], F32)
                        xb = R["xb"]
                        for bi, (p0, n) in enumerate(FB):
                            if p0 == 0 and not with_ctx:
                                continue
                            t0 = NL if p0 == 0 else p0 - NCX
                            v = 2 if p0 == 0 else s
                            x = xb[bi % 2]
                            S.dma("sp", x.ap[:, :, :n], xT_view(s, t0, n), reads=[xT_b[s]], writes=[x])
                            for fc in range(KC):
                                fs = slice(fc * 128, (fc + 1) * 128)
                                S.group("pe", [lambda kc=kc, fs=fs, p0=p0, n=n: T.matmul(PSre.ap[:, :n], w1.ap[:, kc, fs], hF.ap[:, kc, p0:p0 + n],
                                                                                         start=(kc == 0), stop=(kc == KC - 1)) for kc in range(KC)], reads=[w1, hF], writes=[PSre])
                                S.group("pe", [lambda kc=kc, fs=fs, p0=p0, n=n: T.matmul(PSim.ap[:, :n], w2.ap[:, kc, fs], hF.ap[:, kc, p0:p0 + n],
                                                                                         start=(kc == 0), stop=(kc == KC - 1)) for kc in range(KC)], reads=[w2, hF], writes=[PSim])
                                g_ = sg_[fc % 2]
                                S.op("act", lambda g_=g_, n=n: A.activation(g_.ap[:, :n], PSim.ap[:, :n], AF.Sigmoid), reads=[PSim], writes=[g_])
                                S.op("dve", lambda g_=g_, n=n: V.tensor_tensor(g_.ap[:, :n], g_.ap[:, :n], PSre.ap[:, :n], ALU.mult), reads=[g_, PSre], writes=[g_])
                                S.op("dve", lambda g_=g_, n=n, fc=fc, x=x, v=v: V.scalar_tensor_tensor(x.ap[:, fc, :n], g_.ap[:, :n], mod.ap[:, 16 + fc, v:v + 1], x.ap[:, fc, :n],
                                                                                                       ALU.mult, ALU.add), reads=[g_, mod, x], writes=[x])
                            S.dma("act", xT_view(s, t0, n), x.ap[:, :, :n], reads=[x], writes=[xT_b[s]])

        MIXERS = {0: attn, 1: s5}
        cur = -1
        for (i, part) in cfg:
            if i != cur:
                adaln(i)
                cur = i
            if part == "mix":
                MIXERS[i % 2](i)
            else:
                moe_full(i)
        final()
        S.finish()
    return nc, S


_CONST = {}


def _consts():
    if not _CONST:
        _CONST["k_ident"] = np.eye(128, dtype=np.float32)
        t = np.arange(NL)
        row = (t // 64).astype(np.float32)
        col = (t % 64).astype(np.float32)
        inv = (10000.0 ** (-np.arange(16, dtype=np.float32) / 16)).astype(np.float32)
        C = np.zeros((128, NL), np.float32)
        Sg = np.zeros((128, NL), np.float32)
        for p in range(128):
            dd = p % 64
            pos = row if dd < 32 else col
            ang = pos * inv[dd % 16]
            C[p] = np.cos(ang)
            Sg[p] = np.sin(ang) * (-1.0 if (dd % 32) < 16 else 1.0)
        _CONST["k_ropec"] = C
        _CONST["k_ropes"] = Sg
    return _CONST


_NC_CACHE = {}


def kernel(**inputs):
    n = 8
    if "nc" not in _NC_CACHE:
        _NC_CACHE["nc"] = build()[0]
    nc = _NC_CACHE["nc"]
    shared = {k: np.ascontiguousarray(v) for k, v in inputs.items() if k not in ("x", "c", "ctx")}
    shared.update(_consts())
    in_maps = []
    for r in range(n):
        m = dict(shared)
        m["x"] = np.ascontiguousarray(inputs["x"][2 * r:2 * r + 2])
        m["c"] = np.ascontiguousarray(inputs["c"][2 * r:2 * r + 2])
        m["ctx"] = np.ascontiguousarray(inputs["ctx"][2 * r:2 * r + 2])
        in_maps.append(m)
    res = run_bass_kernel_spmd(nc, in_maps, core_ids=list(range(n)))
    return np.concatenate([r["out"] for r in res.results], axis=0).astype(np.float32)
```

```python
import math
from contextlib import ExitStack
import numpy as np
import ml_dtypes
import concourse.bass as bass
import concourse.mybir as mybir
from concourse.bass_utils import run_bass_kernel_spmd

F32 = mybir.dt.float32
BF16 = mybir.dt.bfloat16
U32 = mybir.dt.uint32
I32 = mybir.dt.int32
AF = mybir.ActivationFunctionType
ALU = mybir.AluOpType

D = 1024
NL = 2048
NCX = 256
NT = NL + NCX
KC = 8
NE = 16
FF = 2048
DEPTH = 4
EPS = 1e-6
NSAMP = 2
CAPL = 256
CAPC = 32
NCOL = 2 * CAPL + 2 * CAPC


class Buf:
    __slots__ = ("name", "ap", "writer", "readers")

    def __init__(self, name, ap):
        self.name = name
        self.ap = ap
        self.writer = None
        self.readers = []


class Sync:
    NDMA = 6

    def __init__(self, nc):
        self.nc = nc
        self.engs = {"pe": nc.tensor, "act": nc.scalar, "dve": nc.vector, "pool": nc.gpsimd, "sp": nc.sync}
        self.sem = {k: nc.alloc_semaphore(name=f"s_{k}") for k in self.engs}
        self.cnt = {k: 0 for k in self.engs}
        self.seen = {k: {} for k in self.engs}
        self.dsem = {k: [nc.alloc_semaphore(name=f"d_{k}{i}") for i in range(self.NDMA)] for k in ("sp", "act", "pool")}
        self.dcnt = {k: 0 for k in self.dsem}
        self.dlast = {k: [None] * self.NDMA for k in self.dsem}
        self.out_tickets = []
        self.ninstr = 0

    def _wait(self, e, tick):
        if tick is None:
            return
        sem, val = tick
        key = sem.name
        if e == "pe" and key == "s_pe":
            return
        if self.seen[e].get(key, 0) >= val:
            return
        self.engs[e].wait_ge(sem, val)
        self.seen[e][key] = val

    def deps(self, e, reads, writes):
        for b in reads:
            self._wait(e, b.writer)
        for b in writes:
            self._wait(e, b.writer)
            for r in b.readers:
                self._wait(e, r)

    def commit(self, tick, reads, writes):
        for b in reads:
            b.readers.append(tick)
            if len(b.readers) > 48:
                last = {}
                for t in b.readers:
                    last[t[0].name] = t
                b.readers = list(last.values())
        for b in writes:
            b.writer = tick
            b.readers = []

    def op(self, e, fn, reads=(), writes=()):
        self.deps(e, reads, writes)
        ins = fn()
        self.cnt[e] += 1
        ins.then_inc(self.sem[e], 1)
        tick = (self.sem[e], self.cnt[e])
        self.commit(tick, reads, writes)
        self.ninstr += 1
        return tick

    def group(self, e, fns, reads=(), writes=()):
        self.deps(e, reads, writes)
        ins = None
        for fn in fns:
            ins = fn()
            self.ninstr += 1
        self.cnt[e] += 1
        ins.then_inc(self.sem[e], 1)
        tick = (self.sem[e], self.cnt[e])
        self.commit(tick, reads, writes)
        return tick

    def _dma_common(self, q, issue, reads, writes, is_output):
        j = self.dcnt[q]
        slot = j % self.NDMA
        self._wait(q, self.dlast[q][slot])
        self.deps(q, reads, writes)
        sem = self.dsem[q][slot]
        val = 16 * (j // self.NDMA + 1)
        issue().then_inc(sem, 16)
        self.dcnt[q] += 1
        tick = (sem, val)
        self.dlast[q][slot] = tick
        self.commit(tick, reads, writes)
        if is_output:
            self.out_tickets.append(tick)
        self.ninstr += 1
        return tick

    def dma(self, q, out_ap, in_ap, reads=(), writes=(), is_output=False, **kw):
        return self._dma_common(q, lambda: self.engs[q].dma_start(out=out_ap, in_=in_ap, **kw), reads, writes, is_output)

    def idma(self, reads=(), writes=(), **kw):
        return self._dma_common("pool", lambda: self.nc.gpsimd.indirect_dma_start(**kw), reads, writes, False)

    def barrier(self):
        ticks = [(self.sem[e], self.cnt[e]) for e in self.engs if self.cnt[e] > 0]
        for q in self.dsem:
            ticks += [t for t in self.dlast[q] if t is not None]
        for e in self.engs:
            for t in ticks:
                self._wait(e, t)

    def finish(self):
        for q in self.dsem:
            for t in self.dlast[q]:
                self._wait(q, t)
        for t in self.out_tickets:
            self._wait("sp", t)


class Phase:
    def __init__(self, nc, S, name):
        self.nc, self.S, self.name = nc, S, name
        self.stack = ExitStack()
        self.n = 0

    def __enter__(self):
        self.stack.__enter__()
        return self

    def __exit__(self, *a):
        self.S.barrier()
        return self.stack.__exit__(*a)

    def sb(self, name, shape, dt):
        self.n += 1
        t = self.stack.enter_context(self.nc.sbuf_tensor(f"{self.name}_{name}_{self.n}", list(shape), dt))
        return Buf(name, t.ap())

    def ps(self, name, shape, dt=F32):
        self.n += 1
        t = self.stack.enter_context(self.nc.psum_tensor(f"{self.name}_{name}_{self.n}", list(shape), dt))
        return Buf(name, t.ap())

    def rot(self, name, n, shape, dt):
        return [self.sb(f"{name}{i}", shape, dt) for i in range(n)]


def build(cfg=None, dbg=False):
    if cfg is None:
        cfg = [(i, p) for i in range(DEPTH) for p in ("mix", "moe")]
    nc = bass.Bass("TRN2", target_bir_lowering=False)

    def din(name, shape, dt=F32):
        return nc.dram_tensor(name, list(shape), dt, kind="ExternalInput").ap()

    x_in = din("x", [NSAMP, NL, D])
    c_in = din("c", [NSAMP, D])
    ctx_in = din("ctx", [NSAMP, NCX, D])
    cctx_in = din("c_ctx", [D])
    ada_w = din("ada_w", [DEPTH, D, 6 * D])
    ada_b = din("ada_b", [DEPTH, 6 * D])
    norm1_g = din("norm1_g", [DEPTH, D])
    norm2_g = din("norm2_g", [DEPTH, D])
    final_g = din("final_g", [D])
    attn_w_qkv = din("attn_w_qkv", [2, D, 3 * D])
    attn_w_o = din("attn_w_o", [2, D, D])
    attn_lam = [din(n, [2, 64]) for n in ("attn_lam_q1", "attn_lam_k1", "attn_lam_q2", "attn_lam_k2")]
    attn_subln_g = din("attn_subln_g", [2, 128])
    ssm_lam_re = din("ssm_lam_re", [2, 2, 64, 64])
    ssm_lam_im = din("ssm_lam_im", [2, 2, 64, 64])
    ssm_log_dt = din("ssm_log_dt", [2, 2, 64])
    ssm_b_re = din("ssm_b_re", [2, 2, 64, 64, 16])
    ssm_b_im = din("ssm_b_im", [2, 2, 64, 64, 16])
    ssm_c_re = din("ssm_c_re", [2, 2, 64, 16, 64])
    ssm_c_im = din("ssm_c_im", [2, 2, 64, 16, 64])
    ssm_d = din("ssm_d", [2, D])
    ssm_w_glu1 = din("ssm_w_glu1", [2, D, D])
    ssm_w_glu2 = din("ssm_w_glu2", [2, D, D])
    moe_w_router = din("moe_w_router", [DEPTH, D, NE])
    moe_b_router = din("moe_b_router", [DEPTH, NE])
    moe_w_gate = din("moe_w_gate", [DEPTH, NE, D, FF])
    moe_w_up = din("moe_w_up", [DEPTH, NE, D, FF])
    moe_w_down = din("moe_w_down", [DEPTH, NE, FF, D])
    ident_in = din("k_ident", [128, 128])
    ropec_in = din("k_ropec", [128, NL])
    ropes_in = din("k_ropes", [128, NL])
    out_d = nc.dram_tensor("out", [NSAMP, NL, D], F32, kind="ExternalOutput").ap()

    xT_t = nc.dram_tensor("xT_scr", [NSAMP, KC, 128, NT], F32, kind="ExternalOutput" if dbg else "Internal").ap()
    h2tok_t = nc.dram_tensor("h2tok_scr", [NSAMP, NT, D], BF16, kind="Internal").ap()
    macc_t = nc.dram_tensor("macc_scr", [NSAMP, NT, D], F32, kind="Internal").ap()
    s5w_t = nc.dram_tensor("s5w_scr", [64, 5, 128, 128], BF16, kind="Internal").ap()
    s5tab_t = nc.dram_tensor("s5tab_scr", [64, 2, 128, 512], F32, kind="Internal").ap()

    S = Sync(nc)
    xT_b = [Buf(f"xT{s}", xT_t[s]) for s in range(NSAMP)]
    h2l_b = [Buf(f"h2l{s}", h2tok_t[s, 0:NL, :]) for s in range(NSAMP)]
    h2c_b = [Buf(f"h2c{s}", h2tok_t[s, NL:NT, :]) for s in range(NSAMP)]
    mal_b = [Buf(f"mal{s}", macc_t[s, 0:NL, :]) for s in range(NSAMP)]
    mac_b = [Buf(f"mac{s}", macc_t[s, NL:NT, :]) for s in range(NSAMP)]

    def xT_view(s, t0, n):
        return xT_t[s].rearrange("k p t -> p k t")[:, :, t0:t0 + n]

    BLOCKS = [(0, 512), (512, 512), (1024, 512), (1536, 512), (2048, 256)]

    V, T, G, A = nc.vector, nc.tensor, nc.gpsimd, nc.scalar

    with ExitStack() as gstack:
        def gsb(name, shape, dt):
            t = gstack.enter_context(nc.sbuf_tensor("g_" + name, list(shape), dt))
            return Buf(name, t.ap())

        ident = gsb("ident", [128, 128], F32)
        identb = gsb("identb", [128, 128], BF16)
        ones = gsb("ones", [128, 128], F32)
        zeros = gsb("zeros", [128, 1024], F32)
        scT = gsb("scT", [128, KC, 4], F32)
        mod = gsb("mod", [128, 48, 4], F32)
        A1 = gsb("A1", [128, KC, 4], F32)
        A2 = gsb("A2", [128, KC, 4], F32)
        n1g = gsb("n1g", [128, KC], F32)
        n2g = gsb("n2g", [128, KC], F32)
        adab = gsb("adab", [128, 48], F32)

        with Phase(nc, S, "p0") as ph:
            S.dma("sp", ident.ap, ident_in, writes=[ident])
            S.op("dve", lambda: V.tensor_copy(identb.ap, ident.ap), reads=[ident], writes=[identb])
            S.op("pool", lambda: G.memset(ones.ap, 1.0), writes=[ones])
            S.op("pool", lambda: G.memset(zeros.ap, 0.0), writes=[zeros])
            S.op("pool", lambda: G.memset(scT.ap, 0.0), writes=[scT])
            craw = ph.sb("craw", [128, KC, 4], F32)
            S.op("pool", lambda: G.memset(craw.ap, 0.0), writes=[craw])
            for v in range(3):
                src = c_in[v] if v < 2 else cctx_in
                S.dma("sp", craw.ap[:, :, v], src.rearrange("(k p) -> p k", p=128), writes=[craw],
                      allow_slow_non_contiguous=True)
            S.op("act", lambda: A.activation(scT.ap, craw.ap, AF.Silu), reads=[craw], writes=[scT])
            xin = ph.rot("xin", 2, [128, D], F32)
            xtt = ph.rot("xtt", 2, [128, KC, 128], F32)
            pst = [ph.ps("pst0", [128, 512]), ph.ps("pst1", [128, 512])]
            it = 0
            for s in range(NSAMP):
                for tt in range(NT // 128):
                    src = x_in[s, tt * 128:(tt + 1) * 128, :] if tt < 16 else ctx_in[s, (tt - 16) * 128:(tt - 15) * 128, :]
                    xi = xin[it % 2]
                    xo = xtt[it % 2]
                    S.dma("sp" if it % 2 == 0 else "act", xi.ap, src, writes=[xi])
                    for half in range(2):
                        p = pst[half]
                        S.group("pe", [lambda j=j, p=p, xi=xi, half=half: T.transpose(
                            p.ap[:, j * 128:(j + 1) * 128], xi.ap[:, (half * 4 + j) * 128:(half * 4 + j + 1) * 128], ident.ap)
                            for j in range(4)], reads=[xi, ident], writes=[p])
                        S.op("dve" if half == 0 else "act",
                             (lambda p=p, xo=xo, half=half: V.tensor_copy(
                                 xo.ap[:, half * 4:half * 4 + 4, :], p.ap.rearrange("p (k t) -> p k t", k=4))) if half == 0 else
                             (lambda p=p, xo=xo, half=half: A.activation(
                                 xo.ap[:, half * 4:half * 4 + 4, :], p.ap.rearrange("p (k t) -> p k t", k=4), AF.Copy)),
                             reads=[p], writes=[xo])
                    S.dma("sp" if it % 2 == 1 else "act", xT_view(s, tt * 128, 128), xo.ap, reads=[xo], writes=[xT_b[s]])
                    it += 1

        def adaln(i):
            with Phase(nc, S, f"ada{i}") as ph:
                S.dma("sp", adab.ap, ada_b[i].rearrange("(j p) -> p j", p=128), writes=[adab], allow_slow_non_contiguous=True)
                S.dma("act", n1g.ap, norm1_g[i].rearrange("(k p) -> p k", p=128), writes=[n1g], allow_slow_non_contiguous=True)
                S.dma("act", n2g.ap, norm2_g[i].rearrange("(k p) -> p k", p=128), writes=[n2g], allow_slow_non_contiguous=True)
                wst = ph.rot("wst", 2, [128, KC, 512], F32)
                pm = ph.ps("pm", [128, 48, 4])
                for jb in range(12):
                    w = wst[jb % 2]
                    S.dma("sp" if jb % 2 == 0 else "act", w.ap,
                          ada_w[i][:, jb * 512:(jb + 1) * 512].rearrange("(k p) f -> p k f", p=128), writes=[w])
                    for fs in range(4):
                        j = jb * 4 + fs
                        S.group("pe", [lambda kc=kc, j=j, fs=fs, w=w: T.matmul(
                            pm.ap[:, j, :], w.ap[:, kc, fs * 128:(fs + 1) * 128], scT.ap[:, kc, :], start=(kc == 0), stop=(kc == KC - 1))
                            for kc in range(KC)], reads=[w, scT], writes=[pm])
                for v in range(3):
                    S.op("dve", lambda v=v: V.tensor_tensor(mod.ap[:, :, v], pm.ap[:, :, v], adab.ap, ALU.add),
                         reads=[pm, adab], writes=[mod])
                for v in range(3):
                    S.op("dve", lambda v=v: V.scalar_tensor_tensor(A1.ap[:, :, v], mod.ap[:, 8:16, v], 1.0, n1g.ap, ALU.add, ALU.mult),
                         reads=[mod, n1g], writes=[A1])
                    S.op("dve", lambda v=v: V.scalar_tensor_tensor(A2.ap[:, :, v], mod.ap[:, 32:40, v], 1.0, n2g.ap, ALU.add, ALU.mult),
                         reads=[mod, n2g], writes=[A2])

        def norm_block(ph, R, s, t0, n, Acoef, shbase, h32, hb_ap, hb_buf, it):
            v = s if t0 < NL else 2
            xb = R["xb"][it % 2]
            sq = R["sq"]
            S.dma("sp" if it % 2 == 0 else "act", xb.ap[:, :, :n], xT_view(s, t0, n), reads=[xT_b[s]], writes=[xb])
            S.op("act", lambda: A.activation(sq.ap[:, :, :n], xb.ap[:, :, :n], AF.Square), reads=[xb], writes=[sq])
            ssp = R["ssp"]
            S.group("pe", [lambda kc=kc: T.matmul(ssp.ap[:, :n], ones.ap, sq.ap[:, kc, :n], start=(kc == 0), stop=(kc == KC - 1))
                           for kc in range(KC)], reads=[ones, sq], writes=[ssp])
            rstd = R["rstd"]
            S.op("act", lambda: A.activation(rstd.ap[:, :n], ssp.ap[:, :n], AF.Sqrt, bias=R["eps"].ap, scale=1.0 / D),
                 reads=[ssp, R["eps"]], writes=[rstd])
            S.op("dve", lambda: V.reciprocal(rstd.ap[:, :n], rstd.ap[:, :n]), reads=[rstd], writes=[rstd])
            for kc in range(KC):
                S.op("dve", lambda kc=kc: V.tensor_tensor(sq.ap[:, kc, :n], xb.ap[:, kc, :n], rstd.ap[:, :n], ALU.mult),
                     reads=[xb, rstd], writes=[sq])
            for kc in range(KC):
                S.op("pool", lambda kc=kc: G.tensor_scalar(h32.ap[:, kc, :n], sq.ap[:, kc, :n], Acoef.ap[:, kc, v:v + 1],
                                                           mod.ap[:, shbase + kc, v:v + 1], ALU.mult, ALU.add),
                     reads=[sq, Acoef, mod], writes=[h32])
            S.op("act", lambda: A.activation(hb_ap, h32.ap[:, :, :n], AF.Copy), reads=[h32], writes=[hb_buf])

        def norm_resources(ph, ssp=None):
            R = {"xb": ph.rot("xb", 2, [128, KC, 512], F32), "sq": ph.sb("sq", [128, KC, 512], F32),
                 "ssp": ssp if ssp is not None else ph.ps("ssp", [128, 512]), "rstd": ph.sb("rstd", [128, 512], F32), "eps": ph.sb("eps", [128, 1], F32)}
            S.op("pool", lambda: G.memset(R["eps"].ap, EPS), writes=[R["eps"]])
            return R

        def moe_full(i):
            with_ctx = i < DEPTH - 1
            blocks = BLOCKS if with_ctx else BLOCKS[:4]
            with Phase(nc, S, f"moeO{i}") as pho:
                idxT = [pho.sb(f"idxT{s}", [128, 3, NE], U32) for s in range(NSAMP)]
                valT = [pho.sb(f"valT{s}", [128, 3, NE], F32) for s in range(NSAMP)]
                for s in range(NSAMP):
                    S.op("pool", lambda s=s: G.memset(idxT[s].ap, 0), writes=[idxT[s]])
                    S.op("pool", lambda s=s: G.memset(valT[s].ap, 0.0), writes=[valT[s]])
                with Phase(nc, S, f"moeR{i}") as ph:
                    R = norm_resources(ph)
                    wr = ph.sb("wr", [128, KC, NE], F32)
                    br = ph.sb("br", [NE, 1], F32)
                    S.dma("sp", wr.ap, moe_w_router[i].rearrange("(k p) e -> p k e", p=128), writes=[wr])
                    S.dma("sp", br.ap, moe_b_router[i].rearrange("(e o) -> e o", o=1), writes=[br])
                    zi = 0
                    for s in range(NSAMP):
                        for tt in range(NT // 128 if with_ctx else NL // 128):
                            S.dma("sp" if zi % 2 == 0 else "act", macc_t[s, tt * 128:(tt + 1) * 128, :], zeros.ap,
                                  reads=[zeros], writes=[mal_b[s] if tt < 16 else mac_b[s]])
                            zi += 1
                    h32 = ph.sb("h32", [128, KC, 512], F32)
                    hb = ph.rot("hb", 2, [128, KC, 512], BF16)
                    expT = [ph.sb(f"expT{s}", [NE, NT], F32) for s in range(NSAMP)]
                    aff = [ph.sb(f"aff{s}", [NE, NT], F32) for s in range(NSAMP)]
                    lgp = ph.ps("lgp", [128, 512])
                    smp = ph.ps("smp", [128, 512])
                    rs = ph.sb("rs", [NE, 512], F32)
                    ptb = ph.ps("ptb", [128, 512])
                    ptb_bf = ptb.ap.bitcast(BF16)
                    tok = ph.rot("tok", 2, [128, D], BF16)
                    it = 0
                    ti = 0
                    for s in range(NSAMP):
                        for (t0, n) in blocks:
                            hbb = hb[it % 2]
                            norm_block(ph, R, s, t0, n, A2, 24, h32, hbb.ap[:, :, :n], hbb, it)
                            S.group("pe", [lambda kc=kc, n=n: T.matmul(lgp.ap[0:NE, :n], wr.ap[:, kc, :], h32.ap[:, kc, :n],
                                                                         start=(kc == 0), stop=(kc == KC - 1)) for kc in range(KC)],
                                    reads=[wr, h32], writes=[lgp])
                            S.op("act", lambda s=s, t0=t0, n=n: A.activation(expT[s].ap[:, t0:t0 + n], lgp.ap[0:NE, :n], AF.Exp, bias=br.ap),
                                 reads=[lgp, br], writes=[expT[s]])
                            S.group("pe", [lambda s=s, t0=t0, n=n: T.matmul(smp.ap[0:NE, :n], ones.ap[0:NE, 0:NE], expT[s].ap[:, t0:t0 + n],
                                                                          start=True, stop=True)], reads=[ones, expT[s]], writes=[smp])
                            S.op("dve", lambda n=n: V.reciprocal(rs.ap[:, :n], smp.ap[0:NE, :n]), reads=[smp], writes=[rs])
                            S.op("dve", lambda s=s, t0=t0, n=n: V.tensor_tensor(aff[s].ap[:, t0:t0 + n], expT[s].ap[:, t0:t0 + n], rs.ap[:, :n], ALU.mult),
                                 reads=[expT[s], rs], writes=[aff[s]])
                            for tt in range(n // 128):
                                tk = tok[ti % 2]
                                S.group("pe", [lambda kc=kc, tt=tt, hbb=hbb: T.transpose(
                                    ptb_bf[:, kc * 128:(kc + 1) * 128], hbb.ap[:, kc, tt * 128:(tt + 1) * 128], identb.ap) for kc in range(KC)],
                                    reads=[hbb, identb], writes=[ptb])
                                S.op("dve", lambda tk=tk: V.tensor_copy(tk.ap, ptb_bf), reads=[ptb], writes=[tk])
                                r0 = t0 + tt * 128
                                S.dma("sp" if ti % 2 == 0 else "act", h2tok_t[s, r0:r0 + 128, :], tk.ap, reads=[tk],
                                      writes=[h2l_b[s] if r0 < NL else h2c_b[s]])
                                ti += 1
                            it += 1
                    vals = [ph.sb(f"vals{s}", [NE, CAPL + CAPC], F32) for s in range(NSAMP)]
                    idx = [ph.sb(f"idx{s}", [NE, CAPL + CAPC], U32) for s in range(NSAMP)]
                    idxf = [ph.sb(f"idxf{s}", [NE, CAPL + CAPC], F32) for s in range(NSAMP)]
                    segs = [(0, NL, 0, CAPL // 8)] + ([(NL, NCX, CAPL, CAPC // 8)] if with_ctx else [])
                    for (a0, an, c0, rounds) in segs:
                        for r in range(rounds):
                            for s in range(NSAMP):
                                av = aff[s].ap[:, a0:a0 + an]
                                vv = vals[s].ap[:, c0 + r * 8:c0 + r * 8 + 8]
                                S.op("dve", lambda av=av, vv=vv: V.max(vv, av), reads=[aff[s]], writes=[vals[s]])
                                S.op("dve", lambda av=av, vv=vv, s=s, c0=c0, r=r: V.max_index(idx[s].ap[:, c0 + r * 8:c0 + r * 8 + 8], vv, av),
                                     reads=[aff[s], vals[s]], writes=[idx[s]])
                                S.op("dve", lambda av=av, vv=vv: V.match_replace(av, vv, av, -1.0), reads=[vals[s]], writes=[aff[s]])
                    ncap = CAPL + (CAPC if with_ctx else 0)
                    for s in range(NSAMP):
                        S.op("dve", lambda s=s: V.tensor_copy(idxf[s].ap[:, :ncap], idx[s].ap[:, :ncap]), reads=[idx[s]], writes=[idxf[s]])
                        pieces = [(0, 128, 0), (128, 128, 1)] + ([(256, 32, 2)] if with_ctx else [])
                        for (c0, cn, slot) in pieces:
                            S.group("pe", [lambda s=s, c0=c0, cn=cn: T.transpose(lgp.ap[0:cn, 0:NE], idxf[s].ap[:, c0:c0 + cn], ident.ap[0:NE, 0:NE])],
                                    reads=[idxf[s], ident], writes=[lgp])
                            S.op("dve", lambda s=s, cn=cn, slot=slot: V.tensor_copy(idxT[s].ap[0:cn, slot, :], lgp.ap[0:cn, 0:NE]),
                                 reads=[lgp], writes=[idxT[s]])
                            S.group("pe", [lambda s=s, c0=c0, cn=cn: T.transpose(smp.ap[0:cn, 0:NE], vals[s].ap[:, c0:c0 + cn], ident.ap[0:NE, 0:NE])],
                                    reads=[vals[s], ident], writes=[smp])
                            S.op("dve", lambda s=s, cn=cn, slot=slot: V.tensor_copy(valT[s].ap[0:cn, slot, :], smp.ap[0:cn, 0:NE]),
                                 reads=[smp], writes=[valT[s]])

                with Phase(nc, S, f"moeE{i}") as ph:
                    xg = ph.rot("xg", 4, [128, D], BF16)
                    xinT = ph.rot("xinT", 2, [128, KC, NCOL], BF16)
                    wgs = ph.rot("wgs", 3, [128, KC, 256], F32)
                    wus = ph.rot("wus", 3, [128, KC, 256], F32)
                    wgb = ph.rot("wgb", 2, [128, KC, 256], BF16)
                    wub = ph.rot("wub", 2, [128, KC, 256], BF16)
                    wds = ph.rot("wds", 2, [128, 2, D], F32)
                    wdb = ph.sb("wdb", [128, 16, D], BF16)
                    hidT = ph.sb("hidT", [128, 16, NCOL], BF16)
                    sg = ph.rot("sg", 2, [128, NCOL], F32)
                    yT = ph.sb("yT", [128, KC, NCOL], F32)
                    yo = ph.rot("yo", 4, [128, D], F32)
                    Gl = [ph.ps(f"Gl{k}", [128, 512]) for k in range(2)]
                    Ul = [ph.ps(f"Ul{k}", [128, 512]) for k in range(2)]
                    GUc = ph.ps("GUc", [128, 512])
                    Yl2 = [ph.ps(f"Yl{k}", [128, 512]) for k in range(2)]
                    Yc = Buf("Yc", GUc.ap[:, 128:256])
                    ptr = ph.ps("ptr", [128, 512])
                    ptr_bf = ptr.ap.bitcast(BF16)
                    nctx = 2 * CAPC if with_ctx else 0
                    gi = 0
                    wi = 0
                    di = 0
                    fi = 0
                    yi = 0
                    gl = []
                    for s in range(NSAMP):
                        for hf in range(2):
                            gl.append((s, hf, 128, s * CAPL + hf * 128, None, h2l_b[s]))
                    if with_ctx:
                        for s in range(NSAMP):
                            gl.append((s, 2, CAPC, 2 * CAPL + s * CAPC, None, h2c_b[s]))
                    gctr = [0]

                    def gather(e):
                        xt = xinT[e % 2]
                        for (s, slot, cn, col0, src, srcb) in gl:
                            g = xg[gctr[0] % 4]
                            gctr[0] += 1
                            S.idma(out=g.ap[0:cn, :], out_offset=None, in_=h2tok_t.rearrange("s t d -> (s t) d"),
                                   in_offset=bass.IndirectOffsetOnAxis(ap=idxT[s].ap[0:cn, slot, e:e + 1], axis=0),
                                   element_offset=(s * NT + (0 if slot < 2 else NL)) * D,
                                   reads=[idxT[s], srcb], writes=[g])
                            S.group("pe", [lambda kc=kc, g=g, cn=cn: T.transpose(
                                ptr_bf[:, kc * 128:kc * 128 + cn], g.ap[0:cn, kc * 128:(kc + 1) * 128], identb.ap[0:cn, 0:cn]) for kc in range(KC)],
                                reads=[g, identb], writes=[ptr])
                            S.op("dve", lambda xt=xt, col0=col0, cn=cn: V.tensor_copy(
                                xt.ap[:, :, col0:col0 + cn], ptr_bf.rearrange("p (k c) -> p k c", k=KC)[:, :, 0:cn]),
                                reads=[ptr], writes=[xt])

                    gather(0)
                    sc_cur = {}
                    sc_all = []
                    for e in range(NE):
                        xt = xinT[e % 2]
                        def prep(k):
                            e_, p_ = divmod(k, 8)
                            ws_g, ws_u, wb_g, wb_u = wgs[k % 3], wus[k % 3], wgb[k % 2], wub[k % 2]
                            S.dma("sp", ws_g.ap, moe_w_gate[i, e_][:, p_ * 256:(p_ + 1) * 256].rearrange("(k q) f -> q k f", q=128), writes=[ws_g])
                            S.dma("sp", ws_u.ap, moe_w_up[i, e_][:, p_ * 256:(p_ + 1) * 256].rearrange("(k q) f -> q k f", q=128), writes=[ws_u])
                            w = wds[k % 2]
                            S.dma("sp", w.ap, moe_w_down[i, e_, p_ * 256:(p_ + 1) * 256, :].rearrange("(c q) d -> q c d", q=128), writes=[w])
                            S.op("dve", lambda a=wb_g, b=ws_g: V.tensor_copy(a.ap, b.ap), reads=[ws_g], writes=[wb_g])
                            S.op("dve", lambda a=wb_u, b=ws_u: V.tensor_copy(a.ap, b.ap), reads=[ws_u], writes=[wb_u])
                            S.op("pool", lambda w=w, p_=p_: G.tensor_copy(wdb.ap[:, p_ * 2:(p_ + 1) * 2, :], w.ap), reads=[w], writes=[wdb])

                        if e == 0:
                            prep(0)
                        for p in range(8):
                            k = e * 8 + p
                            wb_g, wb_u = wgb[k % 2], wub[k % 2]
                            if k + 1 < NE * 8 and (p < 7):
                                prep(k + 1)
                            for fsub in range(2):
                                fc = p * 2 + fsub
                                gl_, ul_ = Gl[fi % 2], Ul[fi % 2]
                                sgt = sg[fi % 2]
                                fi += 1
                                fs = slice(fsub * 128, (fsub + 1) * 128)
                                S.group("pe", [lambda kc=kc, gl_=gl_, wb_g=wb_g, fs=fs, xt=xt: T.matmul(
                                    gl_.ap[:, 0:512], wb_g.ap[:, kc, fs], xt.ap[:, kc, 0:512], start=(kc == 0), stop=(kc == KC - 1))
                                    for kc in range(KC)], reads=[wb_g, xt], writes=[gl_])
                                S.group("pe", [lambda kc=kc, ul_=ul_, wb_u=wb_u, fs=fs, xt=xt: T.matmul(
                                    ul_.ap[:, 0:512], wb_u.ap[:, kc, fs], xt.ap[:, kc, 0:512], start=(kc == 0), stop=(kc == KC - 1))
                                    for kc in range(KC)], reads=[wb_u, xt], writes=[ul_])
                                if with_ctx:
                                    S.group("pe", [lambda kc=kc, wb_g=wb_g, fs=fs, xt=xt: T.matmul(
                                        GUc.ap[:, 0:64], wb_g.ap[:, kc, fs], xt.ap[:, kc, 512:576], start=(kc == 0), stop=(kc == KC - 1))
                                        for kc in range(KC)], reads=[wb_g, xt], writes=[GUc])
                                    S.group("pe", [lambda kc=kc, wb_u=wb_u, fs=fs, xt=xt: T.matmul(
                                        GUc.ap[:, 64:128], wb_u.ap[:, kc, fs], xt.ap[:, kc, 512:576], start=(kc == 0), stop=(kc == KC - 1))
                                        for kc in range(KC)], reads=[wb_u, xt], writes=[GUc])
                                S.op("act", lambda sgt=sgt, gl_=gl_: A.activation(sgt.ap[:, 0:512], gl_.ap, AF.Silu), reads=[gl_], writes=[sgt])
                                S.op("dve", lambda sgt=sgt, ul_=ul_, fc=fc: V.tensor_tensor(hidT.ap[:, fc, 0:512], sgt.ap[:, 0:512], ul_.ap, ALU.mult),
                                     reads=[sgt, ul_], writes=[hidT])
                                if with_ctx:
                                    S.op("act", lambda sgt=sgt: A.activation(sgt.ap[:, 512:576], GUc.ap[:, 0:64], AF.Silu), reads=[GUc], writes=[sgt])
                                    S.op("dve", lambda sgt=sgt, fc=fc: V.tensor_tensor(hidT.ap[:, fc, 512:576], sgt.ap[:, 512:576], GUc.ap[:, 64:128], ALU.mult),
                                         reads=[sgt, GUc], writes=[hidT])
                        if e + 1 < NE:
                            gather(e + 1)
                        for dc in range(KC):
                            ds_ = slice(dc * 128, (dc + 1) * 128)
                            Yl = Yl2[dc % 2]
                            S.group("pe", [lambda fc=fc, ds_=ds_, Yl=Yl: T.matmul(Yl.ap[:, 0:512], wdb.ap[:, fc, ds_], hidT.ap[:, fc, 0:512],
                                                                           start=(fc == 0), stop=(fc == 15)) for fc in range(16)],
                                    reads=[wdb, hidT], writes=[Yl])
                            S.op("act", lambda dc=dc, Yl=Yl: A.activation(yT.ap[:, dc, 0:512], Yl.ap, AF.Copy), reads=[Yl], writes=[yT])
                            if with_ctx:
                                S.group("pe", [lambda fc=fc, ds_=ds_: T.matmul(Yc.ap[:, 0:64], wdb.ap[:, fc, ds_], hidT.ap[:, fc, 512:576],
                                                                               start=(fc == 0), stop=(fc == 15)) for fc in range(16)],
                                        reads=[wdb, hidT], writes=[Yc])
                                S.op("act", lambda dc=dc: A.activation(yT.ap[:, dc, 512:576], Yc.ap[:, 0:64], AF.Copy), reads=[Yc], writes=[yT])
                        if e + 1 < NE:
                            prep((e + 1) * 8)
                        sc_prev, sc_cur = sc_cur, {}
                        for (s, slot, cn, col0, src, srcb) in gl:
                            y = yo[yi % 4]
                            yi += 1
                            for half in range(2):
                                S.group("pe", [lambda j=j, half=half, col0=col0, cn=cn: T.transpose(
                                    ptr.ap[0:cn, j * 128:(j + 1) * 128], yT.ap[:, half * 4 + j, col0:col0 + cn], ident.ap)
                                    for j in range(4)], reads=[yT, ident], writes=[ptr])
                                S.op("dve", lambda y=y, half=half, cn=cn, s=s, slot=slot, e=e: V.tensor_scalar(
                                    y.ap[0:cn, half * 512:(half + 1) * 512], ptr.ap[0:cn, :], valT[s].ap[0:cn, slot, e:e + 1], None, ALU.mult),
                                    reads=[ptr, valT[s]], writes=[y])
                            dstb = mal_b[s] if slot < 2 else mac_b[s]
                            for t_ in sc_prev.get(dstb.name, []):
                                S._wait("pool", t_)
                            S._wait("pool", dstb.writer)
                            tk_ = S.idma(out=macc_t.rearrange("s t d -> (s t) d"),
                                         out_offset=bass.IndirectOffsetOnAxis(ap=idxT[s].ap[0:cn, slot, e:e + 1], axis=0),
                                         in_=y.ap[0:cn, :], in_offset=None, compute_op=ALU.add,
                                         element_offset=(s * NT + (0 if slot < 2 else NL)) * D,
                                         reads=[y, idxT[s]], writes=[])
                            sc_cur.setdefault(dstb.name, []).append(tk_)
                            sc_all.append(tk_)

                    for t_ in sc_all[-12:]:
                        S._wait("pool", t_)
                    for s_ in range(NSAMP):
                        for b_ in (mal_b[s_], mac_b[s_]):
                            S.op("pool", lambda: G.memset(zeros.ap[:, 0:1], 0.0), reads=[], writes=[b_])
                with Phase(nc, S, f"moeU{i}") as ph:
                    xb = ph.rot("xb", 2, [128, KC, 512], F32)
                    mt = ph.rot("mt", 2, [128, D], F32)
                    pt = [ph.ps(f"pt{k}", [128, 512]) for k in range(2)]
                    it = 0
                    mi = 0
                    for s in range(NSAMP):
                        for (t0, n) in blocks:
                            v = s if t0 < NL else 2
                            x = xb[it % 2]
                            it += 1
                            S.dma("sp", x.ap[:, :, :n], xT_view(s, t0, n), reads=[xT_b[s]], writes=[x])
                            for tt in range(n // 128):
                                m = mt[mi % 2]
                                mi += 1
                                r0 = t0 + tt * 128
                                S.dma("act", m.ap, macc_t[s, r0:r0 + 128, :], reads=[mal_b[s] if r0 < NL else mac_b[s]], writes=[m])
                                for half in range(2):
                                    p = pt[half]
                                    S.group("pe", [lambda j=j, half=half, m=m, p=p: T.transpose(
                                        p.ap[:, j * 128:(j + 1) * 128], m.ap[:, (half * 4 + j) * 128:(half * 4 + j + 1) * 128], ident.ap)
                                        for j in range(4)], reads=[m, ident], writes=[p])
                                    for j in range(4):
                                        kc = half * 4 + j
                                        S.op("dve", lambda j=j, kc=kc, p=p, x=x, tt=tt, v=v: V.scalar_tensor_tensor(
                                            x.ap[:, kc, tt * 128:(tt + 1) * 128], p.ap[:, j * 128:(j + 1) * 128], mod.ap[:, 40 + kc, v:v + 1],
                                            x.ap[:, kc, tt * 128:(tt + 1) * 128], ALU.mult, ALU.add), reads=[p, mod, x], writes=[x])
                            S.dma("sp", xT_view(s, t0, n), x.ap[:, :, :n], reads=[x], writes=[xT_b[s]])

        def final():
            with Phase(nc, S, "fin") as ph:
                R = norm_resources(ph)
                fg = ph.sb("fg", [128, KC], F32)
                S.dma("sp", fg.ap, final_g.rearrange("(k p) -> p k", p=128), writes=[fg], allow_slow_non_contiguous=True)
                xn = ph.sb("xn", [128, KC, 512], F32)
                pt = [ph.ps(f"pt{k}", [128, 512]) for k in range(2)]
                ot = ph.rot("ot", 2, [128, D], F32)
                it = 0
                oi = 0
                for s in range(NSAMP):
                    for (t0, n) in BLOCKS[:4]:
                        xb = R["xb"][it % 2]
                        sq = R["sq"]
                        S.dma("sp" if it % 2 == 0 else "act", xb.ap, xT_view(s, t0, n), reads=[xT_b[s]], writes=[xb])
                        it += 1
                        S.op("act", lambda xb=xb: A.activation(sq.ap, xb.ap, AF.Square), reads=[xb], writes=[sq])
                        ssp = R["ssp"]
                        S.group("pe", [lambda kc=kc: T.matmul(ssp.ap, ones.ap, sq.ap[:, kc, :], start=(kc == 0), stop=(kc == KC - 1))
                                       for kc in range(KC)], reads=[ones, sq], writes=[ssp])
                        rstd = R["rstd"]
                        S.op("act", lambda: A.activation(rstd.ap, ssp.ap, AF.Sqrt, bias=R["eps"].ap, scale=1.0 / D), reads=[ssp, R["eps"]], writes=[rstd])
                        S.op("dve", lambda: V.reciprocal(rstd.ap, rstd.ap), reads=[rstd], writes=[rstd])
                        for kc in range(KC):
                            S.op("dve", lambda kc=kc, xb=xb: V.scalar_tensor_tensor(xn.ap[:, kc, :], xb.ap[:, kc, :], fg.ap[:, kc:kc + 1], rstd.ap,
                                                                                    ALU.mult, ALU.mult), reads=[xb, fg, rstd], writes=[xn])
                        for tt in range(4):
                            o = ot[oi % 2]
                            oi += 1
                            for half in range(2):
                                p = pt[half]
                                S.group("pe", [lambda j=j, half=half, tt=tt, p=p: T.transpose(
                                    p.ap[:, j * 128:(j + 1) * 128], xn.ap[:, half * 4 + j, tt * 128:(tt + 1) * 128], ident.ap) for j in range(4)],
                                    reads=[xn, ident], writes=[p])
                                if half == 0:
                                    S.op("dve", lambda o=o, p=p: V.tensor_copy(o.ap[:, 0:512], p.ap), reads=[p], writes=[o])
                                else:
                                    S.op("act", lambda o=o, p=p: A.activation(o.ap[:, 512:1024], p.ap, AF.Copy), reads=[p], writes=[o])
                            r0 = t0 + tt * 128
                            S.dma("sp" if oi % 2 == 0 else "act", out_d[s, r0:r0 + 128, :], o.ap, reads=[o], is_output=True)


        def attn(i):
            j = i // 2
            lam_init = 0.8 - 0.6 * math.exp(-0.3 * i)
            with Phase(nc, S, f"att{i}") as ph:
                PA = ph.ps("PA", [128, 512])
                PB = ph.ps("PB", [128, 512])
                Sp = [ph.ps(f"Sp{k}", [128, 512]) for k in range(2)]
                Op = [ph.ps(f"Op{k}", [128, 512]) for k in range(4)]
                R = norm_resources(ph, ssp=PA)
                hT = ph.sb("hT", [128, KC, NT], BF16)
                h32 = R["sq"]
                onT = ph.sb("onT", [128, 8, NT], BF16)
                ropec = ph.sb("ropec", [128, NL], F32)
                ropes = ph.sb("ropes", [128, NL], F32)
                S.dma("sp", ropec.ap, ropec_in, writes=[ropec])
                S.dma("act", ropes.ap, ropes_in, writes=[ropes])
                wo = ph.sb("wo", [128, 8, D], BF16)
                S.dma("pool", wo.ap, attn_w_o[j].rearrange("(k p) f -> p k f", p=128), writes=[wo])
                lt = ph.sb("lt", [64, 4], F32)
                for a in range(4):
                    S.dma("sp", lt.ap[:, a:a + 1], attn_lam[a][j].rearrange("(p o) -> p o", o=1), writes=[lt], allow_slow_non_contiguous=True)
                pr = ph.sb("pr", [64, 2], F32)
                S.op("dve", lambda: V.tensor_tensor(pr.ap[:, 0:1], lt.ap[:, 0:1], lt.ap[:, 1:2], ALU.mult), reads=[lt], writes=[pr])
                S.op("dve", lambda: V.tensor_tensor(pr.ap[:, 1:2], lt.ap[:, 2:3], lt.ap[:, 3:4], ALU.mult), reads=[lt], writes=[pr])
                S.group("pe", [lambda: T.matmul(PB.ap[:, 0:2], ones.ap[0:64, :], pr.ap, start=True, stop=True)], reads=[ones, pr], writes=[PB])
                ex = ph.sb("ex", [128, 2], F32)
                S.op("act", lambda: A.activation(ex.ap, PB.ap[:, 0:2], AF.Exp), reads=[PB], writes=[ex])
                neglam = ph.sb("neglam", [128, 1], F32)
                S.op("dve", lambda: V.tensor_tensor(neglam.ap, ex.ap[:, 1:2], ex.ap[:, 0:1], ALU.subtract), reads=[ex], writes=[neglam])
                S.op("dve", lambda: V.tensor_scalar(neglam.ap, neglam.ap, -lam_init, None, ALU.add), reads=[neglam], writes=[neglam])
                subg = ph.sb("subg", [128, 1], F32)
                S.dma("sp", subg.ap, attn_subln_g[j].rearrange("(p o) -> p o", o=1), writes=[subg], allow_slow_non_contiguous=True)
                S.op("dve", lambda: V.tensor_scalar(subg.ap, subg.ap, 1.0 - lam_init, None, ALU.mult), reads=[subg], writes=[subg])
                wq = ph.rot("wq", 2, [128, KC, 128], BF16)
                wk = ph.rot("wk", 2, [128, KC, 128], BF16)
                wv = ph.rot("wv", 2, [128, KC, 128], BF16)
                wqs = ph.rot("wqs", 2, [128, KC, 128], BF16)
                wks = ph.rot("wks", 2, [128, KC, 128], BF16)
                qT = ph.sb("qT", [128, NT], BF16)
                kT = ph.sb("kT", [128, NT], BF16)
                vx = ph.sb("vx", [128, NT // 128, 130], BF16)
                S.op("pool", lambda: G.memset(vx.ap, 1.0), writes=[vx])
                t1 = ph.rot("t1", 2, [128, 512], F32)
                t2 = ph.rot("t2", 2, [128, 512], F32)
                Et = ph.rot("Et", 2, [128, 512], BF16)
                otmp = ph.sb("otmp", [128, 4, 128], F32)
                osq = ph.sb("osq", [128, 128], F32)
                sm = ph.rot("sm", 4, [128, 4], F32)
                xb = R["xb"]
                wsrc = attn_w_qkv[j]

                def wview(base):
                    return wsrc[:, base:base + 128].rearrange("(k p) f -> p k f", p=128)

                def wview_sw(base, two):
                    return wsrc[:, base:base + 128].rearrange("(k p) (b two q) -> p k b two q", p=128, two=2, q=16)[:, :, :, two, :]

                it = 0
                hi = 0
                ei = 0
                for s in range(NSAMP):
                    for (t0, n) in BLOCKS:
                        norm_block(ph, R, s, t0, n, A1, 0, h32, hT.ap[:, :, t0:t0 + n], hT, it)
                        it += 1
                    for h in range(8):
                        Wq, Wk, Wv, Wqs, Wks = wq[hi % 2], wk[hi % 2], wv[hi % 2], wqs[hi % 2], wks[hi % 2]
                        hi += 1
                        S.dma("pool", Wq.ap, wview(h * 128), writes=[Wq])
                        S.dma("pool", Wk.ap, wview(D + h * 128), writes=[Wk])
                        S.dma("pool", Wv.ap, wview(2 * D + h * 128), writes=[Wv])
                        for two in range(2):
                            for (Wd_, Ws_) in ((Wqs, Wq), (Wks, Wk)):
                                S.op("pool", lambda Wd_=Wd_, Ws_=Ws_, two=two: G.tensor_copy(
                                    Wd_.ap.rearrange("p k (b two q) -> p k b two q", two=2, q=16)[:, :, :, two, :],
                                    Ws_.ap.rearrange("p k (b two q) -> p k b two q", two=2, q=16)[:, :, :, 1 - two, :]),
                                    reads=[Ws_], writes=[Wd_])
                        for (W, Wsw, dst) in ((Wq, Wqs, qT), (Wk, Wks, kT)):
                            for (t0, n) in BLOCKS:
                                S.group("pe", [lambda kc=kc, W=W, t0=t0, n=n: T.matmul(PA.ap[:, :n], W.ap[:, kc, :], hT.ap[:, kc, t0:t0 + n],
                                                                                        start=(kc == 0), stop=(kc == KC - 1)) for kc in range(KC)],
                                        reads=[W, hT], writes=[PA])
                                if t0 < NL:
                                    S.group("pe", [lambda kc=kc, Wsw=Wsw, t0=t0, n=n: T.matmul(PB.ap[:, :n], Wsw.ap[:, kc, :], hT.ap[:, kc, t0:t0 + n],
                                                                                                start=(kc == 0), stop=(kc == KC - 1)) for kc in range(KC)],
                                            reads=[Wsw, hT], writes=[PB])
                                    a1, a2 = t1[ei % 2], t2[ei % 2]
                                    ei += 1
                                    S.op("dve", lambda a1=a1, t0=t0, n=n: V.tensor_tensor(a1.ap[:, :n], PA.ap[:, :n], ropec.ap[:, t0:t0 + n], ALU.mult),
                                         reads=[PA, ropec], writes=[a1])
                                    S.op("dve", lambda a2=a2, t0=t0, n=n: V.tensor_tensor(a2.ap[:, :n], PB.ap[:, :n], ropes.ap[:, t0:t0 + n], ALU.mult),
                                         reads=[PB, ropes], writes=[a2])
                                    S.op("pool", lambda a1=a1, a2=a2, dst=dst, t0=t0, n=n: G.tensor_tensor(dst.ap[:, t0:t0 + n], a1.ap[:, :n], a2.ap[:, :n], ALU.add),
                                         reads=[a1, a2], writes=[dst])
                                else:
                                    S.op("act", lambda dst=dst, t0=t0, n=n: A.activation(dst.ap[:, t0:t0 + n], PA.ap[:, :n], AF.Copy), reads=[PA], writes=[dst])
                        for tt in range(NT // 128):
                            S.group("pe", [lambda kc=kc, tt=tt, Wv=Wv: T.matmul(PB.ap[:, 0:128], hT.ap[:, kc, tt * 128:(tt + 1) * 128], Wv.ap[:, kc, :],
                                                                                 start=(kc == 0), stop=(kc == KC - 1)) for kc in range(KC)],
                                    reads=[Wv, hT], writes=[PB])
                            S.op("act", lambda tt=tt: A.activation(vx.ap[:, tt, 0:128], PB.ap[:, 0:128], AF.Copy), reads=[PB], writes=[vx])
                        tasks = []
                        for (q0, qn, ktiles) in [(0, 512, list(range(18))), (512, 512, list(range(18))), (1024, 512, list(range(18))),
                                                 (1536, 512, list(range(18))), (2048, 256, [16, 17])]:
                            nq = qn // 128
                            for c in range(2):
                                cs = slice(c * 64, (c + 1) * 64)
                                for ki, kt in enumerate(ktiles):
                                    sp_, et_ = Sp[ei % 2], Et[ei % 2]
                                    ei += 1

                                    def fS(sp_=sp_, kt=kt, cs=cs, q0=q0, qn=qn):
                                        S.group("pe", [lambda: T.matmul(sp_.ap[:, :qn], kT.ap[cs, kt * 128:(kt + 1) * 128], qT.ap[cs, q0:q0 + qn], start=True, stop=True)],
                                                reads=[kT, qT], writes=[sp_])

                                    def fE(sp_=sp_, et_=et_, qn=qn):
                                        S.op("act", lambda: A.activation(et_.ap[:, :qn], sp_.ap[:, :qn], AF.Exp, scale=0.125), reads=[sp_], writes=[et_])

                                    def fPV(et_=et_, kt=kt, ki=ki, nk=len(ktiles), nq=nq):
                                        for qs in range(nq):
                                            S.group("pe", [lambda qs=qs: T.matmul(Op[qs].ap[:, 0:129], et_.ap[:, qs * 128:(qs + 1) * 128], vx.ap[:, kt, 0:129],
                                                                                   start=(ki == 0), stop=(ki == nk - 1))], reads=[et_, vx], writes=[Op[qs]])

                                    def fEpi(c=c, nq=nq, q0=q0, h=h):
                                        for qs in range(nq):
                                            m = sm[qs]
                                            S.op("dve", lambda m=m, qs=qs: V.reciprocal(m.ap[:, 0:1], Op[qs].ap[:, 128:129]), reads=[Op[qs]], writes=[m])
                                            if c == 0:
                                                S.op("dve", lambda m=m, qs=qs: V.tensor_scalar(otmp.ap[:, qs, :], Op[qs].ap[:, 0:128], m.ap[:, 0:1], None, ALU.mult),
                                                     reads=[Op[qs], m], writes=[otmp])
                                            else:
                                                S.op("dve", lambda m=m: V.tensor_tensor(m.ap[:, 1:2], m.ap[:, 0:1], neglam.ap, ALU.mult), reads=[m, neglam], writes=[m])
                                                S.op("dve", lambda m=m, qs=qs: V.scalar_tensor_tensor(otmp.ap[:, qs, :], Op[qs].ap[:, 0:128], m.ap[:, 1:2], otmp.ap[:, qs, :],
                                                                                                      ALU.mult, ALU.add), reads=[Op[qs], m, otmp], writes=[otmp])
                                                S.op("act", lambda m=m, qs=qs: A.activation(osq.ap, otmp.ap[:, qs, :], AF.Square, accum_out=m.ap[:, 2:3]),
                                                     reads=[otmp], writes=[osq, m])
                                                S.op("act", lambda m=m: A.activation(m.ap[:, 3:4], m.ap[:, 2:3], AF.Sqrt, bias=R["eps"].ap, scale=1.0 / 128),
                                                     reads=[m, R["eps"]], writes=[m])
                                                S.op("dve", lambda m=m: V.reciprocal(m.ap[:, 3:4], m.ap[:, 3:4]), reads=[m], writes=[m])
                                                S.op("dve", lambda m=m, qs=qs: V.tensor_scalar(otmp.ap[:, qs, :], otmp.ap[:, qs, :], m.ap[:, 3:4], None, ALU.mult),
                                                     reads=[otmp, m], writes=[otmp])
                                                S.group("pe", [lambda qs=qs: T.transpose(PA.ap[:, qs * 128:(qs + 1) * 128], otmp.ap[:, qs, :], ident.ap)],
                                                        reads=[otmp, ident], writes=[PA])
                                                S.op("dve", lambda qs=qs: V.tensor_scalar(onT.ap[:, h, q0 + qs * 128:q0 + (qs + 1) * 128],
                                                                                          PA.ap[:, qs * 128:(qs + 1) * 128], subg.ap[:, 0:1], None, ALU.mult),
                                                     reads=[PA, subg], writes=[onT])

                                    tasks.append((fS, fE, fPV, fEpi if ki == len(ktiles) - 1 else None))
                        tasks[0][0]()
                        for ti_, (fS, fE, fPV, fEpi) in enumerate(tasks):
                            if ti_ + 1 < len(tasks):
                                tasks[ti_ + 1][0]()
                            fE()
                            fPV()
                            if fEpi is not None:
                                fEpi()
                    for (t0, n) in BLOCKS:
                        v = s if t0 < NL else 2
                        x = xb[it % 2]
                        it += 1
                        S.dma("sp", x.ap[:, :, :n], xT_view(s, t0, n), reads=[xT_b[s]], writes=[x])
                        for fc in range(KC):
                            pp = PA if fc % 2 == 0 else PB
                            S.group("pe", [lambda hh=hh, fc=fc, pp=pp, t0=t0, n=n: T.matmul(pp.ap[:, :n], wo.ap[:, hh, fc * 128:(fc + 1) * 128], onT.ap[:, hh, t0:t0 + n],
                                                                                             start=(hh == 0), stop=(hh == 7)) for hh in range(8)],
                                    reads=[wo, onT], writes=[pp])
                            S.op("dve", lambda fc=fc, pp=pp, x=x, n=n, v=v: V.scalar_tensor_tensor(x.ap[:, fc, :n], pp.ap[:, :n], mod.ap[:, 16 + fc, v:v + 1],
                                                                                                   x.ap[:, fc, :n], ALU.mult, ALU.add), reads=[pp, mod, x], writes=[x])
                        S.dma("act", xT_view(s, t0, n), x.ap[:, :, :n], reads=[x], writes=[xT_b[s]])


        def s5(i):
            j = i // 2
            with_ctx = i < DEPTH - 1
            FB = [(0, 256), (256, 512), (768, 512), (1280, 512), (1792, 512)]
            TWO_PI = 2.0 * math.pi
            PIC = 3.1415925
            with Phase(nc, S, f"s5o{i}") as pho:
                R = norm_resources(pho)
                hF = pho.sb("hF", [128, KC, NT], BF16)
                PSre = pho.ps("PSre", [128, 512])
                PSim = pho.ps("PSim", [128, 512])
                PSy = pho.ps("PSy", [128, 512])
                PT = pho.ps("PT", [128, 512])
                PSre2 = pho.ps("PSre2", [128, 512])
                PSim2 = pho.ps("PSim2", [128, 512])
                PSy2 = pho.ps("PSy2", [128, 512])

                def P(name, shape=(128, 64), dt=F32):
                    return pho.sb(name, list(shape), dt)

                lre, lim, ldt, mag, ang, red, red2, sn, cs = [P(n) for n in ("lre", "lim", "ldt", "mag", "ang", "red", "red2", "sn", "cs")]
                abre, abim, nr, den, cre, cim, ncim, tq = [P(n) for n in ("abre", "abim", "nr", "den", "cre", "cim", "ncim", "tq")]
                ki = P("ki", dt=I32)
                rc = P("rc", (128, 12, 64))
                rsn = P("rsn", (128, 12, 64))
                nrs = P("nrs", (128, 12, 64))
                dsk = P("dsk", (128, KC))
                for (dst, src) in ((lre, ssm_lam_re), (lim, ssm_lam_im)):
                    for d in range(2):
                        S.dma("sp", dst.ap[:, d * 32:(d + 1) * 32], src[j, d].rearrange("g n -> (g n)").rearrange("(gp p) -> p gp", p=128),
                              writes=[dst], allow_slow_non_contiguous=True)
                S.dma("act", dsk.ap, ssm_d[j].rearrange("(k p) -> p k", p=128), writes=[dsk], allow_slow_non_contiguous=True)
                ldrow = P("ldrow", (1, 128))
                S.dma("sp", ldrow.ap, ssm_log_dt[j].rearrange("d g -> (d g)").rearrange("(o f) -> o f", o=1), writes=[ldrow])
                S.group("pe", [lambda: T.matmul(PT.ap[:, 0:128], ones.ap[0:1, :], ldrow.ap, start=True, stop=True)], reads=[ones, ldrow], writes=[PT])
                for g2 in range(2):
                    S.op("dve", lambda g2=g2: V.tensor_copy(
                        ldt.ap[g2 * 64:(g2 + 1) * 64, :].rearrange("p (d g) -> p d g", d=2),
                        PT.ap[g2 * 64:(g2 + 1) * 64, 0:128].rearrange("p (d g two) -> p d g two", d=2, two=2)[:, :, :, g2]),
                        reads=[PT], writes=[ldt])
                S.op("act", lambda: A.activation(ldt.ap, ldt.ap, AF.Exp), reads=[ldt], writes=[ldt])
                S.op("dve", lambda: V.tensor_tensor(mag.ap, lre.ap, ldt.ap, ALU.mult), reads=[lre, ldt], writes=[mag])
                S.op("act", lambda: A.activation(mag.ap, mag.ap, AF.Exp), reads=[mag], writes=[mag])
                S.op("dve", lambda: V.tensor_tensor(ang.ap, lim.ap, ldt.ap, ALU.mult), reads=[lim, ldt], writes=[ang])
                S.op("dve", lambda: V.tensor_scalar(ki.ap, ang.ap, 1.0 / TWO_PI, None, ALU.mult), reads=[ang], writes=[ki])
                S.op("dve", lambda: V.tensor_copy(tq.ap, ki.ap), reads=[ki], writes=[tq])
                S.op("dve", lambda: V.scalar_tensor_tensor(red.ap, tq.ap, -TWO_PI, ang.ap, ALU.mult, ALU.add), reads=[tq, ang], writes=[red])
                S.op("dve", lambda: V.tensor_scalar(red.ap, red.ap, PIC, -PIC, ALU.min, ALU.max), reads=[red], writes=[red])
                S.op("act", lambda: A.activation(sn.ap, red.ap, AF.Sin), reads=[red], writes=[sn])
                S.op("dve", lambda: V.tensor_scalar(tq.ap, red.ap, math.pi / 2, -TWO_PI, ALU.is_gt, ALU.mult), reads=[red], writes=[tq])
                S.op("dve", lambda: V.scalar_tensor_tensor(red2.ap, red.ap, math.pi / 2, tq.ap, ALU.add, ALU.add), reads=[red, tq], writes=[red2])
                S.op("dve", lambda: V.tensor_scalar(red2.ap, red2.ap, PIC, -PIC, ALU.min, ALU.max), reads=[red2], writes=[red2])
                S.op("act", lambda: A.activation(cs.ap, red2.ap, AF.Sin), reads=[red2], writes=[cs])
                S.op("dve", lambda: V.tensor_tensor(abre.ap, mag.ap, cs.ap, ALU.mult), reads=[mag, cs], writes=[abre])
                S.op("dve", lambda: V.tensor_tensor(abim.ap, mag.ap, sn.ap, ALU.mult), reads=[mag, sn], writes=[abim])
                S.op("dve", lambda: V.tensor_scalar(nr.ap, abre.ap, -1.0, None, ALU.add), reads=[abre], writes=[nr])
                S.op("dve", lambda: V.tensor_tensor(den.ap, lre.ap, lre.ap, ALU.mult), reads=[lre], writes=[den])
                S.op("dve", lambda: V.tensor_tensor(tq.ap, lim.ap, lim.ap, ALU.mult), reads=[lim], writes=[tq])
                S.op("dve", lambda: V.tensor_tensor(den.ap, den.ap, tq.ap, ALU.add), reads=[den, tq], writes=[den])
                S.op("dve", lambda: V.reciprocal(den.ap, den.ap), reads=[den], writes=[den])
                S.op("dve", lambda: V.tensor_tensor(cre.ap, nr.ap, lre.ap, ALU.mult), reads=[nr, lre], writes=[cre])
                S.op("dve", lambda: V.tensor_tensor(tq.ap, abim.ap, lim.ap, ALU.mult), reads=[abim, lim], writes=[tq])
                S.op("dve", lambda: V.tensor_tensor(cre.ap, cre.ap, tq.ap, ALU.add), reads=[cre, tq], writes=[cre])
                S.op("dve", lambda: V.tensor_tensor(cre.ap, cre.ap, den.ap, ALU.mult), reads=[cre, den], writes=[cre])
                S.op("dve", lambda: V.tensor_tensor(cim.ap, abim.ap, lre.ap, ALU.mult), reads=[abim, lre], writes=[cim])
                S.op("dve", lambda: V.tensor_tensor(tq.ap, nr.ap, lim.ap, ALU.mult), reads=[nr, lim], writes=[tq])
                S.op("dve", lambda: V.tensor_tensor(cim.ap, cim.ap, tq.ap, ALU.subtract), reads=[cim, tq], writes=[cim])
                S.op("dve", lambda: V.tensor_tensor(cim.ap, cim.ap, den.ap, ALU.mult), reads=[cim, den], writes=[cim])
                S.op("dve", lambda: V.tensor_scalar(ncim.ap, cim.ap, -1.0, None, ALU.mult), reads=[cim], writes=[ncim])
                S.op("dve", lambda: V.tensor_copy(rc.ap[:, 0, :], cs.ap), reads=[cs], writes=[rc])
                S.op("dve", lambda: V.tensor_copy(rsn.ap[:, 0, :], sn.ap), reads=[sn], writes=[rsn])
                for k in range(1, 12):
                    S.op("dve", lambda k=k: V.tensor_tensor(tq.ap, rsn.ap[:, k - 1, :], rsn.ap[:, k - 1, :], ALU.mult), reads=[rsn], writes=[tq])
                    S.op("dve", lambda k=k: V.tensor_tensor(rc.ap[:, k, :], rc.ap[:, k - 1, :], rc.ap[:, k - 1, :], ALU.mult), reads=[rc], writes=[rc])
                    S.op("dve", lambda k=k: V.tensor_tensor(rc.ap[:, k, :], rc.ap[:, k, :], tq.ap, ALU.subtract), reads=[rc, tq], writes=[rc])
                    S.op("dve", lambda k=k: V.scalar_tensor_tensor(rsn.ap[:, k, :], rc.ap[:, k - 1, :], 2.0, rsn.ap[:, k - 1, :], ALU.mult, ALU.mult),
                         reads=[rc, rsn], writes=[rsn])
                S.op("dve", lambda: V.tensor_scalar(nrs.ap, rsn.ap, -1.0, None, ALU.mult), reads=[rsn], writes=[nrs])

                bsrc = [pho.sb(f"bsrc{k}", [128, 64, 16], F32) for k in range(2)]
                csrc = [pho.sb(f"csrc{k}", [128, 64, 16], F32) for k in range(2)]
                for k, (bs_, cs_) in enumerate(((ssm_b_re, ssm_c_re), (ssm_b_im, ssm_c_im))):
                    for d in range(2):
                        S.dma("sp", bsrc[k].ap[:, d * 32:(d + 1) * 32, :],
                              bs_[j, d].rearrange("g n q -> (g n) q").rearrange("(gp p) q -> p gp q", p=128), writes=[bsrc[k]])
                        for g2 in range(2):
                            for gp_ in range(32):
                                S.dma("act" if g2 else "sp", csrc[k].ap[g2 * 64:(g2 + 1) * 64, d * 32 + gp_, :],
                                      cs_[j, d, gp_ * 2 + g2].rearrange("p n -> n p"), writes=[csrc[k]],
                                      allow_slow_non_contiguous=True)

                wB = [Buf(f"s5w{c}", s5w_t[c]) for c in range(64)]
                tB = [Buf(f"s5t{c}", s5tab_t[c]) for c in range(64)]
                with Phase(nc, S, f"s5p{i}") as ph:
                    pad_all = [[ph.sb(f"pad{k}_{r}", [128, 128], F32) for r in range(2)] for k in range(4)]
                    for k in range(4):
                        for r in range(2):
                            S.op("pool", lambda k=k, r=r: G.memset(pad_all[k][r].ap, 0.0), writes=[pad_all[k][r]])
                    bbt_all = ph.rot("bbt", 2, [128, 2, 16], F32)
                    W5_all = ph.rot("W5", 2, [128, 5, 128], BF16)
                    CS_all = ph.rot("CS", 2, [128, 2, 512], F32)
                    tta_all = ph.rot("tta", 2, [128, 256], F32)
                    ttb_all = ph.rot("ttb", 2, [128, 256], F32)
                    pi_ = 0
                    for d in range(2):
                        for gp in range(32):
                            gpl = gp % 4
                            col = d * 32 + gp
                            cc = slice(col, col + 1)
                            r_ = pi_ % 2
                            pi_ += 1
                            bbt, W5, CS, tta, ttb = bbt_all[r_], W5_all[r_], CS_all[r_], tta_all[r_], ttb_all[r_]
                            S.op("pool", lambda W5=W5: G.memset(W5.ap[:, 2:5, :], 0.0), writes=[W5])
                            for k in range(2):
                                pd = pad_all[gpl][k]
                                a_, b_ = (bsrc[0], bsrc[1]) if k == 0 else (bsrc[1], bsrc[0])
                                sc2 = ncim if k == 0 else cim
                                S.op("pool", lambda a_=a_, k=k, col=col, cc=cc, bbt=bbt: G.tensor_scalar(bbt.ap[:, k, :], a_.ap[:, col, :], cre.ap[:, cc], None, ALU.mult),
                                     reads=[a_, cre], writes=[bbt])
                                S.op("dve", lambda b_=b_, k=k, col=col, cc=cc, sc2=sc2, bbt=bbt: V.scalar_tensor_tensor(
                                    bbt.ap[:, k, :], b_.ap[:, col, :], sc2.ap[:, cc], bbt.ap[:, k, :], ALU.mult, ALU.add), reads=[b_, sc2, bbt], writes=[bbt])
                                for g2 in range(2):
                                    blk = (gpl * 2 + g2) * 16
                                    S.op("pool", lambda g2=g2, blk=blk, k=k, pd=pd, bbt=bbt: G.tensor_copy(pd.ap[g2 * 64:(g2 + 1) * 64, blk:blk + 16],
                                                                                                  bbt.ap[g2 * 64:(g2 + 1) * 64, k, :]), reads=[bbt], writes=[pd])
                                S.group("pe", [lambda pd=pd: T.transpose(PT.ap[:, 0:128], pd.ap, ident.ap)], reads=[pd, ident], writes=[PT])
                                S.op("act", lambda k=k, W5=W5: A.activation(W5.ap[:, k, :], PT.ap[:, 0:128], AF.Copy), reads=[PT], writes=[W5])
                            for g2 in range(2):
                                blk = (gpl * 2 + g2) * 16
                                for (kk, src_k, sgn) in ((2, 0, 1.0), (3, 0, -1.0), (4, 1, -1.0)):
                                    S.op("act", lambda g2=g2, blk=blk, kk=kk, src_k=src_k, sgn=sgn, col=col, W5=W5: A.activation(
                                        W5.ap[g2 * 64:(g2 + 1) * 64, kk, blk:blk + 16], csrc[src_k].ap[g2 * 64:(g2 + 1) * 64, col, :],
                                        AF.Copy, scale=sgn), reads=[csrc[src_k]], writes=[W5])
                            Ct_ap, St_ap = CS.ap[:, 0, :], CS.ap[:, 1, :]
                            S.op("pool", lambda Ct_ap=Ct_ap: G.memset(Ct_ap[:, 0:1], 1.0), writes=[CS])
                            S.op("pool", lambda St_ap=St_ap: G.memset(St_ap[:, 0:1], 0.0), writes=[CS])
                            for k in range(9):
                                L = 1 << k
                                ck, sk, nsk = rc.ap[:, k, cc], rsn.ap[:, k, cc], nrs.ap[:, k, cc]
                                S.op("act", lambda L=L, nsk=nsk, tta=tta, St_ap=St_ap: A.activation(tta.ap[:, 0:L], St_ap[:, 0:L], AF.Copy, scale=nsk), reads=[CS, nrs], writes=[tta])
                                S.op("act", lambda L=L, sk=sk, ttb=ttb, Ct_ap=Ct_ap: A.activation(ttb.ap[:, 0:L], Ct_ap[:, 0:L], AF.Copy, scale=sk), reads=[CS, rsn], writes=[ttb])
                                S.op("dve", lambda L=L, ck=ck, tta=tta, Ct_ap=Ct_ap: V.scalar_tensor_tensor(Ct_ap[:, L:2 * L], Ct_ap[:, 0:L], ck, tta.ap[:, 0:L], ALU.mult, ALU.add),
                                     reads=[CS, rc, tta], writes=[CS])
                                S.op("dve", lambda L=L, ck=ck, ttb=ttb, St_ap=St_ap: V.scalar_tensor_tensor(St_ap[:, L:2 * L], St_ap[:, 0:L], ck, ttb.ap[:, 0:L], ALU.mult, ALU.add),
                                     reads=[CS, rc, ttb], writes=[CS])
                            S.dma("sp", s5w_t[col].rearrange("k p f -> p k f"), W5.ap, reads=[W5], writes=[wB[col]])
                            S.dma("act", s5tab_t[col].rearrange("k p f -> p k f"), CS.ap, reads=[CS], writes=[tB[col]])

                for s in range(NSAMP):
                    it = 0
                    for (t0, n) in BLOCKS:
                        pos = t0 + NCX if t0 < NL else 0
                        norm_block(pho, R, s, t0, n, A1, 0, R["sq"], hF.ap[:, :, pos:pos + n], hF, it)
                        it += 1
                    with Phase(nc, S, f"s5s{i}_{s}") as ph:
                        hBk = ph.sb("hBk", [128, NT], BF16)
                        W5_all = ph.rot("W5", 3, [128, 5, 128], BF16)
                        CS_all = ph.rot("CS", 3, [128, 2, 512], F32)
                        PSre_all = [PSre, PSre2]
                        PSim_all = [PSim, PSim2]
                        PSy_all = [PSy, PSy2]
                        wre = ph.rot("wre", 4, [128, 512], F32)
                        wim = ph.rot("wim", 4, [128, 512], F32)
                        prod = [ph.rot(f"prod{k}", 2, [128, 512], BF16) for k in range(4)]
                        ini = ph.rot("ini", 2, [128, 4], F32)
                        yacc = ph.sb("yacc", [128, NT], F32)
                        ga = ph.sb("ga", [128, NT], F32)
                        tm_all = [[ph.sb(f"tm{k}_{r}", [128, 512], F32) for k in range(4)] for r in range(2)]
                        bi = 0
                        pi_ = 0
                        for kc in range(KC):
                            S.op("act", lambda kc=kc: A.activation(hBk.ap[:, 0:NCX], hF.ap[:, kc, 0:NCX][:, ::-1], AF.Copy), reads=[hF], writes=[hBk])
                            S.op("act", lambda kc=kc: A.activation(hBk.ap[:, NCX:NT], hF.ap[:, kc, NCX:NT][:, ::-1], AF.Copy), reads=[hF], writes=[hBk])
                            S.op("pool", lambda: G.memset(yacc.ap, 0.0), writes=[yacc])
                            loads = []
                            tasks = []
                            for d in range(2):
                                for gpl in range(4):
                                    gp = kc * 4 + gpl
                                    col = d * 32 + gp
                                    cc = slice(col, col + 1)
                                    W5, CS = W5_all[pi_ % 3], CS_all[pi_ % 3]
                                    pi_ += 1

                                    def load(W5=W5, CS=CS, col=col):
                                        S.dma("sp", W5.ap, s5w_t[col].rearrange("k p f -> p k f"), reads=[wB[col]], writes=[W5])
                                        S.dma("act", CS.ap, s5tab_t[col].rearrange("k p f -> p k f"), reads=[tB[col]], writes=[CS])
                                    loads.append(load)
                                    prev = None
                                    for (p0, n) in FB:
                                        src = hF.ap[:, kc, p0:p0 + n] if d == 0 else hBk.ap[:, p0:p0 + n]
                                        sb_ = hF if d == 0 else hBk
                                        wr_, wi_ = wre[bi % 4], wim[bi % 4]
                                        pr_ = [prod[k][bi % 2] for k in range(4)]
                                        in_ = ini[bi % 2]
                                        tm = tm_all[bi % 2]
                                        Pre, Pim, Py = PSre_all[bi % 2], PSim_all[bi % 2], PSy_all[bi % 2]
                                        bi += 1
                                        cb, sb2 = CS.ap[:, 0, 0:n], CS.ap[:, 1, 0:n]

                                        def stA(src=src, sb_=sb_, n=n, W5=W5, CS=CS, Pre=Pre, Pim=Pim, tm=tm, wr_=wr_, wi_=wi_, cb=cb, sb2=sb2):
                                            S.group("pe", [lambda: T.matmul(Pre.ap[:, :n], W5.ap[:, 0, :], src, start=True, stop=True)], reads=[W5, sb_], writes=[Pre])
                                            S.group("pe", [lambda: T.matmul(Pim.ap[:, :n], W5.ap[:, 1, :], src, start=True, stop=True)], reads=[W5, sb_], writes=[Pim])
                                            S.op("dve", lambda: V.tensor_tensor(tm[0].ap[:, :n], Pre.ap[:, :n], cb, ALU.mult), reads=[Pre, CS], writes=[tm[0]])
                                            S.op("dve", lambda: V.tensor_tensor(tm[1].ap[:, :n], Pim.ap[:, :n], sb2, ALU.mult), reads=[Pim, CS], writes=[tm[1]])
                                            S.op("pool", lambda: G.tensor_tensor(wr_.ap[:, :n], tm[0].ap[:, :n], tm[1].ap[:, :n], ALU.add), reads=[tm[0], tm[1]], writes=[wr_])
                                            S.op("dve", lambda: V.tensor_tensor(tm[2].ap[:, :n], Pim.ap[:, :n], cb, ALU.mult), reads=[Pim, CS], writes=[tm[2]])
                                            S.op("dve", lambda: V.tensor_tensor(tm[3].ap[:, :n], Pre.ap[:, :n], sb2, ALU.mult), reads=[Pre, CS], writes=[tm[3]])
                                            S.op("pool", lambda: G.tensor_tensor(wi_.ap[:, :n], tm[2].ap[:, :n], tm[3].ap[:, :n], ALU.subtract), reads=[tm[2], tm[3]], writes=[wi_])

                                        def stB(prev=prev, in_=in_, wr_=wr_, wi_=wi_, n=n, cc=cc):
                                            if prev is not None:
                                                (pw_r, pw_i, pn) = prev
                                                lvl = 8 if pn == 256 else 9
                                                cl, sl, nsl = rc.ap[:, lvl, cc], rsn.ap[:, lvl, cc], nrs.ap[:, lvl, cc]
                                                er, ei_ = pw_r.ap[:, pn - 1:pn], pw_i.ap[:, pn - 1:pn]
                                                S.op("act", lambda: A.activation(in_.ap[:, 2:3], ei_, AF.Copy, scale=nsl), reads=[pw_i, nrs], writes=[in_])
                                                S.op("act", lambda: A.activation(in_.ap[:, 3:4], er, AF.Copy, scale=sl), reads=[pw_r, rsn], writes=[in_])
                                                S.op("act", lambda: A.activation(in_.ap[:, 0:1], er, AF.Identity, scale=cl, bias=in_.ap[:, 2:3]),
                                                     reads=[pw_r, rc, in_], writes=[in_])
                                                S.op("act", lambda: A.activation(in_.ap[:, 1:2], ei_, AF.Identity, scale=cl, bias=in_.ap[:, 3:4]),
                                                     reads=[pw_i, rc, in_], writes=[in_])
                                                i_re, i_im = in_.ap[:, 0:1], in_.ap[:, 1:2]
                                                rd = [in_]
                                            else:
                                                i_re, i_im = 0.0, 0.0
                                                rd = []
                                            S.op("dve", lambda: V.tensor_tensor_scan(
                                                wr_.ap[:, :n], mag.ap[:, cc].to_broadcast([128, n]), wr_.ap[:, :n], i_re, ALU.mult, ALU.add), reads=[wr_, mag] + rd, writes=[wr_])
                                            S.op("dve", lambda: V.tensor_tensor_scan(
                                                wi_.ap[:, :n], mag.ap[:, cc].to_broadcast([128, n]), wi_.ap[:, :n], i_im, ALU.mult, ALU.add), reads=[wi_, mag] + rd, writes=[wi_])

                                        def stC(n=n, p0=p0, d=d, W5=W5, CS=CS, wr_=wr_, wi_=wi_, pr_=pr_, Py=Py, cb=cb, sb2=sb2):
                                            S.op("dve", lambda: V.tensor_tensor(pr_[0].ap[:, :n], wr_.ap[:, :n], cb, ALU.mult), reads=[wr_, CS], writes=[pr_[0]])
                                            S.op("pool", lambda: G.tensor_tensor(pr_[1].ap[:, :n], wi_.ap[:, :n], sb2, ALU.mult), reads=[wi_, CS], writes=[pr_[1]])
                                            S.op("pool", lambda: G.tensor_tensor(pr_[2].ap[:, :n], wr_.ap[:, :n], sb2, ALU.mult), reads=[wr_, CS], writes=[pr_[2]])
                                            S.op("dve", lambda: V.tensor_tensor(pr_[3].ap[:, :n], wi_.ap[:, :n], cb, ALU.mult), reads=[wi_, CS], writes=[pr_[3]])
                                            lts = [2, 3, 4, 4]
                                            S.group("pe", [lambda q=q: T.matmul(Py.ap[:, :n], W5.ap[:, lts[q], :], pr_[q].ap[:, :n], start=(q == 0), stop=(q == 3))
                                                           for q in range(4)], reads=[W5] + pr_, writes=[Py])

                                        def stD(n=n, p0=p0, d=d, Py=Py):
                                            if d == 0:
                                                ya = yacc.ap[:, p0:p0 + n]
                                            elif p0 == 0:
                                                ya = yacc.ap[:, 0:NCX][:, ::-1]
                                            else:
                                                hi_ = NT - (p0 - NCX)
                                                ya = yacc.ap[:, hi_ - n:hi_][:, ::-1]
                                            S.op("dve", lambda: V.tensor_tensor(ya, ya, Py.ap[:, :n], ALU.add), reads=[Py, yacc], writes=[yacc])

                                        tasks.append((stA, stB, stC, len(loads) - 1 if p0 == 0 else None, stD))
                                        prev = (wr_, wi_, n)
                            loads[0]()
                            loads[1]()
                            tasks[0][0]()
                            tasks[1][0]()
                            for ti_, (stA, stB, stC, li, stD) in enumerate(tasks):
                                if li is not None and li + 2 < len(loads):
                                    loads[li + 2]()
                                stB()
                                if ti_ + 2 < len(tasks):
                                    tasks[ti_ + 2][0]()
                                if ti_ >= 1:
                                    tasks[ti_ - 1][4]()
                                stC()
                            tasks[-1][4]()
                            S.op("dve", lambda kc=kc: V.scalar_tensor_tensor(yacc.ap, hF.ap[:, kc, :], dsk.ap[:, kc:kc + 1], yacc.ap, ALU.mult, ALU.add),
                                 reads=[hF, dsk, yacc], writes=[yacc])
                            S.op("act", lambda: A.activation(ga.ap, yacc.ap, AF.Square), reads=[yacc], writes=[ga])
                            S.op("pool", lambda: G.tensor_scalar(ga.ap, ga.ap, 0.044715, 1.0, ALU.mult, ALU.add), reads=[ga], writes=[ga])
                            S.op("pool", lambda: G.tensor_tensor(ga.ap, ga.ap, yacc.ap, ALU.mult), reads=[ga, yacc], writes=[ga])
                            S.op("act", lambda: A.activation(ga.ap, ga.ap, AF.Tanh, scale=0.7978845608028654), reads=[ga], writes=[ga])
                            S.op("dve", lambda: V.scalar_tensor_tensor(ga.ap, ga.ap, 1.0, yacc.ap, ALU.add, ALU.mult), reads=[ga, yacc], writes=[ga])
                            S.op("act", lambda kc=kc: A.activation(hF.ap[:, kc, :], ga.ap, AF.Copy, scale=0.5), reads=[ga], writes=[hF])
                    with Phase(nc, S, f"s5g{i}_{s}") as ph:
                        w1 = ph.sb("w1", [128, KC, D], BF16)
                        w2 = ph.sb("w2", [128, KC, D], BF16)
                        S.dma("pool", w1.ap, ssm_w_glu1[j].rearrange("(k p) f -> p k f", p=128), writes=[w1])
                        S.dma("pool", w2.ap, ssm_w_glu2[j].rearrange("(k p) f -> p k f", p=128), writes=[w2])
                        sg_ = ph.rot("sg", 2, [128, 512], F32)
                        xb = R["xb"]
                        for bi, (p0, n) in enumerate(FB):
                            if p0 == 0 and not with_ctx:
                                continue
                            t0 = NL if p0 == 0 else p0 - NCX
                            v = 2 if p0 == 0 else s
                            x = xb[bi % 2]
                            S.dma("sp", x.ap[:, :, :n], xT_view(s, t0, n), reads=[xT_b[s]], writes=[x])
                            for fc in range(KC):
                                fs = slice(fc * 128, (fc + 1) * 128)
                                S.group("pe", [lambda kc=kc, fs=fs, p0=p0, n=n: T.matmul(PSre.ap[:, :n], w1.ap[:, kc, fs], hF.ap[:, kc, p0:p0 + n],
                                                                                         start=(kc == 0), stop=(kc == KC - 1)) for kc in range(KC)], reads=[w1, hF], writes=[PSre])
                                S.group("pe", [lambda kc=kc, fs=fs, p0=p0, n=n: T.matmul(PSim.ap[:, :n], w2.ap[:, kc, fs], hF.ap[:, kc, p0:p0 + n],
                                                                                         start=(kc == 0), stop=(kc == KC - 1)) for kc in range(KC)], reads=[w2, hF], writes=[PSim])
                                g_ = sg_[fc % 2]
                                S.op("act", lambda g_=g_, n=n: A.activation(g_.ap[:, :n], PSim.ap[:, :n], AF.Sigmoid), reads=[PSim], writes=[g_])
                                S.op("dve", lambda g_=g_, n=n: V.tensor_tensor(g_.ap[:, :n], g_.ap[:, :n], PSre.ap[:, :n], ALU.mult), reads=[g_, PSre], writes=[g_])
                                S.op("dve", lambda g_=g_, n=n, fc=fc, x=x, v=v: V.scalar_tensor_tensor(x.ap[:, fc, :n], g_.ap[:, :n], mod.ap[:, 16 + fc, v:v + 1], x.ap[:, fc, :n],
                                                                                                       ALU.mult, ALU.add), reads=[g_, mod, x], writes=[x])
                            S.dma("act", xT_view(s, t0, n), x.ap[:, :, :n], reads=[x], writes=[xT_b[s]])

        MIXERS = {0: attn, 1: s5}
        cur = -1
        for (i, part) in cfg:
            if i != cur:
                adaln(i)
                cur = i
            if part == "mix":
                MIXERS[i % 2](i)
            else:
                moe_full(i)
        final()
        S.finish()
    return nc, S


_CONST = {}


def _consts():
    if not _CONST:
        _CONST["k_ident"] = np.eye(128, dtype=np.float32)
        t = np.arange(NL)
        row = (t // 64).astype(np.float32)
        col = (t % 64).astype(np.float32)
        inv = (10000.0 ** (-np.arange(16, dtype=np.float32) / 16)).astype(np.float32)
        C = np.zeros((128, NL), np.float32)
        Sg = np.zeros((128, NL), np.float32)
        for p in range(128):
            dd = p % 64
            pos = row if dd < 32 else col
            ang = pos * inv[dd % 16]
            C[p] = np.cos(ang)
            Sg[p] = np.sin(ang) * (-1.0 if (dd % 32) < 16 else 1.0)
        _CONST["k_ropec"] = C
        _CONST["k_ropes"] = Sg
    return _CONST


_NC_CACHE = {}


def kernel(**inputs):
    n = 8
    if "nc" not in _NC_CACHE:
        _NC_CACHE["nc"] = build()[0]
    nc = _NC_CACHE["nc"]
    shared = {k: np.ascontiguousarray(v) for k, v in inputs.items() if k not in ("x", "c", "ctx")}
    shared.update(_consts())
    in_maps = []
    for r in range(n):
        m = dict(shared)
        m["x"] = np.ascontiguousarray(inputs["x"][2 * r:2 * r + 2])
        m["c"] = np.ascontiguousarray(inputs["c"][2 * r:2 * r + 2])
        m["ctx"] = np.ascontiguousarray(inputs["ctx"][2 * r:2 * r + 2])
        in_maps.append(m)
    res = run_bass_kernel_spmd(nc, in_maps, core_ids=list(range(n)))
    return np.concatenate([r["out"] for r in res.results], axis=0).astype(np.float32)
```

```python
import math
from contextlib import ExitStack
import numpy as np
import ml_dtypes
import concourse.bass as bass
import concourse.mybir as mybir
from concourse.bass_utils import run_bass_kernel_spmd

F32 = mybir.dt.float32
BF16 = mybir.dt.bfloat16
U32 = mybir.dt.uint32
I32 = mybir.dt.int32
AF = mybir.ActivationFunctionType
ALU = mybir.AluOpType

D = 1024
NL = 2048
NCX = 256
NT = NL + NCX
KC = 8
NE = 16
FF = 2048
DEPTH = 4
EPS = 1e-6
NSAMP = 2
CAPL = 256
CAPC = 32
NCOL = 2 * CAPL + 2 * CAPC


class Buf:
    __slots__ = ("name", "ap", "writer", "readers")

    def __init__(self, name, ap):
        self.name = name
        self.ap = ap
        self.writer = None
        self.readers = []


class Sync:
    NDMA = 6

    def __init__(self, nc):
        self.nc = nc
        self.engs = {"pe": nc.tensor, "act": nc.scalar, "dve": nc.vector, "pool": nc.gpsimd, "sp": nc.sync}
        self.sem = {k: nc.alloc_semaphore(name=f"s_{k}") for k in self.engs}
        self.cnt = {k: 0 for k in self.engs}
        self.seen = {k: {} for k in self.engs}
        self.dsem = {k: [nc.alloc_semaphore(name=f"d_{k}{i}") for i in range(self.NDMA)] for k in ("sp", "act", "pool")}
        self.dcnt = {k: 0 for k in self.dsem}
        self.dlast = {k: [None] * self.NDMA for k in self.dsem}
        self.out_tickets = []
        self.ninstr = 0

    def _wait(self, e, tick):
        if tick is None:
            return
        sem, val = tick
        key = sem.name
        if e == "pe" and key == "s_pe":
            return
        if self.seen[e].get(key, 0) >= val:
            return
        self.engs[e].wait_ge(sem, val)
        self.seen[e][key] = val

    def deps(self, e, reads, writes):
        for b in reads:
            self._wait(e, b.writer)
        for b in writes:
            self._wait(e, b.writer)
            for r in b.readers:
                self._wait(e, r)

    def commit(self, tick, reads, writes):
        for b in reads:
            b.readers.append(tick)
            if len(b.readers) > 48:
                last = {}
                for t in b.readers:
                    last[t[0].name] = t
                b.readers = list(last.values())
        for b in writes:
            b.writer = tick
            b.readers = []

    def op(self, e, fn, reads=(), writes=()):
        self.deps(e, reads, writes)
        ins = fn()
        self.cnt[e] += 1
        ins.then_inc(self.sem[e], 1)
        tick = (self.sem[e], self.cnt[e])
        self.commit(tick, reads, writes)
        self.ninstr += 1
        return tick

    def group(self, e, fns, reads=(), writes=()):
        self.deps(e, reads, writes)
        ins = None
        for fn in fns:
            ins = fn()
            self.ninstr += 1
        self.cnt[e] += 1
        ins.then_inc(self.sem[e], 1)
        tick = (self.sem[e], self.cnt[e])
        self.commit(tick, reads, writes)
        return tick

    def _dma_common(self, q, issue, reads, writes, is_output):
        j = self.dcnt[q]
        slot = j % self.NDMA
        self._wait(q, self.dlast[q][slot])
        self.deps(q, reads, writes)
        sem = self.dsem[q][slot]
        val = 16 * (j // self.NDMA + 1)
        issue().then_inc(sem, 16)
        self.dcnt[q] += 1
        tick = (sem, val)
        self.dlast[q][slot] = tick
        self.commit(tick, reads, writes)
        if is_output:
            self.out_tickets.append(tick)
        self.ninstr += 1
        return tick

    def dma(self, q, out_ap, in_ap, reads=(), writes=(), is_output=False, **kw):
        return self._dma_common(q, lambda: self.engs[q].dma_start(out=out_ap, in_=in_ap, **kw), reads, writes, is_output)

    def idma(self, reads=(), writes=(), **kw):
        return self._dma_common("pool", lambda: self.nc.gpsimd.indirect_dma_start(**kw), reads, writes, False)

    def barrier(self):
        ticks = [(self.sem[e], self.cnt[e]) for e in self.engs if self.cnt[e] > 0]
        for q in self.dsem:
            ticks += [t for t in self.dlast[q] if t is not None]
        for e in self.engs:
            for t in ticks:
                self._wait(e, t)

    def finish(self):
        for q in self.dsem:
            for t in self.dlast[q]:
                self._wait(q, t)
        for t in self.out_tickets:
            self._wait("sp", t)


class Phase:
    def __init__(self, nc, S, name):
        self.nc, self.S, self.name = nc, S, name
        self.stack = ExitStack()
        self.n = 0

    def __enter__(self):
        self.stack.__enter__()
        return self

    def __exit__(self, *a):
        self.S.barrier()
        return self.stack.__exit__(*a)

    def sb(self, name, shape, dt):
        self.n += 1
        t = self.stack.enter_context(self.nc.sbuf_tensor(f"{self.name}_{name}_{self.n}", list(shape), dt))
        return Buf(name, t.ap())

    def ps(self, name, shape, dt=F32):
        self.n += 1
        t = self.stack.enter_context(self.nc.psum_tensor(f"{self.name}_{name}_{self.n}", list(shape), dt))
        return Buf(name, t.ap())

    def rot(self, name, n, shape, dt):
        return [self.sb(f"{name}{i}", shape, dt) for i in range(n)]


def build(cfg=None, dbg=False):
    if cfg is None:
        cfg = [(i, p) for i in range(DEPTH) for p in ("mix", "moe")]
    nc = bass.Bass("TRN2", target_bir_lowering=False)

    def din(name, shape, dt=F32):
        return nc.dram_tensor(name, list(shape), dt, kind="ExternalInput").ap()

    x_in = din("x", [NSAMP, NL, D])
    c_in = din("c", [NSAMP, D])
    ctx_in = din("ctx", [NSAMP, NCX, D])
    cctx_in = din("c_ctx", [D])
    ada_w = din("ada_w", [DEPTH, D, 6 * D])
    ada_b = din("ada_b", [DEPTH, 6 * D])
    norm1_g = din("norm1_g", [DEPTH, D])
    norm2_g = din("norm2_g", [DEPTH, D])
    final_g = din("final_g", [D])
    attn_w_qkv = din("attn_w_qkv", [2, D, 3 * D])
    attn_w_o = din("attn_w_o", [2, D, D])
    attn_lam = [din(n, [2, 64]) for n in ("attn_lam_q1", "attn_lam_k1", "attn_lam_q2", "attn_lam_k2")]
    attn_subln_g = din("attn_subln_g", [2, 128])
    ssm_lam_re = din("ssm_lam_re", [2, 2, 64, 64])
    ssm_lam_im = din("ssm_lam_im", [2, 2, 64, 64])
    ssm_log_dt = din("ssm_log_dt", [2, 2, 64])
    ssm_b_re = din("ssm_b_re", [2, 2, 64, 64, 16])
    ssm_b_im = din("ssm_b_im", [2, 2, 64, 64, 16])
    ssm_c_re = din("ssm_c_re", [2, 2, 64, 16, 64])
    ssm_c_im = din("ssm_c_im", [2, 2, 64, 16, 64])
    ssm_d = din("ssm_d", [2, D])
    ssm_w_glu1 = din("ssm_w_glu1", [2, D, D])
    ssm_w_glu2 = din("ssm_w_glu2", [2, D, D])
    moe_w_router = din("moe_w_router", [DEPTH, D, NE])
    moe_b_router = din("moe_b_router", [DEPTH, NE])
    moe_w_gate = din("moe_w_gate", [DEPTH, NE, D, FF])
    moe_w_up = din("moe_w_up", [DEPTH, NE, D, FF])
    moe_w_down = din("moe_w_down", [DEPTH, NE, FF, D])
    ident_in = din("k_ident", [128, 128])
    ropec_in = din("k_ropec", [128, NL])
    ropes_in = din("k_ropes", [128, NL])
    out_d = nc.dram_tensor("out", [NSAMP, NL, D], F32, kind="ExternalOutput").ap()

    xT_t = nc.dram_tensor("xT_scr", [NSAMP, KC, 128, NT], F32, kind="ExternalOutput" if dbg else "Internal").ap()
    h2tok_t = nc.dram_tensor("h2tok_scr", [NSAMP, NT, D], BF16, kind="Internal").ap()
    macc_t = nc.dram_tensor("macc_scr", [NSAMP, NT, D], F32, kind="Internal").ap()
    s5w_t = nc.dram_tensor("s5w_scr", [64, 5, 128, 128], BF16, kind="Internal").ap()
    s5tab_t = nc.dram_tensor("s5tab_scr", [64, 2, 128, 512], F32, kind="Internal").ap()

    S = Sync(nc)
    xT_b = [Buf(f"xT{s}", xT_t[s]) for s in range(NSAMP)]
    h2l_b = [Buf(f"h2l{s}", h2tok_t[s, 0:NL, :]) for s in range(NSAMP)]
    h2c_b = [Buf(f"h2c{s}", h2tok_t[s, NL:NT, :]) for s in range(NSAMP)]
    mal_b = [Buf(f"mal{s}", macc_t[s, 0:NL, :]) for s in range(NSAMP)]
    mac_b = [Buf(f"mac{s}", macc_t[s, NL:NT, :]) for s in range(NSAMP)]

    def xT_view(s, t0, n):
        return xT_t[s].rearrange("k p t -> p k t")[:, :, t0:t0 + n]

    BLOCKS = [(0, 512), (512, 512), (1024, 512), (1536, 512), (2048, 256)]

    V, T, G, A = nc.vector, nc.tensor, nc.gpsimd, nc.scalar

    with ExitStack() as gstack:
        def gsb(name, shape, dt):
            t = gstack.enter_context(nc.sbuf_tensor("g_" + name, list(shape), dt))
            return Buf(name, t.ap())

        ident = gsb("ident", [128, 128], F32)
        identb = gsb("identb", [128, 128], BF16)
        ones = gsb("ones", [128, 128], F32)
        zeros = gsb("zeros", [128, 1024], F32)
        scT = gsb("scT", [128, KC, 4], F32)
        mod = gsb("mod", [128, 48, 4], F32)
        A1 = gsb("A1", [128, KC, 4], F32)
        A2 = gsb("A2", [128, KC, 4], F32)
        n1g = gsb("n1g", [128, KC], F32)
        n2g = gsb("n2g", [128, KC], F32)
        adab = gsb("adab", [128, 48], F32)

        with Phase(nc, S, "p0") as ph:
            S.dma("sp", ident.ap, ident_in, writes=[ident])
            S.op("dve", lambda: V.tensor_copy(identb.ap, ident.ap), reads=[ident], writes=[identb])
            S.op("pool", lambda: G.memset(ones.ap, 1.0), writes=[ones])
            S.op("pool", lambda: G.memset(zeros.ap, 0.0), writes=[zeros])
            S.op("pool", lambda: G.memset(scT.ap, 0.0), writes=[scT])
            craw = ph.sb("craw", [128, KC, 4], F32)
            S.op("pool", lambda: G.memset(craw.ap, 0.0), writes=[craw])
            for v in range(3):
                src = c_in[v] if v < 2 else cctx_in
                S.dma("sp", craw.ap[:, :, v], src.rearrange("(k p) -> p k", p=128), writes=[craw],
                      allow_slow_non_contiguous=True)
            S.op("act", lambda: A.activation(scT.ap, craw.ap, AF.Silu), reads=[craw], writes=[scT])
            xin = ph.rot("xin", 2, [128, D], F32)
            xtt = ph.rot("xtt", 2, [128, KC, 128], F32)
            pst = [ph.ps("pst0", [128, 512]), ph.ps("pst1", [128, 512])]
            it = 0
            for s in range(NSAMP):
                for tt in range(NT // 128):
                    src = x_in[s, tt * 128:(tt + 1) * 128, :] if tt < 16 else ctx_in[s, (tt - 16) * 128:(tt - 15) * 128, :]
                    xi = xin[it % 2]
                    xo = xtt[it % 2]
                    S.dma("sp" if it % 2 == 0 else "act", xi.ap, src, writes=[xi])
                    for half in range(2):
                        p = pst[half]
                        S.group("pe", [lambda j=j, p=p, xi=xi, half=half: T.transpose(
                            p.ap[:, j * 128:(j + 1) * 128], xi.ap[:, (half * 4 + j) * 128:(half * 4 + j + 1) * 128], ident.ap)
                            for j in range(4)], reads=[xi, ident], writes=[p])
                        S.op("dve" if half == 0 else "act",
                             (lambda p=p, xo=xo, half=half: V.tensor_copy(
                                 xo.ap[:, half * 4:half * 4 + 4, :], p.ap.rearrange("p (k t) -> p k t", k=4))) if half == 0 else
                             (lambda p=p, xo=xo, half=half: A.activation(
                                 xo.ap[:, half * 4:half * 4 + 4, :], p.ap.rearrange("p (k t) -> p k t", k=4), AF.Copy)),
                             reads=[p], writes=[xo])
                    S.dma("sp" if it % 2 == 1 else "act", xT_view(s, tt * 128, 128), xo.ap, reads=[xo], writes=[xT_b[s]])
                    it += 1

        def adaln(i):
            with Phase(nc, S, f"ada{i}") as ph:
                S.dma("sp", adab.ap, ada_b[i].rearrange("(j p) -> p j", p=128), writes=[adab], allow_slow_non_contiguous=True)
                S.dma("act", n1g.ap, norm1_g[i].rearrange("(k p) -> p k", p=128), writes=[n1g], allow_slow_non_contiguous=True)
                S.dma("act", n2g.ap, norm2_g[i].rearrange("(k p) -> p k", p=128), writes=[n2g], allow_slow_non_contiguous=True)
                wst = ph.rot("wst", 2, [128, KC, 512], F32)
                pm = ph.ps("pm", [128, 48, 4])
                for jb in range(12):
                    w = wst[jb % 2]
                    S.dma("sp" if jb % 2 == 0 else "act", w.ap,
                          ada_w[i][:, jb * 512:(jb + 1) * 512].rearrange("(k p) f -> p k f", p=128), writes=[w])
                    for fs in range(4):
                        j = jb * 4 + fs
                        S.group("pe", [lambda kc=kc, j=j, fs=fs, w=w: T.matmul(
                            pm.ap[:, j, :], w.ap[:, kc, fs * 128:(fs + 1) * 128], scT.ap[:, kc, :], start=(kc == 0), stop=(kc == KC - 1))
                            for kc in range(KC)], reads=[w, scT], writes=[pm])
                for v in range(3):
                    S.op("dve", lambda v=v: V.tensor_tensor(mod.ap[:, :, v], pm.ap[:, :, v], adab.ap, ALU.add),
                         reads=[pm, adab], writes=[mod])
                for v in range(3):
                    S.op("dve", lambda v=v: V.scalar_tensor_tensor(A1.ap[:, :, v], mod.ap[:, 8:16, v], 1.0, n1g.ap, ALU.add, ALU.mult),
                         reads=[mod, n1g], writes=[A1])
                    S.op("dve", lambda v=v: V.scalar_tensor_tensor(A2.ap[:, :, v], mod.ap[:, 32:40, v], 1.0, n2g.ap, ALU.add, ALU.mult),
                         reads=[mod, n2g], writes=[A2])

        def norm_block(ph, R, s, t0, n, Acoef, shbase, h32, hb_ap, hb_buf, it):
            v = s if t0 < NL else 2
            xb = R["xb"][it % 2]
            sq = R["sq"]
            S.dma("sp" if it % 2 == 0 else "act", xb.ap[:, :, :n], xT_view(s, t0, n), reads=[xT_b[s]], writes=[xb])
            S.op("act", lambda: A.activation(sq.ap[:, :, :n], xb.ap[:, :, :n], AF.Square), reads=[xb], writes=[sq])
            ssp = R["ssp"]
            S.group("pe", [lambda kc=kc: T.matmul(ssp.ap[:, :n], ones.ap, sq.ap[:, kc, :n], start=(kc == 0), stop=(kc == KC - 1))
                           for kc in range(KC)], reads=[ones, sq], writes=[ssp])
            rstd = R["rstd"]
            S.op("act", lambda: A.activation(rstd.ap[:, :n], ssp.ap[:, :n], AF.Sqrt, bias=R["eps"].ap, scale=1.0 / D),
                 reads=[ssp, R["eps"]], writes=[rstd])
            S.op("dve", lambda: V.reciprocal(rstd.ap[:, :n], rstd.ap[:, :n]), reads=[rstd], writes=[rstd])
            for kc in range(KC):
                S.op("dve", lambda kc=kc: V.tensor_tensor(sq.ap[:, kc, :n], xb.ap[:, kc, :n], rstd.ap[:, :n], ALU.mult),
                     reads=[xb, rstd], writes=[sq])
            for kc in range(KC):
                S.op("pool", lambda kc=kc: G.tensor_scalar(h32.ap[:, kc, :n], sq.ap[:, kc, :n], Acoef.ap[:, kc, v:v + 1],
                                                           mod.ap[:, shbase + kc, v:v + 1], ALU.mult, ALU.add),
                     reads=[sq, Acoef, mod], writes=[h32])
            S.op("act", lambda: A.activation(hb_ap, h32.ap[:, :, :n], AF.Copy), reads=[h32], writes=[hb_buf])

        def norm_resources(ph, ssp=None):
            R = {"xb": ph.rot("xb", 2, [128, KC, 512], F32), "sq": ph.sb("sq", [128, KC, 512], F32),
                 "ssp": ssp if ssp is not None else ph.ps("ssp", [128, 512]), "rstd": ph.sb("rstd", [128, 512], F32), "eps": ph.sb("eps", [128, 1], F32)}
            S.op("pool", lambda: G.memset(R["eps"].ap, EPS), writes=[R["eps"]])
            return R

        def moe_full(i):
            with_ctx = i < DEPTH - 1
            blocks = BLOCKS if with_ctx else BLOCKS[:4]
            with Phase(nc, S, f"moeO{i}") as pho:
                idxT = [pho.sb(f"idxT{s}", [128, 3, NE], U32) for s in range(NSAMP)]
                valT = [pho.sb(f"valT{s}", [128, 3, NE], F32) for s in range(NSAMP)]
                for s in range(NSAMP):
                    S.op("pool", lambda s=s: G.memset(idxT[s].ap, 0), writes=[idxT[s]])
                    S.op("pool", lambda s=s: G.memset(valT[s].ap, 0.0), writes=[valT[s]])
                with Phase(nc, S, f"moeR{i}") as ph:
                    R = norm_resources(ph)
                    wr = ph.sb("wr", [128, KC, NE], F32)
                    br = ph.sb("br", [NE, 1], F32)
                    S.dma("sp", wr.ap, moe_w_router[i].rearrange("(k p) e -> p k e", p=128), writes=[wr])
                    S.dma("sp", br.ap, moe_b_router[i].rearrange("(e o) -> e o", o=1), writes=[br])
                    zi = 0
                    for s in range(NSAMP):
                        for tt in range(NT // 128 if with_ctx else NL // 128):
                            S.dma("sp" if zi % 2 == 0 else "act", macc_t[s, tt * 128:(tt + 1) * 128, :], zeros.ap,
                                  reads=[zeros], writes=[mal_b[s] if tt < 16 else mac_b[s]])
                            zi += 1
                    h32 = ph.sb("h32", [128, KC, 512], F32)
                    hb = ph.rot("hb", 2, [128, KC, 512], BF16)
                    expT = [ph.sb(f"expT{s}", [NE, NT], F32) for s in range(NSAMP)]
                    aff = [ph.sb(f"aff{s}", [NE, NT], F32) for s in range(NSAMP)]
                    lgp = ph.ps("lgp", [128, 512])
                    smp = ph.ps("smp", [128, 512])
                    rs = ph.sb("rs", [NE, 512], F32)
                    ptb = ph.ps("ptb", [128, 512])
                    ptb_bf = ptb.ap.bitcast(BF16)
                    tok = ph.rot("tok", 2, [128, D], BF16)
                    it = 0
                    ti = 0
                    for s in range(NSAMP):
                        for (t0, n) in blocks:
                            hbb = hb[it % 2]
                            norm_block(ph, R, s, t0, n, A2, 24, h32, hbb.ap[:, :, :n], hbb, it)
                            S.group("pe", [lambda kc=kc, n=n: T.matmul(lgp.ap[0:NE, :n], wr.ap[:, kc, :], h32.ap[:, kc, :n],
                                                                         start=(kc == 0), stop=(kc == KC - 1)) for kc in range(KC)],
                                    reads=[wr, h32], writes=[lgp])
                            S.op("act", lambda s=s, t0=t0, n=n: A.activation(expT[s].ap[:, t0:t0 + n], lgp.ap[0:NE, :n], AF.Exp, bias=br.ap),
                                 reads=[lgp, br], writes=[expT[s]])
                            S.group("pe", [lambda s=s, t0=t0, n=n: T.matmul(smp.ap[0:NE, :n], ones.ap[0:NE, 0:NE], expT[s].ap[:, t0:t0 + n],
                                                                          start=True, stop=True)], reads=[ones, expT[s]], writes=[smp])
                            S.op("dve", lambda n=n: V.reciprocal(rs.ap[:, :n], smp.ap[0:NE, :n]), reads=[smp], writes=[rs])
                            S.op("dve", lambda s=s, t0=t0, n=n: V.tensor_tensor(aff[s].ap[:, t0:t0 + n], expT[s].ap[:, t0:t0 + n], rs.ap[:, :n], ALU.mult),
                                 reads=[expT[s], rs], writes=[aff[s]])
                            for tt in range(n // 128):
                                tk = tok[ti % 2]
                                S.group("pe", [lambda kc=kc, tt=tt, hbb=hbb: T.transpose(
                                    ptb_bf[:, kc * 128:(kc + 1) * 128], hbb.ap[:, kc, tt * 128:(tt + 1) * 128], identb.ap) for kc in range(KC)],
                                    reads=[hbb, identb], writes=[ptb])
                                S.op("dve", lambda tk=tk: V.tensor_copy(tk.ap, ptb_bf), reads=[ptb], writes=[tk])
                                r0 = t0 + tt * 128
                                S.dma("sp" if ti % 2 == 0 else "act", h2tok_t[s, r0:r0 + 128, :], tk.ap, reads=[tk],
                                      writes=[h2l_b[s] if r0 < NL else h2c_b[s]])
                                ti += 1
                            it += 1
                    vals = [ph.sb(f"vals{s}", [NE, CAPL + CAPC], F32) for s in range(NSAMP)]
                    idx = [ph.sb(f"idx{s}", [NE, CAPL + CAPC], U32) for s in range(NSAMP)]
                    idxf = [ph.sb(f"idxf{s}", [NE, CAPL + CAPC], F32) for s in range(NSAMP)]
                    segs = [(0, NL, 0, CAPL // 8)] + ([(NL, NCX, CAPL, CAPC // 8)] if with_ctx else [])
                    for (a0, an, c0, rounds) in segs:
                        for r in range(rounds):
                            for s in range(NSAMP):
                                av = aff[s].ap[:, a0:a0 + an]
                                vv = vals[s].ap[:, c0 + r * 8:c0 + r * 8 + 8]
                                S.op("dve", lambda av=av, vv=vv: V.max(vv, av), reads=[aff[s]], writes=[vals[s]])
                                S.op("dve", lambda av=av, vv=vv, s=s, c0=c0, r=r: V.max_index(idx[s].ap[:, c0 + r * 8:c0 + r * 8 + 8], vv, av),
                                     reads=[aff[s], vals[s]], writes=[idx[s]])
                                S.op("dve", lambda av=av, vv=vv: V.match_replace(av, vv, av, -1.0), reads=[vals[s]], writes=[aff[s]])
                    ncap = CAPL + (CAPC if with_ctx else 0)
                    for s in range(NSAMP):
                        S.op("dve", lambda s=s: V.tensor_copy(idxf[s].ap[:, :ncap], idx[s].ap[:, :ncap]), reads=[idx[s]], writes=[idxf[s]])
                        pieces = [(0, 128, 0), (128, 128, 1)] + ([(256, 32, 2)] if with_ctx else [])
                        for (c0, cn, slot) in pieces:
                            S.group("pe", [lambda s=s, c0=c0, cn=cn: T.transpose(lgp.ap[0:cn, 0:NE], idxf[s].ap[:, c0:c0 + cn], ident.ap[0:NE, 0:NE])],
                                    reads=[idxf[s], ident], writes=[lgp])
                            S.op("dve", lambda s=s, cn=cn, slot=slot: V.tensor_copy(idxT[s].ap[0:cn, slot, :], lgp.ap[0:cn, 0:NE]),
                                 reads=[lgp], writes=[idxT[s]])
                            S.group("pe", [lambda s=s, c0=c0, cn=cn: T.transpose(smp.ap[0:cn, 0:NE], vals[s].ap[:, c0:c0 + cn], ident.ap[0:NE, 0:NE])],
                                    reads=[vals[s], ident], writes=[smp])
                            S.op("dve", lambda s=s, cn=cn, slot=slot: V.tensor_copy(valT[s].ap[0:cn, slot, :], smp.ap[0:cn, 0:NE]),
                                 reads=[smp], writes=[valT[s]])

                with Phase(nc, S, f"moeE{i}") as ph:
                    xg = ph.rot("xg", 4, [128, D], BF16)
                    xinT = ph.rot("xinT", 2, [128, KC, NCOL], BF16)
                    wgs = ph.rot("wgs", 3, [128, KC, 256], F32)
                    wus = ph.rot("wus", 3, [128, KC, 256], F32)
                    wgb = ph.rot("wgb", 2, [128, KC, 256], BF16)
                    wub = ph.rot("wub", 2, [128, KC, 256], BF16)
                    wds = ph.rot("wds", 2, [128, 2, D], F32)
                    wdb = ph.sb("wdb", [128, 16, D], BF16)
                    hidT = ph.sb("hidT", [128, 16, NCOL], BF16)
                    sg = ph.rot("sg", 2, [128, NCOL], F32)
                    yT = ph.sb("yT", [128, KC, NCOL], F32)
                    yo = ph.rot("yo", 4, [128, D], F32)
                    Gl = [ph.ps(f"Gl{k}", [128, 512]) for k in range(2)]
                    Ul = [ph.ps(f"Ul{k}", [128, 512]) for k in range(2)]
                    GUc = ph.ps("GUc", [128, 512])
                    Yl2 = [ph.ps(f"Yl{k}", [128, 512]) for k in range(2)]
                    Yc = Buf("Yc", GUc.ap[:, 128:256])
                    ptr = ph.ps("ptr", [128, 512])
                    ptr_bf = ptr.ap.bitcast(BF16)
                    nctx = 2 * CAPC if with_ctx else 0
                    gi = 0
                    wi = 0
                    di = 0
                    fi = 0
                    yi = 0
                    gl = []
                    for s in range(NSAMP):
                        for hf in range(2):
                            gl.append((s, hf, 128, s * CAPL + hf * 128, None, h2l_b[s]))
                    if with_ctx:
                        for s in range(NSAMP):
                            gl.append((s, 2, CAPC, 2 * CAPL + s * CAPC, None, h2c_b[s]))
                    gctr = [0]

                    def gather(e):
                        xt = xinT[e % 2]
                        for (s, slot, cn, col0, src, srcb) in gl:
                            g = xg[gctr[0] % 4]
                            gctr[0] += 1
                            S.idma(out=g.ap[0:cn, :], out_offset=None, in_=h2tok_t.rearrange("s t d -> (s t) d"),
                                   in_offset=bass.IndirectOffsetOnAxis(ap=idxT[s].ap[0:cn, slot, e:e + 1], axis=0),
                                   element_offset=(s * NT + (0 if slot < 2 else NL)) * D,
                                   reads=[idxT[s], srcb], writes=[g])
                            S.group("pe", [lambda kc=kc, g=g, cn=cn: T.transpose(
                                ptr_bf[:, kc * 128:kc * 128 + cn], g.ap[0:cn, kc * 128:(kc + 1) * 128], identb.ap[0:cn, 0:cn]) for kc in range(KC)],
                                reads=[g, identb], writes=[ptr])
                            S.op("dve", lambda xt=xt, col0=col0, cn=cn: V.tensor_copy(
                                xt.ap[:, :, col0:col0 + cn], ptr_bf.rearrange("p (k c) -> p k c", k=KC)[:, :, 0:cn]),
                                reads=[ptr], writes=[xt])

                    gather(0)
                    sc_cur = {}
                    sc_all = []
                    for e in range(NE):
                        xt = xinT[e % 2]
                        def prep(k):
                            e_, p_ = divmod(k, 8)
                            ws_g, ws_u, wb_g, wb_u = wgs[k % 3], wus[k % 3], wgb[k % 2], wub[k % 2]
                            S.dma("sp", ws_g.ap, moe_w_gate[i, e_][:, p_ * 256:(p_ + 1) * 256].rearrange("(k q) f -> q k f", q=128), writes=[ws_g])
                            S.dma("sp", ws_u.ap, moe_w_up[i, e_][:, p_ * 256:(p_ + 1) * 256].rearrange("(k q) f -> q k f", q=128), writes=[ws_u])
                            w = wds[k % 2]
                            S.dma("sp", w.ap, moe_w_down[i, e_, p_ * 256:(p_ + 1) * 256, :].rearrange("(c q) d -> q c d", q=128), writes=[w])
                            S.op("dve", lambda a=wb_g, b=ws_g: V.tensor_copy(a.ap, b.ap), reads=[ws_g], writes=[wb_g])
                            S.op("dve", lambda a=wb_u, b=ws_u: V.tensor_copy(a.ap, b.ap), reads=[ws_u], writes=[wb_u])
                            S.op("pool", lambda w=w, p_=p_: G.tensor_copy(wdb.ap[:, p_ * 2:(p_ + 1) * 2, :], w.ap), reads=[w], writes=[wdb])

                        if e == 0:
                            prep(0)
                        for p in range(8):
                            k = e * 8 + p
                            wb_g, wb_u = wgb[k % 2], wub[k % 2]
                            if k + 1 < NE * 8 and (p < 7):
                                prep(k + 1)
                            for fsub in range(2):
                                fc = p * 2 + fsub
                                gl_, ul_ = Gl[fi % 2], Ul[fi % 2]
                                sgt = sg[fi % 2]
                                fi += 1
                                fs = slice(fsub * 128, (fsub + 1) * 128)
                                S.group("pe", [lambda kc=kc, gl_=gl_, wb_g=wb_g, fs=fs, xt=xt: T.matmul(
                                    gl_.ap[:, 0:512], wb_g.ap[:, kc, fs], xt.ap[:, kc, 0:512], start=(kc == 0), stop=(kc == KC - 1))
                                    for kc in range(KC)], reads=[wb_g, xt], writes=[gl_])
                                S.group("pe", [lambda kc=kc, ul_=ul_, wb_u=wb_u, fs=fs, xt=xt: T.matmul(
                                    ul_.ap[:, 0:512], wb_u.ap[:, kc, fs], xt.ap[:, kc, 0:512], start=(kc == 0), stop=(kc == KC - 1))
                                    for kc in range(KC)], reads=[wb_u, xt], writes=[ul_])
                                if with_ctx:
                                    S.group("pe", [lambda kc=kc, wb_g=wb_g, fs=fs, xt=xt: T.matmul(
                                        GUc.ap[:, 0:64], wb_g.ap[:, kc, fs], xt.ap[:, kc, 512:576], start=(kc == 0), stop=(kc == KC - 1))
                                        for kc in range(KC)], reads=[wb_g, xt], writes=[GUc])
                                    S.group("pe", [lambda kc=kc, wb_u=wb_u, fs=fs, xt=xt: T.matmul(
                                        GUc.ap[:, 64:128], wb_u.ap[:, kc, fs], xt.ap[:, kc, 512:576], start=(kc == 0), stop=(kc == KC - 1))
                                        for kc in range(KC)], reads=[wb_u, xt], writes=[GUc])
                                S.op("act", lambda sgt=sgt, gl_=gl_: A.activation(sgt.ap[:, 0:512], gl_.ap, AF.Silu), reads=[gl_], writes=[sgt])
                                S.op("dve", lambda sgt=sgt, ul_=ul_, fc=fc: V.tensor_tensor(hidT.ap[:, fc, 0:512], sgt.ap[:, 0:512], ul_.ap, ALU.mult),
                                     reads=[sgt, ul_], writes=[hidT])
                                if with_ctx:
                                    S.op("act", lambda sgt=sgt: A.activation(sgt.ap[:, 512:576], GUc.ap[:, 0:64], AF.Silu), reads=[GUc], writes=[sgt])
                                    S.op("dve", lambda sgt=sgt, fc=fc: V.tensor_tensor(hidT.ap[:, fc, 512:576], sgt.ap[:, 512:576], GUc.ap[:, 64:128], ALU.mult),
                                         reads=[sgt, GUc], writes=[hidT])
                        if e + 1 < NE:
                            gather(e + 1)
                        for dc in range(KC):
                            ds_ = slice(dc * 128, (dc + 1) * 128)
                            Yl = Yl2[dc % 2]
                            S.group("pe", [lambda fc=fc, ds_=ds_, Yl=Yl: T.matmul(Yl.ap[:, 0:512], wdb.ap[:, fc, ds_], hidT.ap[:, fc, 0:512],
                                                                           start=(fc == 0), stop=(fc == 15)) for fc in range(16)],
                                    reads=[wdb, hidT], writes=[Yl])
                            S.op("act", lambda dc=dc, Yl=Yl: A.activation(yT.ap[:, dc, 0:512], Yl.ap, AF.Copy), reads=[Yl], writes=[yT])
                            if with_ctx:
                                S.group("pe", [lambda fc=fc, ds_=ds_: T.matmul(Yc.ap[:, 0:64], wdb.ap[:, fc, ds_], hidT.ap[:, fc, 512:576],
                                                                               start=(fc == 0), stop=(fc == 15)) for fc in range(16)],
                                        reads=[wdb, hidT], writes=[Yc])
                                S.op("act", lambda dc=dc: A.activation(yT.ap[:, dc, 512:576], Yc.ap[:, 0:64], AF.Copy), reads=[Yc], writes=[yT])
                        if e + 1 < NE:
                            prep((e + 1) * 8)
                        sc_prev, sc_cur = sc_cur, {}
                        for (s, slot, cn, col0, src, srcb) in gl:
                            y = yo[yi % 4]
                            yi += 1
                            for half in range(2):
                                S.group("pe", [lambda j=j, half=half, col0=col0, cn=cn: T.transpose(
                                    ptr.ap[0:cn, j * 128:(j + 1) * 128], yT.ap[:, half * 4 + j, col0:col0 + cn], ident.ap)
                                    for j in range(4)], reads=[yT, ident], writes=[ptr])
                                S.op("dve", lambda y=y, half=half, cn=cn, s=s, slot=slot, e=e: V.tensor_scalar(
                                    y.ap[0:cn, half * 512:(half + 1) * 512], ptr.ap[0:cn, :], valT[s].ap[0:cn, slot, e:e + 1], None, ALU.mult),
                                    reads=[ptr, valT[s]], writes=[y])
                            dstb = mal_b[s] if slot < 2 else mac_b[s]
                            for t_ in sc_prev.get(dstb.name, []):
                                S._wait("pool", t_)
                            S._wait("pool", dstb.writer)
                            tk_ = S.idma(out=macc_t.rearrange("s t d -> (s t) d"),
                                         out_offset=bass.IndirectOffsetOnAxis(ap=idxT[s].ap[0:cn, slot, e:e + 1], axis=0),
                                         in_=y.ap[0:cn, :], in_offset=None, compute_op=ALU.add,
                                         element_offset=(s * NT + (0 if slot < 2 else NL)) * D,
                                         reads=[y, idxT[s]], writes=[])
                            sc_cur.setdefault(dstb.name, []).append(tk_)
                            sc_all.append(tk_)

                    for t_ in sc_all[-12:]:
                        S._wait("pool", t_)
                    for s_ in range(NSAMP):
                        for b_ in (mal_b[s_], mac_b[s_]):
                            S.op("pool", lambda: G.memset(zeros.ap[:, 0:1], 0.0), reads=[], writes=[b_])
                with Phase(nc, S, f"moeU{i}") as ph:
                    xb = ph.rot("xb", 2, [128, KC, 512], F32)
                    mt = ph.rot("mt", 2, [128, D], F32)
                    pt = [ph.ps(f"pt{k}", [128, 512]) for k in range(2)]
                    it = 0
                    mi = 0
                    for s in range(NSAMP):
                        for (t0, n) in blocks:
                            v = s if t0 < NL else 2
                            x = xb[it % 2]
                            it += 1
                            S.dma("sp", x.ap[:, :, :n], xT_view(s, t0, n), reads=[xT_b[s]], writes=[x])
                            for tt in range(n // 128):
                                m = mt[mi % 2]
                                mi += 1
                                r0 = t0 + tt * 128
                                S.dma("act", m.ap, macc_t[s, r0:r0 + 128, :], reads=[mal_b[s] if r0 < NL else mac_b[s]], writes=[m])
                                for half in range(2):
                                    p = pt[half]
                                    S.group("pe", [lambda j=j, half=half, m=m, p=p: T.transpose(
                                        p.ap[:, j * 128:(j + 1) * 128], m.ap[:, (half * 4 + j) * 128:(half * 4 + j + 1) * 128], ident.ap)
                                        for j in range(4)], reads=[m, ident], writes=[p])
                                    for j in range(4):
                                        kc = half * 4 + j
                                        S.op("dve", lambda j=j, kc=kc, p=p, x=x, tt=tt, v=v: V.scalar_tensor_tensor(
                                            x.ap[:, kc, tt * 128:(tt + 1) * 128], p.ap[:, j * 128:(j + 1) * 128], mod.ap[:, 40 + kc, v:v + 1],
                                            x.ap[:, kc, tt * 128:(tt + 1) * 128], ALU.mult, ALU.add), reads=[p, mod, x], writes=[x])
                            S.dma("sp", xT_view(s, t0, n), x.ap[:, :, :n], reads=[x], writes=[xT_b[s]])

        def final():
            with Phase(nc, S, "fin") as ph:
                R = norm_resources(ph)
                fg = ph.sb("fg", [128, KC], F32)
                S.dma("sp", fg.ap, final_g.rearrange("(k p) -> p k", p=128), writes=[fg], allow_slow_non_contiguous=True)
                xn = ph.sb("xn", [128, KC, 512], F32)
                pt = [ph.ps(f"pt{k}", [128, 512]) for k in range(2)]
                ot = ph.rot("ot", 2, [128, D], F32)
                it = 0
                oi = 0
                for s in range(NSAMP):
                    for (t0, n) in BLOCKS[:4]:
                        xb = R["xb"][it % 2]
                        sq = R["sq"]
                        S.dma("sp" if it % 2 == 0 else "act", xb.ap, xT_view(s, t0, n), reads=[xT_b[s]], writes=[xb])
                        it += 1
                        S.op("act", lambda xb=xb: A.activation(sq.ap, xb.ap, AF.Square), reads=[xb], writes=[sq])
                        ssp = R["ssp"]
                        S.group("pe", [lambda kc=kc: T.matmul(ssp.ap, ones.ap, sq.ap[:, kc, :], start=(kc == 0), stop=(kc == KC - 1))
                                       for kc in range(KC)], reads=[ones, sq], writes=[ssp])
                        rstd = R["rstd"]
                        S.op("act", lambda: A.activation(rstd.ap, ssp.ap, AF.Sqrt, bias=R["eps"].ap, scale=1.0 / D), reads=[ssp, R["eps"]], writes=[rstd])
                        S.op("dve", lambda: V.reciprocal(rstd.ap, rstd.ap), reads=[rstd], writes=[rstd])
                        for kc in range(KC):
                            S.op("dve", lambda kc=kc, xb=xb: V.scalar_tensor_tensor(xn.ap[:, kc, :], xb.ap[:, kc, :], fg.ap[:, kc:kc + 1], rstd.ap,
                                                                                    ALU.mult, ALU.mult), reads=[xb, fg, rstd], writes=[xn])
                        for tt in range(4):
                            o = ot[oi % 2]
                            oi += 1
                            for half in range(2):
                                p = pt[half]
                                S.group("pe", [lambda j=j, half=half, tt=tt, p=p: T.transpose(
                                    p.ap[:, j * 128:(j + 1) * 128], xn.ap[:, half * 4 + j, tt * 128:(tt + 1) * 128], ident.ap) for j in range(4)],
                                    reads=[xn, ident], writes=[p])
                                if half == 0:
                                    S.op("dve", lambda o=o, p=p: V.tensor_copy(o.ap[:, 0:512], p.ap), reads=[p], writes=[o])
                                else:
                                    S.op("act", lambda o=o, p=p: A.activation(o.ap[:, 512:1024], p.ap, AF.Copy), reads=[p], writes=[o])
                            r0 = t0 + tt * 128
                            S.dma("sp" if oi % 2 == 0 else "act", out_d[s, r0:r0 + 128, :], o.ap, reads=[o], is_output=True)


        def attn(i):
            j = i // 2
            lam_init = 0.8 - 0.6 * math.exp(-0.3 * i)
            with Phase(nc, S, f"att{i}") as ph:
                PA = ph.ps("PA", [128, 512])
                PB = ph.ps("PB", [128, 512])
                Sp = [ph.ps(f"Sp{k}", [128, 512]) for k in range(2)]
                Op = [ph.ps(f"Op{k}", [128, 512]) for k in range(4)]
                R = norm_resources(ph, ssp=PA)
                hT = ph.sb("hT", [128, KC, NT], BF16)
                h32 = R["sq"]
                onT = ph.sb("onT", [128, 8, NT], BF16)
                ropec = ph.sb("ropec", [128, NL], F32)
                ropes = ph.sb("ropes", [128, NL], F32)
                S.dma("sp", ropec.ap, ropec_in, writes=[ropec])
                S.dma("act", ropes.ap, ropes_in, writes=[ropes])
                wo = ph.sb("wo", [128, 8, D], BF16)
                S.dma("pool", wo.ap, attn_w_o[j].rearrange("(k p) f -> p k f", p=128), writes=[wo])
                lt = ph.sb("lt", [64, 4], F32)
                for a in range(4):
                    S.dma("sp", lt.ap[:, a:a + 1], attn_lam[a][j].rearrange("(p o) -> p o", o=1), writes=[lt], allow_slow_non_contiguous=True)
                pr = ph.sb("pr", [64, 2], F32)
                S.op("dve", lambda: V.tensor_tensor(pr.ap[:, 0:1], lt.ap[:, 0:1], lt.ap[:, 1:2], ALU.mult), reads=[lt], writes=[pr])
                S.op("dve", lambda: V.tensor_tensor(pr.ap[:, 1:2], lt.ap[:, 2:3], lt.ap[:, 3:4], ALU.mult), reads=[lt], writes=[pr])
                S.group("pe", [lambda: T.matmul(PB.ap[:, 0:2], ones.ap[0:64, :], pr.ap, start=True, stop=True)], reads=[ones, pr], writes=[PB])
                ex = ph.sb("ex", [128, 2], F32)
                S.op("act", lambda: A.activation(ex.ap, PB.ap[:, 0:2], AF.Exp), reads=[PB], writes=[ex])
                neglam = ph.sb("neglam", [128, 1], F32)
                S.op("dve", lambda: V.tensor_tensor(neglam.ap, ex.ap[:, 1:2], ex.ap[:, 0:1], ALU.subtract), reads=[ex], writes=[neglam])
                S.op("dve", lambda: V.tensor_scalar(neglam.ap, neglam.ap, -lam_init, None, ALU.add), reads=[neglam], writes=[neglam])
                subg = ph.sb("subg", [128, 1], F32)
                S.dma("sp", subg.ap, attn_subln_g[j].rearrange("(p o) -> p o", o=1), writes=[subg], allow_slow_non_contiguous=True)
                S.op("dve", lambda: V.tensor_scalar(subg.ap, subg.ap, 1.0 - lam_init, None, ALU.mult), reads=[subg], writes=[subg])
                wq = ph.rot("wq", 2, [128, KC, 128], BF16)
                wk = ph.rot("wk", 2, [128, KC, 128], BF16)
                wv = ph.rot("wv", 2, [128, KC, 128], BF16)
                wqs = ph.rot("wqs", 2, [128, KC, 128], BF16)
                wks = ph.rot("wks", 2, [128, KC, 128], BF16)
                qT = ph.sb("qT", [128, NT], BF16)
                kT = ph.sb("kT", [128, NT], BF16)
                vx = ph.sb("vx", [128, NT // 128, 130], BF16)
                S.op("pool", lambda: G.memset(vx.ap, 1.0), writes=[vx])
                t1 = ph.rot("t1", 2, [128, 512], F32)
                t2 = ph.rot("t2", 2, [128, 512], F32)
                Et = ph.rot("Et", 2, [128, 512], BF16)
                otmp = ph.sb("otmp", [128, 4, 128], F32)
                osq = ph.sb("osq", [128, 128], F32)
                sm = ph.rot("sm", 4, [128, 4], F32)
                xb = R["xb"]
                wsrc = attn_w_qkv[j]

                def wview(base):
                    return wsrc[:, base:base + 128].rearrange("(k p) f -> p k f", p=128)

                def wview_sw(base, two):
                    return wsrc[:, base:base + 128].rearrange("(k p) (b two q) -> p k b two q", p=128, two=2, q=16)[:, :, :, two, :]

                it = 0
                hi = 0
                ei = 0
                for s in range(NSAMP):
                    for (t0, n) in BLOCKS:
                        norm_block(ph, R, s, t0, n, A1, 0, h32, hT.ap[:, :, t0:t0 + n], hT, it)
                        it += 1
                    for h in range(8):
                        Wq, Wk, Wv, Wqs, Wks = wq[hi % 2], wk[hi % 2], wv[hi % 2], wqs[hi % 2], wks[hi % 2]
                        hi += 1
                        S.dma("pool", Wq.ap, wview(h * 128), writes=[Wq])
                        S.dma("pool", Wk.ap, wview(D + h * 128), writes=[Wk])
                        S.dma("pool", Wv.ap, wview(2 * D + h * 128), writes=[Wv])
                        for two in range(2):
                            for (Wd_, Ws_) in ((Wqs, Wq), (Wks, Wk)):
                                S.op("pool", lambda Wd_=Wd_, Ws_=Ws_, two=two: G.tensor_copy(
                                    Wd_.ap.rearrange("p k (b two q) -> p k b two q", two=2, q=16)[:, :, :, two, :],
                                    Ws_.ap.rearrange("p k (b two q) -> p k b two q", two=2, q=16)[:, :, :, 1 - two, :]),
                                    reads=[Ws_], writes=[Wd_])
                        for (W, Wsw, dst) in ((Wq, Wqs, qT), (Wk, Wks, kT)):
                            for bx, (t0, n) in enumerate(BLOCKS):
                                pa, pb = (PA, PB) if bx % 2 == 0 else (Sp[0], Sp[1])
                                S.group("pe", [lambda kc=kc, W=W, t0=t0, n=n, pa=pa: T.matmul(pa.ap[:, :n], W.ap[:, kc, :], hT.ap[:, kc, t0:t0 + n],
                                                                                           start=(kc == 0), stop=(kc == KC - 1)) for kc in range(KC)],
                                        reads=[W, hT], writes=[pa])
                                if t0 < NL:
                                    S.group("pe", [lambda kc=kc, Wsw=Wsw, t0=t0, n=n, pb=pb: T.matmul(pb.ap[:, :n], Wsw.ap[:, kc, :], hT.ap[:, kc, t0:t0 + n],
                                                                                                   start=(kc == 0), stop=(kc == KC - 1)) for kc in range(KC)],
                                            reads=[Wsw, hT], writes=[pb])
                                    a1, a2 = t1[ei % 2], t2[ei % 2]
                                    ei += 1
                                    S.op("dve", lambda a1=a1, t0=t0, n=n, pa=pa: V.tensor_tensor(a1.ap[:, :n], pa.ap[:, :n], ropec.ap[:, t0:t0 + n], ALU.mult),
                                         reads=[pa, ropec], writes=[a1])
                                    S.op("dve", lambda a2=a2, t0=t0, n=n, pb=pb: V.tensor_tensor(a2.ap[:, :n], pb.ap[:, :n], ropes.ap[:, t0:t0 + n], ALU.mult),
                                         reads=[pb, ropes], writes=[a2])
                                    S.op("pool", lambda a1=a1, a2=a2, dst=dst, t0=t0, n=n: G.tensor_tensor(dst.ap[:, t0:t0 + n], a1.ap[:, :n], a2.ap[:, :n], ALU.add),
                                         reads=[a1, a2], writes=[dst])
                                else:
                                    S.op("act", lambda dst=dst, t0=t0, n=n, pa=pa: A.activation(dst.ap[:, t0:t0 + n], pa.ap[:, :n], AF.Copy), reads=[pa], writes=[dst])
                        for tt in range(NT // 128):
                            pv_ = (PA, PB, Sp[0], Sp[1])[tt % 4]
                            S.group("pe", [lambda kc=kc, tt=tt, Wv=Wv, pv_=pv_: T.matmul(pv_.ap[:, 0:128], hT.ap[:, kc, tt * 128:(tt + 1) * 128], Wv.ap[:, kc, :],
                                                                                          start=(kc == 0), stop=(kc == KC - 1)) for kc in range(KC)],
                                    reads=[Wv, hT], writes=[pv_])
                            S.op("act", lambda tt=tt, pv_=pv_: A.activation(vx.ap[:, tt, 0:128], pv_.ap[:, 0:128], AF.Copy), reads=[pv_], writes=[vx])
                        tasks = []
                        for (q0, qn, ktiles) in [(0, 512, list(range(18))), (512, 512, list(range(18))), (1024, 512, list(range(18))),
                                                 (1536, 512, list(range(18))), (2048, 256, [16, 17])]:
                            nq = qn // 128
                            for c in range(2):
                                cs = slice(c * 64, (c + 1) * 64)
                                for ki, kt in enumerate(ktiles):
                                    sp_, et_ = Sp[ei % 2], Et[ei % 2]
                                    ei += 1

                                    def fS(sp_=sp_, kt=kt, cs=cs, q0=q0, qn=qn):
                                        S.group("pe", [lambda: T.matmul(sp_.ap[:, :qn], kT.ap[cs, kt * 128:(kt + 1) * 128], qT.ap[cs, q0:q0 + qn], start=True, stop=True)],
                                                reads=[kT, qT], writes=[sp_])

                                    def fE(sp_=sp_, et_=et_, qn=qn):
                                        S.op("act", lambda: A.activation(et_.ap[:, :qn], sp_.ap[:, :qn], AF.Exp, scale=0.125), reads=[sp_], writes=[et_])

                                    def fPV(et_=et_, kt=kt, ki=ki, nk=len(ktiles), nq=nq):
                                        for qs in range(nq):
                                            S.group("pe", [lambda qs=qs: T.matmul(Op[qs].ap[:, 0:129], et_.ap[:, qs * 128:(qs + 1) * 128], vx.ap[:, kt, 0:129],
                                                                                   start=(ki == 0), stop=(ki == nk - 1))], reads=[et_, vx], writes=[Op[qs]])

                                    def fEpi(c=c, nq=nq, q0=q0, h=h):
                                        for qs in range(nq):
                                            m = sm[qs]
                                            S.op("dve", lambda m=m, qs=qs: V.reciprocal(m.ap[:, 0:1], Op[qs].ap[:, 128:129]), reads=[Op[qs]], writes=[m])
                                            if c == 0:
                                                S.op("dve", lambda m=m, qs=qs: V.tensor_scalar(otmp.ap[:, qs, :], Op[qs].ap[:, 0:128], m.ap[:, 0:1], None, ALU.mult),
                                                     reads=[Op[qs], m], writes=[otmp])
                                            else:
                                                S.op("dve", lambda m=m: V.tensor_tensor(m.ap[:, 1:2], m.ap[:, 0:1], neglam.ap, ALU.mult), reads=[m, neglam], writes=[m])
                                                S.op("dve", lambda m=m, qs=qs: V.scalar_tensor_tensor(otmp.ap[:, qs, :], Op[qs].ap[:, 0:128], m.ap[:, 1:2], otmp.ap[:, qs, :],
                                                                                                      ALU.mult, ALU.add), reads=[Op[qs], m, otmp], writes=[otmp])
                                                S.op("act", lambda m=m, qs=qs: A.activation(osq.ap, otmp.ap[:, qs, :], AF.Square, accum_out=m.ap[:, 2:3]),
                                                     reads=[otmp], writes=[osq, m])
                                                S.op("act", lambda m=m: A.activation(m.ap[:, 3:4], m.ap[:, 2:3], AF.Sqrt, bias=R["eps"].ap, scale=1.0 / 128),
                                                     reads=[m, R["eps"]], writes=[m])
                                                S.op("dve", lambda m=m: V.reciprocal(m.ap[:, 3:4], m.ap[:, 3:4]), reads=[m], writes=[m])
                                                S.op("dve", lambda m=m, qs=qs: V.tensor_scalar(otmp.ap[:, qs, :], otmp.ap[:, qs, :], m.ap[:, 3:4], None, ALU.mult),
                                                     reads=[otmp, m], writes=[otmp])
                                                S.group("pe", [lambda qs=qs: T.transpose(PA.ap[:, qs * 128:(qs + 1) * 128], otmp.ap[:, qs, :], ident.ap)],
                                                        reads=[otmp, ident], writes=[PA])
                                                S.op("dve", lambda qs=qs: V.tensor_scalar(onT.ap[:, h, q0 + qs * 128:q0 + (qs + 1) * 128],
                                                                                          PA.ap[:, qs * 128:(qs + 1) * 128], subg.ap[:, 0:1], None, ALU.mult),
                                                     reads=[PA, subg], writes=[onT])

                                    tasks.append((fS, fE, fPV, fEpi if ki == len(ktiles) - 1 else None))
                        tasks[0][0]()
                        for ti_, (fS, fE, fPV, fEpi) in enumerate(tasks):
                            if ti_ + 1 < len(tasks):
                                tasks[ti_ + 1][0]()
                            fE()
                            fPV()
                            if fEpi is not None:
                                fEpi()
                    for (t0, n) in BLOCKS:
                        v = s if t0 < NL else 2
                        x = xb[it % 2]
                        it += 1
                        S.dma("sp", x.ap[:, :, :n], xT_view(s, t0, n), reads=[xT_b[s]], writes=[x])
                        for fc in range(KC):
                            pp = PA if fc % 2 == 0 else PB
                            S.group("pe", [lambda hh=hh, fc=fc, pp=pp, t0=t0, n=n: T.matmul(pp.ap[:, :n], wo.ap[:, hh, fc * 128:(fc + 1) * 128], onT.ap[:, hh, t0:t0 + n],
                                                                                             start=(hh == 0), stop=(hh == 7)) for hh in range(8)],
                                    reads=[wo, onT], writes=[pp])
                            S.op("dve", lambda fc=fc, pp=pp, x=x, n=n, v=v: V.scalar_tensor_tensor(x.ap[:, fc, :n], pp.ap[:, :n], mod.ap[:, 16 + fc, v:v + 1],
                                                                                                   x.ap[:, fc, :n], ALU.mult, ALU.add), reads=[pp, mod, x], writes=[x])
                        S.dma("act", xT_view(s, t0, n), x.ap[:, :, :n], reads=[x], writes=[xT_b[s]])


        def s5(i):
            j = i // 2
            with_ctx = i < DEPTH - 1
            FB = [(0, 256), (256, 512), (768, 512), (1280, 512), (1792, 512)]
            TWO_PI = 2.0 * math.pi
            PIC = 3.1415925
            with Phase(nc, S, f"s5o{i}") as pho:
                R = norm_resources(pho)
                hF = pho.sb("hF", [128, KC, NT], BF16)
                PSre = pho.ps("PSre", [128, 512])
                PSim = pho.ps("PSim", [128, 512])
                PSy = pho.ps("PSy", [128, 512])
                PT = pho.ps("PT", [128, 512])
                PSre2 = pho.ps("PSre2", [128, 512])
                PSim2 = pho.ps("PSim2", [128, 512])
                PSy2 = pho.ps("PSy2", [128, 512])

                def P(name, shape=(128, 64), dt=F32):
                    return pho.sb(name, list(shape), dt)

                lre, lim, ldt, mag, ang, red, red2, sn, cs = [P(n) for n in ("lre", "lim", "ldt", "mag", "ang", "red", "red2", "sn", "cs")]
                abre, abim, nr, den, cre, cim, ncim, tq = [P(n) for n in ("abre", "abim", "nr", "den", "cre", "cim", "ncim", "tq")]
                ki = P("ki", dt=I32)
                rc = P("rc", (128, 12, 64))
                rsn = P("rsn", (128, 12, 64))
                nrs = P("nrs", (128, 12, 64))
                dsk = P("dsk", (128, KC))
                for (dst, src) in ((lre, ssm_lam_re), (lim, ssm_lam_im)):
                    for d in range(2):
                        S.dma("sp", dst.ap[:, d * 32:(d + 1) * 32], src[j, d].rearrange("g n -> (g n)").rearrange("(gp p) -> p gp", p=128),
                              writes=[dst], allow_slow_non_contiguous=True)
                S.dma("act", dsk.ap, ssm_d[j].rearrange("(k p) -> p k", p=128), writes=[dsk], allow_slow_non_contiguous=True)
                ldrow = P("ldrow", (1, 128))
                S.dma("sp", ldrow.ap, ssm_log_dt[j].rearrange("d g -> (d g)").rearrange("(o f) -> o f", o=1), writes=[ldrow])
                S.group("pe", [lambda: T.matmul(PT.ap[:, 0:128], ones.ap[0:1, :], ldrow.ap, start=True, stop=True)], reads=[ones, ldrow], writes=[PT])
                for g2 in range(2):
                    S.op("dve", lambda g2=g2: V.tensor_copy(
                        ldt.ap[g2 * 64:(g2 + 1) * 64, :].rearrange("p (d g) -> p d g", d=2),
                        PT.ap[g2 * 64:(g2 + 1) * 64, 0:128].rearrange("p (d g two) -> p d g two", d=2, two=2)[:, :, :, g2]),
                        reads=[PT], writes=[ldt])
                S.op("act", lambda: A.activation(ldt.ap, ldt.ap, AF.Exp), reads=[ldt], writes=[ldt])
                S.op("dve", lambda: V.tensor_tensor(mag.ap, lre.ap, ldt.ap, ALU.mult), reads=[lre, ldt], writes=[mag])
                S.op("act", lambda: A.activation(mag.ap, mag.ap, AF.Exp), reads=[mag], writes=[mag])
                S.op("dve", lambda: V.tensor_tensor(ang.ap, lim.ap, ldt.ap, ALU.mult), reads=[lim, ldt], writes=[ang])
                S.op("dve", lambda: V.tensor_scalar(ki.ap, ang.ap, 1.0 / TWO_PI, None, ALU.mult), reads=[ang], writes=[ki])
                S.op("dve", lambda: V.tensor_copy(tq.ap, ki.ap), reads=[ki], writes=[tq])
                S.op("dve", lambda: V.scalar_tensor_tensor(red.ap, tq.ap, -TWO_PI, ang.ap, ALU.mult, ALU.add), reads=[tq, ang], writes=[red])
                S.op("dve", lambda: V.tensor_scalar(red.ap, red.ap, PIC, -PIC, ALU.min, ALU.max), reads=[red], writes=[red])
                S.op("act", lambda: A.activation(sn.ap, red.ap, AF.Sin), reads=[red], writes=[sn])
                S.op("dve", lambda: V.tensor_scalar(tq.ap, red.ap, math.pi / 2, -TWO_PI, ALU.is_gt, ALU.mult), reads=[red], writes=[tq])
                S.op("dve", lambda: V.scalar_tensor_tensor(red2.ap, red.ap, math.pi / 2, tq.ap, ALU.add, ALU.add), reads=[red, tq], writes=[red2])
                S.op("dve", lambda: V.tensor_scalar(red2.ap, red2.ap, PIC, -PIC, ALU.min, ALU.max), reads=[red2], writes=[red2])
                S.op("act", lambda: A.activation(cs.ap, red2.ap, AF.Sin), reads=[red2], writes=[cs])
                S.op("dve", lambda: V.tensor_tensor(abre.ap, mag.ap, cs.ap, ALU.mult), reads=[mag, cs], writes=[abre])
                S.op("dve", lambda: V.tensor_tensor(abim.ap, mag.ap, sn.ap, ALU.mult), reads=[mag, sn], writes=[abim])
                S.op("dve", lambda: V.tensor_scalar(nr.ap, abre.ap, -1.0, None, ALU.add), reads=[abre], writes=[nr])
                S.op("dve", lambda: V.tensor_tensor(den.ap, lre.ap, lre.ap, ALU.mult), reads=[lre], writes=[den])
                S.op("dve", lambda: V.tensor_tensor(tq.ap, lim.ap, lim.ap, ALU.mult), reads=[lim], writes=[tq])
                S.op("dve", lambda: V.tensor_tensor(den.ap, den.ap, tq.ap, ALU.add), reads=[den, tq], writes=[den])
                S.op("dve", lambda: V.reciprocal(den.ap, den.ap), reads=[den], writes=[den])
                S.op("dve", lambda: V.tensor_tensor(cre.ap, nr.ap, lre.ap, ALU.mult), reads=[nr, lre], writes=[cre])
                S.op("dve", lambda: V.tensor_tensor(tq.ap, abim.ap, lim.ap, ALU.mult), reads=[abim, lim], writes=[tq])
                S.op("dve", lambda: V.tensor_tensor(cre.ap, cre.ap, tq.ap, ALU.add), reads=[cre, tq], writes=[cre])
                S.op("dve", lambda: V.tensor_tensor(cre.ap, cre.ap, den.ap, ALU.mult), reads=[cre, den], writes=[cre])
                S.op("dve", lambda: V.tensor_tensor(cim.ap, abim.ap, lre.ap, ALU.mult), reads=[abim, lre], writes=[cim])
                S.op("dve", lambda: V.tensor_tensor(tq.ap, nr.ap, lim.ap, ALU.mult), reads=[nr, lim], writes=[tq])
                S.op("dve", lambda: V.tensor_tensor(cim.ap, cim.ap, tq.ap, ALU.subtract), reads=[cim, tq], writes=[cim])
                S.op("dve", lambda: V.tensor_tensor(cim.ap, cim.ap, den.ap, ALU.mult), reads=[cim, den], writes=[cim])
                S.op("dve", lambda: V.tensor_scalar(ncim.ap, cim.ap, -1.0, None, ALU.mult), reads=[cim], writes=[ncim])
                S.op("dve", lambda: V.tensor_copy(rc.ap[:, 0, :], cs.ap), reads=[cs], writes=[rc])
                S.op("dve", lambda: V.tensor_copy(rsn.ap[:, 0, :], sn.ap), reads=[sn], writes=[rsn])
                for k in range(1, 12):
                    S.op("dve", lambda k=k: V.tensor_tensor(tq.ap, rsn.ap[:, k - 1, :], rsn.ap[:, k - 1, :], ALU.mult), reads=[rsn], writes=[tq])
                    S.op("dve", lambda k=k: V.tensor_tensor(rc.ap[:, k, :], rc.ap[:, k - 1, :], rc.ap[:, k - 1, :], ALU.mult), reads=[rc], writes=[rc])
                    S.op("dve", lambda k=k: V.tensor_tensor(rc.ap[:, k, :], rc.ap[:, k, :], tq.ap, ALU.subtract), reads=[rc, tq], writes=[rc])
                    S.op("dve", lambda k=k: V.scalar_tensor_tensor(rsn.ap[:, k, :], rc.ap[:, k - 1, :], 2.0, rsn.ap[:, k - 1, :], ALU.mult, ALU.mult),
                         reads=[rc, rsn], writes=[rsn])
                S.op("dve", lambda: V.tensor_scalar(nrs.ap, rsn.ap, -1.0, None, ALU.mult), reads=[rsn], writes=[nrs])

                bsrc = [pho.sb(f"bsrc{k}", [128, 64, 16], F32) for k in range(2)]
                csrc = [pho.sb(f"csrc{k}", [128, 64, 16], F32) for k in range(2)]
                for k, (bs_, cs_) in enumerate(((ssm_b_re, ssm_c_re), (ssm_b_im, ssm_c_im))):
                    for d in range(2):
                        S.dma("sp", bsrc[k].ap[:, d * 32:(d + 1) * 32, :],
                              bs_[j, d].rearrange("g n q -> (g n) q").rearrange("(gp p) q -> p gp q", p=128), writes=[bsrc[k]])
                        for g2 in range(2):
                            for gp_ in range(32):
                                S.dma("act" if g2 else "sp", csrc[k].ap[g2 * 64:(g2 + 1) * 64, d * 32 + gp_, :],
                                      cs_[j, d, gp_ * 2 + g2].rearrange("p n -> n p"), writes=[csrc[k]],
                                      allow_slow_non_contiguous=True)

                wB = [Buf(f"s5w{c}", s5w_t[c]) for c in range(64)]
                tB = [Buf(f"s5t{c}", s5tab_t[c]) for c in range(64)]
                with Phase(nc, S, f"s5p{i}") as ph:
                    pad_all = [[ph.sb(f"pad{k}_{r}", [128, 128], F32) for r in range(2)] for k in range(4)]
                    for k in range(4):
                        for r in range(2):
                            S.op("pool", lambda k=k, r=r: G.memset(pad_all[k][r].ap, 0.0), writes=[pad_all[k][r]])
                    bbt_all = ph.rot("bbt", 2, [128, 2, 16], F32)
                    W5_all = ph.rot("W5", 2, [128, 5, 128], BF16)
                    CS_all = ph.rot("CS", 2, [128, 2, 512], F32)
                    tta_all = ph.rot("tta", 2, [128, 256], F32)
                    ttb_all = ph.rot("ttb", 2, [128, 256], F32)
                    pi_ = 0
                    for d in range(2):
                        for gp in range(32):
                            gpl = gp % 4
                            col = d * 32 + gp
                            cc = slice(col, col + 1)
                            r_ = pi_ % 2
                            pi_ += 1
                            bbt, W5, CS, tta, ttb = bbt_all[r_], W5_all[r_], CS_all[r_], tta_all[r_], ttb_all[r_]
                            S.op("pool", lambda W5=W5: G.memset(W5.ap[:, 2:5, :], 0.0), writes=[W5])
                            for k in range(2):
                                pd = pad_all[gpl][k]
                                a_, b_ = (bsrc[0], bsrc[1]) if k == 0 else (bsrc[1], bsrc[0])
                                sc2 = ncim if k == 0 else cim
                                S.op("pool", lambda a_=a_, k=k, col=col, cc=cc, bbt=bbt: G.tensor_scalar(bbt.ap[:, k, :], a_.ap[:, col, :], cre.ap[:, cc], None, ALU.mult),
                                     reads=[a_, cre], writes=[bbt])
                                S.op("dve", lambda b_=b_, k=k, col=col, cc=cc, sc2=sc2, bbt=bbt: V.scalar_tensor_tensor(
                                    bbt.ap[:, k, :], b_.ap[:, col, :], sc2.ap[:, cc], bbt.ap[:, k, :], ALU.mult, ALU.add), reads=[b_, sc2, bbt], writes=[bbt])
                                for g2 in range(2):
                                    blk = (gpl * 2 + g2) * 16
                                    S.op("pool", lambda g2=g2, blk=blk, k=k, pd=pd, bbt=bbt: G.tensor_copy(pd.ap[g2 * 64:(g2 + 1) * 64, blk:blk + 16],
                                                                                                  bbt.ap[g2 * 64:(g2 + 1) * 64, k, :]), reads=[bbt], writes=[pd])
                                S.group("pe", [lambda pd=pd: T.transpose(PT.ap[:, 0:128], pd.ap, ident.ap)], reads=[pd, ident], writes=[PT])
                                S.op("act", lambda k=k, W5=W5: A.activation(W5.ap[:, k, :], PT.ap[:, 0:128], AF.Copy), reads=[PT], writes=[W5])
                            for g2 in range(2):
                                blk = (gpl * 2 + g2) * 16
                                for (kk, src_k, sgn) in ((2, 0, 1.0), (3, 0, -1.0), (4, 1, -1.0)):
                                    S.op("act", lambda g2=g2, blk=blk, kk=kk, src_k=src_k, sgn=sgn, col=col, W5=W5: A.activation(
                                        W5.ap[g2 * 64:(g2 + 1) * 64, kk, blk:blk + 16], csrc[src_k].ap[g2 * 64:(g2 + 1) * 64, col, :],
                                        AF.Copy, scale=sgn), reads=[csrc[src_k]], writes=[W5])
                            Ct_ap, St_ap = CS.ap[:, 0, :], CS.ap[:, 1, :]
                            S.op("pool", lambda Ct_ap=Ct_ap: G.memset(Ct_ap[:, 0:1], 1.0), writes=[CS])
                            S.op("pool", lambda St_ap=St_ap: G.memset(St_ap[:, 0:1], 0.0), writes=[CS])
                            for k in range(9):
                                L = 1 << k
                                ck, sk, nsk = rc.ap[:, k, cc], rsn.ap[:, k, cc], nrs.ap[:, k, cc]
                                S.op("act", lambda L=L, nsk=nsk, tta=tta, St_ap=St_ap: A.activation(tta.ap[:, 0:L], St_ap[:, 0:L], AF.Copy, scale=nsk), reads=[CS, nrs], writes=[tta])
                                S.op("act", lambda L=L, sk=sk, ttb=ttb, Ct_ap=Ct_ap: A.activation(ttb.ap[:, 0:L], Ct_ap[:, 0:L], AF.Copy, scale=sk), reads=[CS, rsn], writes=[ttb])
                                S.op("dve", lambda L=L, ck=ck, tta=tta, Ct_ap=Ct_ap: V.scalar_tensor_tensor(Ct_ap[:, L:2 * L], Ct_ap[:, 0:L], ck, tta.ap[:, 0:L], ALU.mult, ALU.add),
                                     reads=[CS, rc, tta], writes=[CS])
                                S.op("dve", lambda L=L, ck=ck, ttb=ttb, St_ap=St_ap: V.scalar_tensor_tensor(St_ap[:, L:2 * L], St_ap[:, 0:L], ck, ttb.ap[:, 0:L], ALU.mult, ALU.add),
                                     reads=[CS, rc, ttb], writes=[CS])
                            S.dma("sp", s5w_t[col].rearrange("k p f -> p k f"), W5.ap, reads=[W5], writes=[wB[col]])
                            S.dma("act", s5tab_t[col].rearrange("k p f -> p k f"), CS.ap, reads=[CS], writes=[tB[col]])

                for s in range(NSAMP):
                    it = 0
                    for (t0, n) in BLOCKS:
                        pos = t0 + NCX if t0 < NL else 0
                        norm_block(pho, R, s, t0, n, A1, 0, R["sq"], hF.ap[:, :, pos:pos + n], hF, it)
                        it += 1
                    with Phase(nc, S, f"s5s{i}_{s}") as ph:
                        hBk = ph.sb("hBk", [128, NT], BF16)
                        W5_all = ph.rot("W5", 3, [128, 5, 128], BF16)
                        CS_all = ph.rot("CS", 3, [128, 2, 512], F32)
                        PSre_all = [PSre, PSre2]
                        PSim_all = [PSim, PSim2]
                        PSy_all = [PSy, PSy2]
                        wre = ph.rot("wre", 4, [128, 512], F32)
                        wim = ph.rot("wim", 4, [128, 512], F32)
                        prod = [ph.rot(f"prod{k}", 2, [128, 512], BF16) for k in range(4)]
                        ini = ph.rot("ini", 2, [128, 4], F32)
                        yacc = ph.sb("yacc", [128, NT], F32)
                        ga = ph.sb("ga", [128, NT], F32)
                        tm_all = [[ph.sb(f"tm{k}_{r}", [128, 512], F32) for k in range(4)] for r in range(2)]
                        bi = 0
                        pi_ = 0
                        for kc in range(KC):
                            S.op("act", lambda kc=kc: A.activation(hBk.ap[:, 0:NCX], hF.ap[:, kc, 0:NCX][:, ::-1], AF.Copy), reads=[hF], writes=[hBk])
                            S.op("act", lambda kc=kc: A.activation(hBk.ap[:, NCX:NT], hF.ap[:, kc, NCX:NT][:, ::-1], AF.Copy), reads=[hF], writes=[hBk])
                            S.op("pool", lambda: G.memset(yacc.ap, 0.0), writes=[yacc])
                            loads = []
                            tasks = []
                            for d in range(2):
                                for gpl in range(4):
                                    gp = kc * 4 + gpl
                                    col = d * 32 + gp
                                    cc = slice(col, col + 1)
                                    W5, CS = W5_all[pi_ % 3], CS_all[pi_ % 3]
                                    pi_ += 1

                                    def load(W5=W5, CS=CS, col=col):
                                        S.dma("sp", W5.ap, s5w_t[col].rearrange("k p f -> p k f"), reads=[wB[col]], writes=[W5])
                                        S.dma("act", CS.ap, s5tab_t[col].rearrange("k p f -> p k f"), reads=[tB[col]], writes=[CS])
                                    loads.append(load)
                                    prev = None
                                    for (p0, n) in FB:
                                        src = hF.ap[:, kc, p0:p0 + n] if d == 0 else hBk.ap[:, p0:p0 + n]
                                        sb_ = hF if d == 0 else hBk
                                        wr_, wi_ = wre[bi % 4], wim[bi % 4]
                                        pr_ = [prod[k][bi % 2] for k in range(4)]
                                        in_ = ini[bi % 2]
                                        tm = tm_all[bi % 2]
                                        Pre, Pim, Py = PSre_all[bi % 2], PSim_all[bi % 2], PSy_all[bi % 2]
                                        bi += 1
                                        cb, sb2 = CS.ap[:, 0, 0:n], CS.ap[:, 1, 0:n]

                                        def stA(src=src, sb_=sb_, n=n, W5=W5, CS=CS, Pre=Pre, Pim=Pim, tm=tm, wr_=wr_, wi_=wi_, cb=cb, sb2=sb2):
                                            S.group("pe", [lambda: T.matmul(Pre.ap[:, :n], W5.ap[:, 0, :], src, start=True, stop=True)], reads=[W5, sb_], writes=[Pre])
                                            S.group("pe", [lambda: T.matmul(Pim.ap[:, :n], W5.ap[:, 1, :], src, start=True, stop=True)], reads=[W5, sb_], writes=[Pim])
                                            S.op("dve", lambda: V.tensor_tensor(tm[0].ap[:, :n], Pre.ap[:, :n], cb, ALU.mult), reads=[Pre, CS], writes=[tm[0]])
                                            S.op("dve", lambda: V.tensor_tensor(tm[1].ap[:, :n], Pim.ap[:, :n], sb2, ALU.mult), reads=[Pim, CS], writes=[tm[1]])
                                            S.op("pool", lambda: G.tensor_tensor(wr_.ap[:, :n], tm[0].ap[:, :n], tm[1].ap[:, :n], ALU.add), reads=[tm[0], tm[1]], writes=[wr_])
                                            S.op("dve", lambda: V.tensor_tensor(tm[2].ap[:, :n], Pim.ap[:, :n], cb, ALU.mult), reads=[Pim, CS], writes=[tm[2]])
                                            S.op("dve", lambda: V.tensor_tensor(tm[3].ap[:, :n], Pre.ap[:, :n], sb2, ALU.mult), reads=[Pre, CS], writes=[tm[3]])
                                            S.op("pool", lambda: G.tensor_tensor(wi_.ap[:, :n], tm[2].ap[:, :n], tm[3].ap[:, :n], ALU.subtract), reads=[tm[2], tm[3]], writes=[wi_])

                                        def stB(prev=prev, in_=in_, wr_=wr_, wi_=wi_, n=n, cc=cc):
                                            if prev is not None:
                                                (pw_r, pw_i, pn) = prev
                                                lvl = 8 if pn == 256 else 9
                                                cl, sl, nsl = rc.ap[:, lvl, cc], rsn.ap[:, lvl, cc], nrs.ap[:, lvl, cc]
                                                er, ei_ = pw_r.ap[:, pn - 1:pn], pw_i.ap[:, pn - 1:pn]
                                                S.op("act", lambda: A.activation(in_.ap[:, 2:3], ei_, AF.Copy, scale=nsl), reads=[pw_i, nrs], writes=[in_])
                                                S.op("act", lambda: A.activation(in_.ap[:, 3:4], er, AF.Copy, scale=sl), reads=[pw_r, rsn], writes=[in_])
                                                S.op("act", lambda: A.activation(in_.ap[:, 0:1], er, AF.Identity, scale=cl, bias=in_.ap[:, 2:3]),
                                                     reads=[pw_r, rc, in_], writes=[in_])
                                                S.op("act", lambda: A.activation(in_.ap[:, 1:2], ei_, AF.Identity, scale=cl, bias=in_.ap[:, 3:4]),
                                                     reads=[pw_i, rc, in_], writes=[in_])
                                                i_re, i_im = in_.ap[:, 0:1], in_.ap[:, 1:2]
                                                rd = [in_]
                                            else:
                                                i_re, i_im = 0.0, 0.0
                                                rd = []
                                            S.op("dve", lambda: V.tensor_tensor_scan(
                                                wr_.ap[:, :n], mag.ap[:, cc].to_broadcast([128, n]), wr_.ap[:, :n], i_re, ALU.mult, ALU.add), reads=[wr_, mag] + rd, writes=[wr_])
                                            S.op("dve", lambda: V.tensor_tensor_scan(
                                                wi_.ap[:, :n], mag.ap[:, cc].to_broadcast([128, n]), wi_.ap[:, :n], i_im, ALU.mult, ALU.add), reads=[wi_, mag] + rd, writes=[wi_])

                                        def stC(n=n, p0=p0, d=d, W5=W5, CS=CS, wr_=wr_, wi_=wi_, pr_=pr_, Py=Py, cb=cb, sb2=sb2):
                                            S.op("dve", lambda: V.tensor_tensor(pr_[0].ap[:, :n], wr_.ap[:, :n], cb, ALU.mult), reads=[wr_, CS], writes=[pr_[0]])
                                            S.op("pool", lambda: G.tensor_tensor(pr_[1].ap[:, :n], wi_.ap[:, :n], sb2, ALU.mult), reads=[wi_, CS], writes=[pr_[1]])
                                            S.op("pool", lambda: G.tensor_tensor(pr_[2].ap[:, :n], wr_.ap[:, :n], sb2, ALU.mult), reads=[wr_, CS], writes=[pr_[2]])
                                            S.op("dve", lambda: V.tensor_tensor(pr_[3].ap[:, :n], wi_.ap[:, :n], cb, ALU.mult), reads=[wi_, CS], writes=[pr_[3]])
                                            lts = [2, 3, 4, 4]
                                            S.group("pe", [lambda q=q: T.matmul(Py.ap[:, :n], W5.ap[:, lts[q], :], pr_[q].ap[:, :n], start=(q == 0), stop=(q == 3))
                                                           for q in range(4)], reads=[W5] + pr_, writes=[Py])

                                        def stD(n=n, p0=p0, d=d, Py=Py):
                                            if d == 0:
                                                ya = yacc.ap[:, p0:p0 + n]
                                            elif p0 == 0:
                                                ya = yacc.ap[:, 0:NCX][:, ::-1]
                                            else:
                                                hi_ = NT - (p0 - NCX)
                                                ya = yacc.ap[:, hi_ - n:hi_][:, ::-1]
                                            S.op("dve", lambda: V.tensor_tensor(ya, ya, Py.ap[:, :n], ALU.add), reads=[Py, yacc], writes=[yacc])

                                        tasks.append((stA, stB, stC, len(loads) - 1 if p0 == 0 else None, stD))
                                        prev = (wr_, wi_, n)
                            loads[0]()
                            loads[1]()
                            tasks[0][0]()
                            tasks[1][0]()
                            for ti_, (stA, stB, stC, li, stD) in enumerate(tasks):
                                if li is not None and li + 2 < len(loads):
                                    loads[li + 2]()
                                stB()
                                if ti_ + 2 < len(tasks):
                                    tasks[ti_ + 2][0]()
                                if ti_ >= 1:
                                    tasks[ti_ - 1][4]()
                                stC()
                            tasks[-1][4]()
                            S.op("dve", lambda kc=kc: V.scalar_tensor_tensor(yacc.ap, hF.ap[:, kc, :], dsk.ap[:, kc:kc + 1], yacc.ap, ALU.mult, ALU.add),
                                 reads=[hF, dsk, yacc], writes=[yacc])
                            S.op("act", lambda: A.activation(ga.ap, yacc.ap, AF.Square), reads=[yacc], writes=[ga])
                            S.op("pool", lambda: G.tensor_scalar(ga.ap, ga.ap, 0.044715, 1.0, ALU.mult, ALU.add), reads=[ga], writes=[ga])
                            S.op("pool", lambda: G.tensor_tensor(ga.ap, ga.ap, yacc.ap, ALU.mult), reads=[ga, yacc], writes=[ga])
                            S.op("act", lambda: A.activation(ga.ap, ga.ap, AF.Tanh, scale=0.7978845608028654), reads=[ga], writes=[ga])
                            S.op("dve", lambda: V.scalar_tensor_tensor(ga.ap, ga.ap, 1.0, yacc.ap, ALU.add, ALU.mult), reads=[ga, yacc], writes=[ga])
                            S.op("act", lambda kc=kc: A.activation(hF.ap[:, kc, :], ga.ap, AF.Copy, scale=0.5), reads=[ga], writes=[hF])
                    with Phase(nc, S, f"s5g{i}_{s}") as ph:
                        w1 = ph.sb("w1", [128, KC, D], BF16)
                        w2 = ph.sb("w2", [128, KC, D], BF16)
                        S.dma("pool", w1.ap, ssm_w_glu1[j].rearrange("(k p) f -> p k f", p=128), writes=[w1])
                        S.dma("pool", w2.ap, ssm_w_glu2[j].rearrange("(k p) f -> p k f", p=128), writes=[w2])
                        sg_ = ph.rot("sg", 2, [128, 512], F32)
                        xb = R["xb"]
                        for bi, (p0, n) in enumerate(FB):
                            if p0 == 0 and not with_ctx:
                                continue
                            t0 = NL if p0 == 0 else p0 - NCX
                            v = 2 if p0 == 0 else s
                            x = xb[bi % 2]
                            S.dma("sp", x.ap[:, :, :n], xT_view(s, t0, n), reads=[xT_b[s]], writes=[x])
                            for fc in range(KC):
                                fs = slice(fc * 128, (fc + 1) * 128)
                                S.group("pe", [lambda kc=kc, fs=fs, p0=p0, n=n: T.matmul(PSre.ap[:, :n], w1.ap[:, kc, fs], hF.ap[:, kc, p0:p0 + n],
                                                                                         start=(kc == 0), stop=(kc == KC - 1)) for kc in range(KC)], reads=[w1, hF], writes=[PSre])
                                S.group("pe", [lambda kc=kc, fs=fs, p0=p0, n=n: T.matmul(PSim.ap[:, :n], w2.ap[:, kc, fs], hF.ap[:, kc, p0:p0 + n],
                                                                                         start=(kc == 0), stop=(kc == KC - 1)) for kc in range(KC)], reads=[w2, hF], writes=[PSim])
                                g_ = sg_[fc % 2]
                                S.op("act", lambda g_=g_, n=n: A.activation(g_.ap[:, :n], PSim.ap[:, :n], AF.Sigmoid), reads=[PSim], writes=[g_])
                                S.op("dve", lambda g_=g_, n=n: V.tensor_tensor(g_.ap[:, :n], g_.ap[:, :n], PSre.ap[:, :n], ALU.mult), reads=[g_, PSre], writes=[g_])
                                S.op("dve", lambda g_=g_, n=n, fc=fc, x=x, v=v: V.scalar_tensor_tensor(x.ap[:, fc, :n], g_.ap[:, :n], mod.ap[:, 16 + fc, v:v + 1], x.ap[:, fc, :n],
                                                                                                       ALU.mult, ALU.add), reads=[g_, mod, x], writes=[x])
                            S.dma("act", xT_view(s, t0, n), x.ap[:, :, :n], reads=[x], writes=[xT_b[s]])

        MIXERS = {0: attn, 1: s5}
        cur = -1
        for (i, part) in cfg:
            if i != cur:
                adaln(i)
                cur = i
            if part == "mix":
                MIXERS[i % 2](i)
            else:
                moe_full(i)
        final()
        S.finish()
    return nc, S


_CONST = {}


def _consts():
    if not _CONST:
        _CONST["k_ident"] = np.eye(128, dtype=np.float32)
        t = np.arange(NL)
        row = (t // 64).astype(np.float32)
        col = (t % 64).astype(np.float32)
        inv = (10000.0 ** (-np.arange(16, dtype=np.float32) / 16)).astype(np.float32)
        C = np.zeros((128, NL), np.float32)
        Sg = np.zeros((128, NL), np.float32)
        for p in range(128):
            dd = p % 64
            pos = row if dd < 32 else col
            ang = pos * inv[dd % 16]
            C[p] = np.cos(ang)
            Sg[p] = np.sin(ang) * (-1.0 if (dd % 32) < 16 else 1.0)
        _CONST["k_ropec"] = C
        _CONST["k_ropes"] = Sg
    return _CONST


_NC_CACHE = {}


def kernel(**inputs):
    n = 8
    if "nc" not in _NC_CACHE:
        _NC_CACHE["nc"] = build()[0]
    nc = _NC_CACHE["nc"]
    shared = {k: np.ascontiguousarray(v) for k, v in inputs.items() if k not in ("x", "c", "ctx")}
    shared.update(_consts())
    in_maps = []
    for r in range(n):
        m = dict(shared)
        m["x"] = np.ascontiguousarray(inputs["x"][2 * r:2 * r + 2])
        m["c"] = np.ascontiguousarray(inputs["c"][2 * r:2 * r + 2])
        m["ctx"] = np.ascontiguousarray(inputs["ctx"][2 * r:2 * r + 2])
        in_maps.append(m)
    res = run_bass_kernel_spmd(nc, in_maps, core_ids=list(range(n)))
    return np.concatenate([r["out"] for r in res.results], axis=0).astype(np.float32)
```
